# Optimizing a Trainium2 kernel written in Bass

```python
import functools
import jax
import jax.numpy as jnp
from jax import lax
import numpy as np

D_MODEL = 1024
BATCH = 4
SEQ = 8192
DEPTH = 4

GRID_W = 64
CTX_LEN = 256
N_MIXERS = 4
BLOCK = 128
ROPE_THETA = 10000.0
EPS = 1e-6
NEG_INF = -1e30
N_MOD = 6

A_HEADS = 8
A_KV_HEADS = 2
A_HEAD_DIM = 128

B_HEADS = 8
B_Q_LORA = 384
B_KV_LORA = 256
B_NOPE_DIM = 128
B_ROPE_DIM = 64
B_V_DIM = 128

C_WIDTH = 1024
C_GROUPS = 8
C_CHUNK = 128

D_HEADS = 16
D_KV_HEADS = 2
D_HEAD_DIM = 64
D_WINDOW = 128

FF_DENSE = 2816
N_EXPERTS = 8
TOP_K = 2
FF_EXPERT = 3584

kernel_name = 'hybrid_interleaved_diffusion_trunk'


def rmsnorm(t, g):
    tf = t.astype(jnp.float32)
    y = tf * lax.rsqrt(jnp.mean(tf * tf, axis=-1, keepdims=True) + EPS)
    return (y * g.astype(jnp.float32)).astype(t.dtype)


def layernorm(t, g, b):
    tf = t.astype(jnp.float32)
    mu = jnp.mean(tf, axis=-1, keepdims=True)
    var = jnp.mean(jnp.square(tf - mu), axis=-1, keepdims=True)
    y = (tf - mu) * lax.rsqrt(var + EPS)
    return (y * g.astype(jnp.float32) + b.astype(jnp.float32)).astype(t.dtype)


def modulate(t, g, shift, scale):
    return rmsnorm(t, g) * (1 + scale) + shift


def axial_rope_tables(rows, cols, dim):
    quarter = dim // 4
    inv_freq = ROPE_THETA ** (-jnp.arange(quarter, dtype=jnp.float32) / quarter)
    ang_r = rows.astype(jnp.float32)[:, None] * inv_freq
    ang_c = cols.astype(jnp.float32)[:, None] * inv_freq
    ang = jnp.concatenate([ang_r, ang_r, ang_c, ang_c], axis=-1)
    return jnp.cos(ang), jnp.sin(ang)


def apply_rope(t, cos, sin):
    shp = t.shape
    tr = t.reshape(shp[:-1] + (2, 2, shp[-1] // 4))
    rot = jnp.stack([-tr[..., 1, :], tr[..., 0, :]], axis=-2).reshape(shp)
    return t * cos[:, None, :].astype(t.dtype) + rot * sin[:, None, :].astype(t.dtype)


def softmax_with_sink(s, sink):
    m = jnp.maximum(jnp.max(s, axis=-1, keepdims=True), sink)
    e = jnp.exp(s - m)
    return e / (jnp.sum(e, axis=-1, keepdims=True) + jnp.exp(sink - m))


def dense_block_attention(q, k, v, scale, sink=None):
    B, L, Hk, G, dq = q.shape
    nb = L // BLOCK
    qb = jnp.moveaxis(q.reshape(B, nb, BLOCK, Hk, G, dq), 1, 0)

    def one_block(qi):
        s = jnp.einsum('bqhgd,bkhd->bhgqk', qi, k, preferred_element_type=jnp.float32) * scale
        if sink is None:
            p = jax.nn.softmax(s, axis=-1)
        else:
            p = softmax_with_sink(s, sink[None, :, :, None, None])
        return jnp.einsum('bhgqk,bkhd->bqhgd', p.astype(v.dtype), v)

    o = lax.map(one_block, qb)
    return jnp.moveaxis(o, 0, 1).reshape(B, L, Hk * G * v.shape[-1])


def window_sink_attention(q, k, v, kc, vc, sink, scale):
    B, S, Hk, G, dh = q.shape
    nb = S // BLOCK
    n_ctx = kc.shape[1]

    def neighbour_blocks(t):
        tb = jnp.pad(t.reshape(B, nb, BLOCK, Hk, t.shape[-1]), ((0, 0), (1, 1), (0, 0), (0, 0), (0, 0)))
        w = jnp.concatenate([tb[:, :-2], tb[:, 1:-1], tb[:, 2:]], axis=2)
        return jnp.moveaxis(w, 1, 0)

    kw, vw = neighbour_blocks(k), neighbour_blocks(v)
    qb = jnp.moveaxis(q.reshape(B, nb, BLOCK, Hk, G, dh), 1, 0)
    offs = jnp.arange(3 * BLOCK)
    rel = offs[None, :] - BLOCK - jnp.arange(BLOCK)[:, None]
    kblock = jnp.arange(nb)[:, None] + offs[None, :] // BLOCK - 1
    mask = (jnp.abs(rel) <= D_WINDOW)[None] & ((kblock >= 0) & (kblock < nb))[:, None, :]
    sink_b = sink[None, :, :, None, None]

    def one_block(args):
        qi, ki, vi, mi = args
        s_ctx = jnp.einsum('bqhgd,bkhd->bhgqk', qi, kc, preferred_element_type=jnp.float32) * scale
        s_win = jnp.einsum('bqhgd,bkhd->bhgqk', qi, ki, preferred_element_type=jnp.float32) * scale
        s = jnp.concatenate([s_ctx, jnp.where(mi, s_win, NEG_INF)], axis=-1)
        p = softmax_with_sink(s, sink_b).astype(v.dtype)
        return (jnp.einsum('bhgqk,bkhd->bqhgd', p[..., :n_ctx], vc)
                + jnp.einsum('bhgqk,bkhd->bqhgd', p[..., n_ctx:], vi))

    o = lax.map(one_block, (qb, kw, vw, mask))
    return jnp.moveaxis(o, 0, 1).reshape(B, S, Hk * G * v.shape[-1])


def gqa_project(t, wqkv, n_heads, n_kv, dh):
    B, L, _ = t.shape
    q, k, v = jnp.split(t @ wqkv, [n_heads * dh, (n_heads + n_kv) * dh], axis=-1)
    return q.reshape(B, L, n_heads, dh), k.reshape(B, L, n_kv, dh), v.reshape(B, L, n_kv, dh)


def mixer_gqa_axial(h, hc, rope, wqkv, q_norm, k_norm, wo, need_ctx):
    B, S, _ = h.shape
    C = hc.shape[1]
    G = A_HEADS // A_KV_HEADS
    scale = A_HEAD_DIM ** -0.5
    q, k, v = gqa_project(h, wqkv, A_HEADS, A_KV_HEADS, A_HEAD_DIM)
    qc, kc, vc = gqa_project(hc, wqkv, A_HEADS, A_KV_HEADS, A_HEAD_DIM)
    q = apply_rope(rmsnorm(q, q_norm), *rope)
    k = apply_rope(rmsnorm(k, k_norm), *rope)
    kc = rmsnorm(kc, k_norm)
    k_all = jnp.concatenate([kc, k], axis=1)
    v_all = jnp.concatenate([vc, v], axis=1)
    o = dense_block_attention(q.reshape(B, S, A_KV_HEADS, G, A_HEAD_DIM), k_all, v_all, scale) @ wo
    oc = None
    if need_ctx:
        qc = rmsnorm(qc, q_norm).reshape(B, C, A_KV_HEADS, G, A_HEAD_DIM)
        oc = dense_block_attention(qc, kc, vc, scale) @ wo
    return o, oc


def mla_project(t, rope, w_down, q_lora_norm, kv_lora_norm, w_uq, w_ukv):
    B, L, _ = t.shape
    dq, dkv, k_rope = jnp.split(t @ w_down, [B_Q_LORA, B_Q_LORA + B_KV_LORA], axis=-1)
    q = (rmsnorm(dq, q_lora_norm) @ w_uq).reshape(B, L, B_HEADS, B_NOPE_DIM + B_ROPE_DIM)
    kv = (rmsnorm(dkv, kv_lora_norm) @ w_ukv).reshape(B, L, B_HEADS, B_NOPE_DIM + B_V_DIM)
    q_nope, q_rope = jnp.split(q, [B_NOPE_DIM], axis=-1)
    k_nope, v = jnp.split(kv, [B_NOPE_DIM], axis=-1)
    k_rope = k_rope[:, :, None, :]
    if rope is not None:
        q_rope = apply_rope(q_rope, *rope)
        k_rope = apply_rope(k_rope, *rope)
    q = jnp.concatenate([q_nope, q_rope], axis=-1)[:, :, :, None, :]
    k = jnp.concatenate([k_nope, jnp.broadcast_to(k_rope, (B, L, B_HEADS, B_ROPE_DIM))], axis=-1)
    return q, k, v


def mixer_mla(h, hc, rope, w_down, q_lora_norm, kv_lora_norm, w_uq, w_ukv, wo, need_ctx):
    scale = (B_NOPE_DIM + B_ROPE_DIM) ** -0.5
    q, k, v = mla_project(h, rope, w_down, q_lora_norm, kv_lora_norm, w_uq, w_ukv)
    qc, kc, vc = mla_project(hc, None, w_down, q_lora_norm, kv_lora_norm, w_uq, w_ukv)
    k_all = jnp.concatenate([kc, k], axis=1)
    v_all = jnp.concatenate([vc, v], axis=1)
    o = dense_block_attention(q, k_all, v_all, scale) @ wo
    oc = dense_block_attention(qc, kc, vc, scale) @ wo if need_ctx else None
    return o, oc


def sgu_sequence(t, w_in, ln_g, ln_b, w_spatial, b_spatial, w_out):
    B, L, _ = t.shape
    u, v = jnp.split(jax.nn.gelu(t @ w_in, approximate=False), 2, axis=-1)
    v = layernorm(v, ln_g, ln_b).reshape(B, L // C_CHUNK, C_CHUNK, C_GROUPS, C_WIDTH // C_GROUPS)
    mixed = jnp.einsum('gpq,bnqgc->bnpgc', w_spatial, v) + b_spatial.T[:, :, None]
    return (u * mixed.reshape(B, L, C_WIDTH)) @ w_out


def mixer_swa_sink(h, hc, rope, wqkv, sinks, wo, need_ctx):
    B, S, _ = h.shape
    C = hc.shape[1]
    G = D_HEADS // D_KV_HEADS
    scale = D_HEAD_DIM ** -0.5
    q, k, v = gqa_project(h, wqkv, D_HEADS, D_KV_HEADS, D_HEAD_DIM)
    qc, kc, vc = gqa_project(hc, wqkv, D_HEADS, D_KV_HEADS, D_HEAD_DIM)
    q = apply_rope(q, *rope)
    k = apply_rope(k, *rope)
    sink = sinks.astype(jnp.float32).reshape(D_KV_HEADS, G)
    o = window_sink_attention(q.reshape(B, S, D_KV_HEADS, G, D_HEAD_DIM), k, v, kc, vc, sink, scale) @ wo
    oc = None
    if need_ctx:
        oc = dense_block_attention(qc.reshape(B, C, D_KV_HEADS, G, D_HEAD_DIM), kc, vc, scale, sink) @ wo
    return o, oc


def swiglu(t, w13, w2):
    a, b = jnp.split(t @ w13, 2, axis=-1)
    return (jax.nn.silu(a) * b) @ w2


def moe_swiglu(t, router, w13, w2):
    logits = jnp.einsum('bld,de->ble', t, router, preferred_element_type=jnp.float32)
    top_val, top_idx = lax.top_k(logits, TOP_K)
    weights = jax.nn.softmax(top_val, axis=-1)
    gates = jnp.einsum('blk,blke->ble', weights,
                       jax.nn.one_hot(top_idx, N_EXPERTS, dtype=jnp.float32)).astype(t.dtype)
    out = jnp.zeros_like(t)
    for e in range(N_EXPERTS):
        out = out + gates[..., e:e + 1] * swiglu(t, w13[e], w2[e])
    return out


def setup_inputs(seed: int = 0) -> dict:
    key = jax.random.key(seed)
    keys = iter(jax.random.split(key, 64))
    f32 = jnp.float32

    def normal(shape, std):
        return std * jax.random.normal(next(keys), shape, f32)

    def gain(shape):
        return 1.0 + 0.05 * jax.random.normal(next(keys), shape, f32)

    D = D_MODEL
    n_cyc = DEPTH // N_MIXERS
    n_pair = DEPTH // 2
    a_qkv = (A_HEADS + 2 * A_KV_HEADS) * A_HEAD_DIM
    d_qkv = (D_HEADS + 2 * D_KV_HEADS) * D_HEAD_DIM
    return {
        'x': normal((BATCH, SEQ, D), 1.0),
        'c': normal((BATCH, D), 1.0),
        'ctx': normal((BATCH, CTX_LEN, D), 1.0),
        'c_ctx': normal((D,), 1.0),
        'ada_w': normal((DEPTH, D, N_MOD * D), 0.5 * D ** -0.5),
        'ada_b': normal((DEPTH, N_MOD * D), 0.02),
        'norm_mix': gain((DEPTH, D)),
        'norm_ffn': gain((DEPTH, D)),
        'final_norm': gain((D,)),
        'a_wqkv': normal((n_cyc, D, a_qkv), D ** -0.5),
        'a_q_norm': gain((n_cyc, A_HEAD_DIM)),
        'a_k_norm': gain((n_cyc, A_HEAD_DIM)),
        'a_wo': normal((n_cyc, A_HEADS * A_HEAD_DIM, D), (A_HEADS * A_HEAD_DIM) ** -0.5),
        'b_w_down': normal((n_cyc, D, B_Q_LORA + B_KV_LORA + B_ROPE_DIM), D ** -0.5),
        'b_q_lora_norm': gain((n_cyc, B_Q_LORA)),
        'b_kv_lora_norm': gain((n_cyc, B_KV_LORA)),
        'b_w_uq': normal((n_cyc, B_Q_LORA, B_HEADS * (B_NOPE_DIM + B_ROPE_DIM)), B_Q_LORA ** -0.5),
        'b_w_ukv': normal((n_cyc, B_KV_LORA, B_HEADS * (B_NOPE_DIM + B_V_DIM)), B_KV_LORA ** -0.5),
        'b_wo': normal((n_cyc, B_HEADS * B_V_DIM, D), (B_HEADS * B_V_DIM) ** -0.5),
        'c_w_in': normal((n_cyc, D, 2 * C_WIDTH), D ** -0.5),
        'c_ln_g': gain((n_cyc, C_WIDTH)),
        'c_ln_b': normal((n_cyc, C_WIDTH), 0.02),
        'c_w_spatial': normal((n_cyc, C_GROUPS, C_CHUNK, C_CHUNK), C_CHUNK ** -0.5),
        'c_b_spatial': 1.0 + normal((n_cyc, C_GROUPS, C_CHUNK), 0.1),
        'c_w_out': normal((n_cyc, C_WIDTH, D), C_WIDTH ** -0.5),
        'd_wqkv': normal((n_cyc, D, d_qkv), D ** -0.5),
        'd_sinks': normal((n_cyc, D_HEADS), 1.0),
        'd_wo': normal((n_cyc, D_HEADS * D_HEAD_DIM, D), (D_HEADS * D_HEAD_DIM) ** -0.5),
        'ffn_w13': normal((n_pair, D, 2 * FF_DENSE), D ** -0.5),
        'ffn_w2': normal((n_pair, FF_DENSE, D), FF_DENSE ** -0.5),
        'moe_router': normal((n_pair, D, N_EXPERTS), D ** -0.5),
        'moe_w13': normal((n_pair, N_EXPERTS, D, 2 * FF_EXPERT), D ** -0.5),
        'moe_w2': normal((n_pair, N_EXPERTS, FF_EXPERT, D), FF_EXPERT ** -0.5),
    }


def reference(x, c, ctx, c_ctx, ada_w, ada_b, norm_mix, norm_ffn, final_norm,
              a_wqkv, a_q_norm, a_k_norm, a_wo,
              b_w_down, b_q_lora_norm, b_kv_lora_norm, b_w_uq, b_w_ukv, b_wo,
              c_w_in, c_ln_g, c_ln_b, c_w_spatial, c_b_spatial, c_w_out,
              d_wqkv, d_sinks, d_wo,
              ffn_w13, ffn_w2, moe_router, moe_w13, moe_w2):
    B, S, _ = x.shape
    n_rows = S // GRID_W
    rows = jnp.repeat(jnp.arange(n_rows, dtype=jnp.int32), GRID_W)
    cols = jnp.tile(jnp.arange(GRID_W, dtype=jnp.int32), n_rows)
    ropes = {d: axial_rope_tables(rows, cols, d) for d in sorted({A_HEAD_DIM, B_ROPE_DIM, D_HEAD_DIM})}

    mod_lat = (jnp.einsum('bd,ldm->lbm', jax.nn.silu(c), ada_w) + ada_b[:, None, :])[:, :, None, :]
    mod_ctx = (jnp.einsum('d,ldm->lm', jax.nn.silu(c_ctx), ada_w) + ada_b)[:, None, None, :]

    h, hc = x, ctx
    for i in range(DEPTH):
        j, kind = i // N_MIXERS, i % N_MIXERS
        need_ctx = i < DEPTH - 1
        sh1, sc1, g1, sh2, sc2, g2 = jnp.split(mod_lat[i], N_MOD, axis=-1)
        csh1, csc1, cg1, csh2, csc2, cg2 = jnp.split(mod_ctx[i], N_MOD, axis=-1)
        a = modulate(h, norm_mix[i], sh1, sc1)
        ac = modulate(hc, norm_mix[i], csh1, csc1)
        if kind == 0:
            o, oc = mixer_gqa_axial(a, ac, ropes[A_HEAD_DIM], a_wqkv[j], a_q_norm[j], a_k_norm[j], a_wo[j], need_ctx)
        elif kind == 1:
            o, oc = mixer_mla(a, ac, ropes[B_ROPE_DIM], b_w_down[j], b_q_lora_norm[j], b_kv_lora_norm[j],
                              b_w_uq[j], b_w_ukv[j], b_wo[j], need_ctx)
        elif kind == 2:
            sgu = functools.partial(sgu_sequence, w_in=c_w_in[j], ln_g=c_ln_g[j], ln_b=c_ln_b[j],
                                    w_spatial=c_w_spatial[j], b_spatial=c_b_spatial[j], w_out=c_w_out[j])
            o = sgu(a)
            oc = sgu(ac) if need_ctx else None
        else:
            o, oc = mixer_swa_sink(a, ac, ropes[D_HEAD_DIM], d_wqkv[j], d_sinks[j], d_wo[j], need_ctx)
        h = h + g1 * o

        p = i // 2
        if i % 2 == 0:
            ffn = functools.partial(swiglu, w13=ffn_w13[p], w2=ffn_w2[p])
        else:
            ffn = functools.partial(moe_swiglu, router=moe_router[p], w13=moe_w13[p], w2=moe_w2[p])
        h = h + g2 * ffn(modulate(h, norm_ffn[i], sh2, sc2))
        if need_ctx:
            hc = hc + cg1 * oc
            hc = hc + cg2 * ffn(modulate(hc, norm_ffn[i], csh2, csc2))
    return rmsnorm(h, final_norm)
```

```python
import contextlib
import numpy as np
import concourse.bass as bass
import concourse.mybir as mybir
from concourse.bass_utils import run_bass_kernel_spmd

F32 = mybir.dt.float32
BF16 = mybir.dt.bfloat16
AF = mybir.ActivationFunctionType
ALU = mybir.AluOpType
AX = mybir.AxisListType

COMPUTE = ("pe", "act", "dve", "pool")
QUEUES = ("sp", "act", "pool")
STREAMS = ("pe", "act", "dve", "pool", "sp")
EPS = 1e-6


class Res:
    __slots__ = ("name", "last_w", "readers", "dma_sem", "dma_cnt")

    def __init__(self, name):
        self.name = name
        self.last_w = None
        self.readers = []
        self.dma_sem = None
        self.dma_cnt = 0


class Op:
    __slots__ = ("idx", "eng", "fn", "is_dma", "waits", "inc", "count", "res", "dma_val", "deps", "dma_sem")

    def __init__(self, idx, eng, fn, is_dma):
        self.idx = idx
        self.eng = eng
        self.fn = fn
        self.is_dma = is_dma
        self.waits = {}
        self.inc = False
        self.count = None
        self.res = None
        self.dma_val = None
        self.deps = ()


class Prog:
    def __init__(self):
        self.ops = []
        self.n_dma_sems = 0
        self.last_compute = {}
        self.last_dma = {}
        self.free_sems = []

    def _add(self, eng, fn, r, w, is_dma, sem_res=None):
        op = Op(len(self.ops), eng, fn, is_dma)
        deps = set()
        for x in r:
            if x.last_w is not None:
                deps.add(x.last_w)
        for x in w:
            if x.last_w is not None:
                deps.add(x.last_w)
            deps.update(x.readers)
        op.deps = deps
        for x in r:
            if not is_dma:
                x.readers = [i for i in x.readers if self.ops[i].is_dma or self.ops[i].eng != eng]
            x.readers.append(op.idx)
        for x in w:
            x.last_w = op.idx
            x.readers = []
        if is_dma:
            if sem_res.dma_sem is None:
                if self.free_sems:
                    sem_res.dma_sem, sem_res.dma_cnt = self.free_sems.pop()
                else:
                    sem_res.dma_sem = self.n_dma_sems
                    self.n_dma_sems += 1
            sem_res.dma_cnt += 16
            op.res = sem_res
            op.dma_sem = sem_res.dma_sem
            op.dma_val = sem_res.dma_cnt
            self.last_dma[sem_res.dma_sem] = op.idx
        else:
            self.last_compute[eng] = op.idx
        self.ops.append(op)
        return op

    def op(self, eng, fn, r=(), w=()):
        return self._add(eng, fn, list(r), list(w), False)

    def dma(self, q, out, in_, r=(), w=(), sem=None):
        return self._add(q, (out, in_), list(r), list(w), True, sem_res=sem)

    def barrier(self):
        deps = set(self.last_compute.values()) | set(self.last_dma.values())
        for s in STREAMS:
            op = Op(len(self.ops), s, None, False)
            op.deps = set(deps)
            self.ops.append(op)

    def plan(self):
        ops = self.ops
        for op in ops:
            for d in op.deps:
                if not ops[d].is_dma:
                    ops[d].inc = True
        cnt = {e: 0 for e in COMPUTE}
        for op in ops:
            if not op.is_dma and op.fn is not None and op.inc:
                cnt[op.eng] += 1
                op.count = cnt[op.eng]
        known = {s: {} for s in STREAMS}
        for op in ops:
            kn = known[op.eng]
            for d in op.deps:
                dop = ops[d]
                if dop.is_dma:
                    key, val = ("d", dop.dma_sem), dop.dma_val
                else:
                    key, val = ("c", dop.eng), dop.count
                if kn.get(key, 0) >= val:
                    continue
                if op.waits.get(key, 0) < val:
                    op.waits[key] = val
            for k, v in op.waits.items():
                kn[k] = max(kn.get(k, 0), v)
        self.final_counts = cnt

    def emit(self, nc):
        self.plan()
        ops = self.ops
        with contextlib.ExitStack() as st:
            csem = {e: st.enter_context(nc.semaphore("c_" + e)) for e in COMPUTE}
            dsem = [st.enter_context(nc.semaphore(f"d{i}")) for i in range(self.n_dma_sems)]
            block = st.enter_context(nc.Block())

            def semof(key):
                return csem[key[1]] if key[0] == "c" else dsem[key[1]]

            streams = {s: [] for s in STREAMS}
            for op in ops:
                streams[op.eng].append(op)
            seen = {}
            for op in ops:
                if op.is_dma:
                    seen[op.dma_sem] = max(seen.get(op.dma_sem, 0), op.dma_val)

            def run(engname, eng):
                for op in streams[engname]:
                    for key, val in op.waits.items():
                        eng.wait_ge(semof(key), val)
                    if op.fn is None:
                        continue
                    if op.is_dma:
                        out, in_ = op.fn
                        eng.dma_start(out=out, in_=in_).then_inc(dsem[op.dma_sem], 16)
                    else:
                        ins = op.fn(eng)
                        if op.inc:
                            ins.then_inc(csem[engname], 1)

            @block.tensor
            def _(e):
                run("pe", e)

            @block.scalar
            def _(e):
                run("act", e)

            @block.vector
            def _(e):
                run("dve", e)

            @block.gpsimd
            def _(e):
                run("pool", e)

            @block.sync
            def _(e):
                run("sp", e)
                for k, v in seen.items():
                    e.wait_ge(dsem[k], v)
                for en in COMPUTE:
                    if self.final_counts[en]:
                        e.wait_ge(csem[en], self.final_counts[en])


class Rot:
    def __init__(self, items):
        self.items = items
        self.i = 0

    def next(self):
        it = self.items[self.i % len(self.items)]
        self.i += 1
        return it


class KB:
    def __init__(self, nc):
        self.nc = nc
        self.P = Prog()
        self.gst = contextlib.ExitStack()
        self.cur = self.gst
        self._n = 0
        self.scope_res = [[]]

    def sb(self, shape, dt=F32, name=None):
        self._n += 1
        name = f"{name or 't'}_{self._n}"
        t = self.cur.enter_context(self.nc.sbuf_tensor(name, list(shape), dt))
        r = Res(name)
        self.scope_res[-1].append(r)
        return t, r

    def sbrot(self, n, shape, dt=F32, name=None):
        return Rot([self.sb(shape, dt, name) for _ in range(n)])

    @contextlib.contextmanager
    def scope(self):
        prev = self.cur
        with contextlib.ExitStack() as st:
            self.cur = st
            self.scope_res.append([])
            yield
            self.P.barrier()
            for r in self.scope_res.pop():
                if r.dma_sem is not None:
                    self.P.free_sems.append((r.dma_sem, r.dma_cnt))
                    r.dma_sem = None
        self.cur = prev

    def dram(self, name, shape, dt, kind="Internal"):
        return self.nc.dram_tensor(name, list(shape), dt, kind=kind).ap()

    def dve(self, fn, r=(), w=()):
        return self.P.op("dve", fn, r, w)

    def act(self, fn, r=(), w=()):
        return self.P.op("act", fn, r, w)

    def pe(self, fn, r=(), w=()):
        return self.P.op("pe", fn, r, w)

    def pool(self, fn, r=(), w=()):
        return self.P.op("pool", fn, r, w)

    def load(self, tile_ap, res, dram_ap, q="sp"):
        return self.P.dma(q, tile_ap, dram_ap, w=[res], sem=res)

    def store(self, dram_ap, tile_ap, res, q="sp"):
        return self.P.dma(q, dram_ap, tile_ap, r=[res], sem=res)

    def setup_globals(self, ident_dram):
        nc = self.nc
        self.pb = []
        for i in range(8):
            t = self.gst.enter_context(nc.psum_tensor(f"pb{i}", [128, 512], F32))
            self.pb.append((t, Res(f"pb{i}")))
        self.identf, self.r_identf = self.sb([128, 128], F32, "identf")
        self.identb, self.r_identb = self.sb([128, 128], BF16, "identb")
        self.load(self.identf[:], self.r_identf, ident_dram)
        self.dve(lambda e: e.tensor_copy(out=self.identb[:], in_=self.identf[:]), r=[self.r_identf], w=[self.r_identb])
        self.nhalf, self.r_nhalf = self.sb([128, 1], F32, "nhalf")
        self.pool(lambda e: e.memset(self.nhalf[:], -0.5), w=[self.r_nhalf])
        self.onesf, self.r_onesf = self.sb([128, 128], F32, "onesf")
        self.pool(lambda e: e.memset(self.onesf[:], 1.0), w=[self.r_onesf])

    def rsqrt_cols(self, out_t, out_r, in_t, in_r, n, scale, tmp_t, tmp_r):
        self.dve(lambda e: e.tensor_scalar(out=tmp_t[:, 0:n], in0=in_t[:, 0:n], scalar1=scale, scalar2=EPS,
                                           op0=ALU.mult, op1=ALU.add), r=[in_r], w=[tmp_r])
        self.pool(lambda e: e.tensor_tensor(out=out_t[:, 0:n], in0=tmp_t[:, 0:n],
                                            in1=self.nhalf[:].broadcast_to([128, n]), op=ALU.pow),
                  r=[tmp_r, self.r_nhalf], w=[out_r])

    def diag_extract(self, col_ap, col_r, bc_ap, bc_r, n, tmp_t, tmp_r):
        self.dve(lambda e: e.tensor_tensor(out=tmp_t[:, 0:n, :], in0=bc_ap.rearrange("p (k j) -> p k j", j=128),
                                           in1=self.identf[:].unsqueeze(1).broadcast_to([128, n, 128]), op=ALU.mult),
                 r=[bc_r, self.r_identf], w=[tmp_r])
        self.dve(lambda e: e.tensor_reduce(out=col_ap, in_=tmp_t[:, 0:n, :], axis=AX.X, op=ALU.add), r=[tmp_r], w=[col_r])

    def modulation(self, c_row, cctx_row, ada_w, ada_b, gmix_row, gffn_row, need_ab2=False):
        m = {}
        m["cols"], m["r_cols"] = self.sb([128, 2, 4, 8], F32, "modcols")
        m["g"], m["r_g"] = self.sb([128, 2, 2, 1024], F32, "modg")
        if need_ab2:
            m["ab2"], m["r_ab2"] = self.sb([128, 2, 2, 1024], F32, "modab2")
        with self.scope():
            if not need_ab2:
                m["ab2"], m["r_ab2"] = self.sb([128, 2, 2, 1024], F32, "modab2")
            cbc, r_cbc = self.sb([128, 2, 1024], F32, "cbc")
            self.load(cbc[:, 0, :], r_cbc, c_row.broadcast_to([128, 1024]))
            self.load(cbc[:, 1, :], r_cbc, cctx_row.broadcast_to([128, 1024]))
            tmp, r_tmp = self.sb([128, 8, 128], F32, "dtmp")
            ccol, r_ccol = self.sb([128, 2, 8], F32, "ccol")
            for s in range(2):
                self.diag_extract(ccol[:, s, :], r_ccol, cbc[:, s, :], r_cbc, 8, tmp, r_tmp)
            scol, r_scol = self.sb([128, 2, 8], F32, "scol")
            self.act(lambda e: e.activation(out=scol[:], in_=ccol[:], func=AF.Silu), r=[r_ccol], w=[r_scol])
            srep, r_srep = self.sb([128, 2, 8, 128], F32, "srep")
            for s in range(2):
                for k in range(8):
                    self.dve(lambda e, s=s, k=k: e.tensor_copy(out=srep[:, s, k, :],
                                                               in_=scol[:, s, k:k + 1].broadcast_to([128, 128])),
                             r=[r_scol], w=[r_srep])
            adab, r_adab = self.sb([128, 6144], F32, "adab")
            self.load(adab[:], r_adab, ada_b.broadcast_to([128, 6144]))
            gn, r_gn = self.sb([128, 2, 1024], F32, "gn")
            self.load(gn[:, 0, :], r_gn, gmix_row.broadcast_to([128, 1024]))
            self.load(gn[:, 1, :], r_gn, gffn_row.broadcast_to([128, 1024]))
            modbc, r_modbc = self.sb([128, 2, 6144], F32, "modbc")
            wrot = self.sbrot(2, [128, 8, 512], F32, "adaw")
            for cg in range(12):
                wt, r_wt = wrot.next()
                self.load(wt[:], r_wt, ada_w[:, cg * 512:(cg + 1) * 512].rearrange("(k p) n -> p k n", p=128))
                for s in range(2):
                    pt, r_pt = self.pb[(cg * 2 + s) % 8]
                    for k in range(8):
                        self.pe(lambda e, s=s, k=k, wt=wt, pt=pt: e.matmul(pt[:], lhsT=srep[:, s, k, :], rhs=wt[:, k, :],
                                                                            start=(k == 0), stop=(k == 7)),
                                r=[r_srep, r_wt], w=[r_pt])
                    self.dve(lambda e, s=s, cg=cg, pt=pt: e.tensor_tensor(out=modbc[:, s, cg * 512:(cg + 1) * 512], in0=pt[:],
                                                                          in1=adab[:, cg * 512:(cg + 1) * 512], op=ALU.add),
                             r=[r_pt, r_adab], w=[r_modbc])
            abc, r_abc = self.sb([128, 1024], F32, "abc")
            for s in range(2):
                for which in range(2):
                    o = which * 3072
                    self.dve(lambda e, s=s, o=o, which=which: e.scalar_tensor_tensor(
                        out=(abc[:] if which == 0 else m["ab2"][:, s, 0, :]), in0=modbc[:, s, o + 1024:o + 2048], scalar=1.0,
                        in1=gn[:, which, :], op0=ALU.add, op1=ALU.mult), r=[r_modbc, r_gn], w=[r_abc if which == 0 else m["r_ab2"]])
                    src_ap = abc[:] if which == 0 else m["ab2"][:, s, 0, :]
                    src_r = r_abc if which == 0 else m["r_ab2"]
                    self.diag_extract(m["cols"][:, s, which * 2, :], m["r_cols"], src_ap, src_r, 8, tmp, r_tmp)
                    self.diag_extract(m["cols"][:, s, which * 2 + 1, :], m["r_cols"], modbc[:, s, o:o + 1024], r_modbc, 8, tmp, r_tmp)
                    self.dve(lambda e, s=s, o=o, which=which: e.tensor_copy(out=m["g"][:, s, which, :], in_=modbc[:, s, o + 2048:o + 3072]),
                             r=[r_modbc], w=[m["r_g"]])
                self.dve(lambda e, s=s: e.tensor_copy(out=m["ab2"][:, s, 1, :], in_=modbc[:, s, 3072:4096]), r=[r_modbc], w=[m["r_ab2"]])
        return m


def _front_bufs(kb, nh=3):
    fb = {}
    fb["h"] = kb.sbrot(nh, [128, 1024], F32, "h")
    fb["junk"] = kb.sb([128, 1024], BF16, "junk")
    fb["st"] = kb.sbrot(2, [128, 4], F32, "st")
    fb["hn"] = kb.sbrot(2, [128, 1024], BF16, "hn")
    return fb


def front(kb, fb, h_dram, mod, s, which, aT_ap, r_aT, pbank=7):
    ht, r_h = fb["h"].next()
    kb.load(ht[:], r_h, h_dram)
    stt, r_st = fb["st"].next()
    junk, r_junk = fb["junk"]
    kb.dve(lambda e: e.scalar_tensor_tensor(out=junk[:], in0=ht[:], scalar=1.0, in1=ht[:], op0=ALU.mult, op1=ALU.mult,
                                            accum_out=stt[:, 0:1]), r=[r_h], w=[r_junk, r_st])
    kb.rsqrt_cols(stt[:, 2:3], r_st, stt[:, 0:1], r_st, 1, 1.0 / 1024, stt[:, 1:2], r_st)
    hn, r_hn = fb["hn"].next()
    kb.act(lambda e: e.activation(out=hn[:], in_=ht[:], func=AF.Copy, scale=stt[:, 2:3]), r=[r_h, r_st], w=[r_hn])
    pt, r_pt = kb.pb[pbank]
    ptb = pt[:].bitcast(BF16).rearrange("p (k j) -> p k j", j=128)
    for k in range(8):
        kb.pe(lambda e, k=k: e.transpose(out=ptb[:, k, :], in_=hn[:, k * 128:(k + 1) * 128], identity=kb.identb[:]),
              r=[r_hn, kb.r_identb], w=[r_pt])
    cols = mod["cols"]
    for k in range(8):
        kb.dve(lambda e, k=k: e.tensor_scalar(out=aT_ap[:, k, :], in0=ptb[:, k, :], scalar1=cols[:, s, which * 2, k:k + 1],
                                              scalar2=cols[:, s, which * 2 + 1, k:k + 1], op0=ALU.mult, op1=ALU.add),
               r=[r_pt, mod["r_cols"]], w=[r_aT])
    return ht, r_h, stt, r_st


def load_weight(kb, dst_ap, res, w_dram, q="pool"):
    K, N = w_dram.shape
    step = 2048
    for c0 in range(0, N, step):
        c1 = min(N, c0 + step)
        kb.P.dma(q, dst_ap[:, :, c0:c1], w_dram[:, c0:c1].rearrange("(k p) n -> p k n", p=128), w=[res], sem=res)


def qk_norm_rope(kb, xv, r_xf, H, D, gain, cs, out_ap, r_out, tb):
    fl = lambda t: t[:, 0:H * D].rearrange("p (h d) -> p h d", d=D)
    t1, r_t1 = tb["t1"]
    t2, r_t2 = tb["t2"]
    t3, r_t3 = tb["t3"]
    st, r_st = tb["st"].next()
    cur, r_cur = xv, r_xf
    if gain is not None:
        g_t, r_g = gain
        kb.dve(lambda e: e.tensor_tensor(out=fl(t1), in0=xv, in1=xv, op=ALU.mult), r=[r_xf], w=[r_t1])
        kb.dve(lambda e: e.tensor_reduce(out=st[:, 0:H], in_=fl(t1), axis=AX.X, op=ALU.add), r=[r_t1], w=[r_st])
        kb.rsqrt_cols(st[:, 32:32 + H], r_st, st[:, 0:H], r_st, H, 1.0 / D, st[:, 16:16 + H], r_st)
        kb.dve(lambda e: e.tensor_tensor(out=fl(t1), in0=xv, in1=st[:, 32:32 + H].unsqueeze(2).broadcast_to([128, H, D]), op=ALU.mult),
               r=[r_xf, r_st], w=[r_t1])
        kb.pool(lambda e: e.tensor_tensor(out=fl(t1), in0=fl(t1), in1=g_t[:, 0:D].unsqueeze(1).broadcast_to([128, H, D]), op=ALU.mult),
                r=[r_t1, r_g], w=[r_t1])
        cur, r_cur = fl(t1), r_t1
    if cs is None:
        kb.dve(lambda e: e.tensor_copy(out=out_ap, in_=cur), r=[r_cur], w=[r_out])
        return
    cs_t, r_cs = cs
    q4 = D // 4
    cv = cur.rearrange("p h (a r i) -> p h a r i", a=2, r=2, i=q4)
    mv = fl(t2).rearrange("p h (a r i) -> p h a r i", a=2, r=2, i=q4)
    sv = cs_t[:, 1, 0:D].rearrange("p (a r i) -> p a r i", a=2, r=2, i=q4)
    for r_ in range(2):
        kb.pool(lambda e, r_=r_: e.tensor_tensor(out=mv[:, :, :, r_, :], in0=cv[:, :, :, 1 - r_, :],
                                                 in1=sv[:, :, r_, :].unsqueeze(1).broadcast_to([128, H, 2, q4]), op=ALU.mult),
                r=[r_cur, r_cs], w=[r_t2])
    kb.dve(lambda e: e.tensor_tensor(out=fl(t3), in0=cur, in1=cs_t[:, 0, 0:D].unsqueeze(1).broadcast_to([128, H, D]), op=ALU.mult),
           r=[r_cur, r_cs], w=[r_t3])
    kb.dve(lambda e: e.tensor_tensor(out=out_ap, in0=fl(t3), in1=fl(t2), op=ALU.add), r=[r_t3, r_t2], w=[r_out])


def norm_bufs(kb):
    return {"t1": kb.sb([128, 1024], F32, "t1"), "t2": kb.sb([128, 1024], F32, "t2"), "t3": kb.sb([128, 1024], F32, "t3"),
            "st": kb.sbrot(2, [128, 48], F32, "qst")}


def attention_core(kb, heads, loaders, groups, dv, O_s, exp_scale):
    ptrot = kb.sbrot(3, [128, 512], BF16, "pT")
    otrot = kb.sbrot(2, [128, 4, dv], BF16, "ot")
    rdrot = kb.sbrot(2, [128, 4], F32, "rden")
    cur_kv = None
    accsel = 0
    srot = 0
    for (h, kvkey) in heads:
        if kvkey != cur_kv:
            kpieces, (vt, r_vt) = loaders["kv"](kvkey)
            cur_kv = kvkey
        qpieces = loaders["q"](h)
        for (q0, nq, kts) in groups:
            nqs = nq // 128
            banks = (1, 2) if accsel == 0 else (3, 4)
            accsel ^= 1
            for ki, kt in enumerate(kts):
                ps, r_ps = kb.pb[5 + (srot % 2)]
                srot += 1
                npz = len(kpieces)
                for pi in range(npz):
                    kt_t, r_kt, nrows, base = kpieces[pi]
                    qt_t, r_qt, _, _ = qpieces[pi]
                    kb.pe(lambda e, kt_t=kt_t, qt_t=qt_t, nrows=nrows, base=base, kt=kt, q0=q0, nq=nq, ps=ps, pi=pi, npz=npz:
                          e.matmul(ps[:, 0:nq], lhsT=kt_t[base:base + nrows, kt * 128:(kt + 1) * 128],
                                   rhs=qt_t[base:base + nrows, q0:q0 + nq], start=(pi == 0), stop=(pi == npz - 1)),
                          r=[r_kt, r_qt], w=[r_ps])
                pT, r_pT = ptrot.next()
                kb.act(lambda e, pT=pT, ps=ps, nq=nq: e.activation(out=pT[:, 0:nq], in_=ps[:, 0:nq], func=AF.Exp, scale=exp_scale),
                       r=[r_ps], w=[r_pT])
                for qs in range(nqs):
                    at, r_at = kb.pb[banks[qs // 2]]
                    c0 = (qs % 2) * (dv + 1)
                    kb.pe(lambda e, at=at, c0=c0, pT=pT, qs=qs, kt=kt, ki=ki, vt=vt:
                          e.matmul(at[:, c0:c0 + dv + 1], lhsT=pT[:, qs * 128:(qs + 1) * 128], rhs=vt[:, kt, :],
                                   start=(ki == 0 and qs % 2 == 0), stop=(ki == len(kts) - 1), skip_group_check=True),
                          r=[r_pT, r_vt], w=[r_at])
            ot, r_ot = otrot.next()
            rd, r_rd = rdrot.next()
            for qs in range(nqs):
                at, r_at = kb.pb[banks[qs // 2]]
                c0 = (qs % 2) * (dv + 1)
                kb.dve(lambda e, at=at, c0=c0, rd=rd, qs=qs: e.reciprocal(out=rd[:, qs:qs + 1], in_=at[:, c0 + dv:c0 + dv + 1]),
                       r=[r_at], w=[r_rd])
                kb.act(lambda e, at=at, c0=c0, rd=rd, qs=qs, ot=ot: e.activation(out=ot[:, qs, :], in_=at[:, c0:c0 + dv], func=AF.Copy,
                                                                                  scale=rd[:, qs:qs + 1]),
                       r=[r_at, r_rd], w=[r_ot])
            kb.store(O_s[q0:q0 + nq, h * dv:(h + 1) * dv].rearrange("(qs p) d -> p qs d", p=128), ot[:, 0:nqs, :], r_ot)


def outproj_pass(kb, O_s, wo_dram, h_src, h_dst, mod, tiles, tile_set):
    with kb.scope():
        wo, r_wo = kb.sb([128, 8, 1024], BF16, "wo")
        load_weight(kb, wo[:], r_wo, wo_dram)
        orot = kb.sbrot(2, [128, 1024], BF16, "o")
        oTrot = kb.sbrot(2, [128, 8, 128], BF16, "oT")
        hrot = kb.sbrot(2, [128, 1024], F32, "h")
        trot = kb.sbrot(2, [128, 1024], F32, "tmp")
        if isinstance(tiles, int):
            tiles = list(range(tiles))
        for tt in tiles:
            oi, t = tt if isinstance(tt, tuple) else (tt, tt)
            s = tile_set(t)
            o, r_o = orot.next()
            kb.load(o[:], r_o, O_s[oi * 128:(oi + 1) * 128, :])
            ht, r_h = hrot.next()
            kb.load(ht[:], r_h, h_src[t * 128:(t + 1) * 128, :])
            pt, r_pt = kb.pb[7]
            ptb = pt[:].bitcast(BF16).rearrange("p (k j) -> p k j", j=128)
            for k in range(8):
                kb.pe(lambda e, k=k, o=o, ptb=ptb: e.transpose(out=ptb[:, k, :], in_=o[:, k * 128:(k + 1) * 128], identity=kb.identb[:]),
                      r=[r_o, kb.r_identb], w=[r_pt])
            oT, r_oT = oTrot.next()
            kb.act(lambda e, oT=oT, ptb=ptb: e.activation(out=oT[:], in_=ptb, func=AF.Copy), r=[r_pt], w=[r_oT])
            tmp, r_tmp = trot.next()
            for half in range(2):
                po, r_po = kb.pb[(t % 2) * 2 + half]
                for k in range(8):
                    kb.pe(lambda e, k=k, half=half, po=po, oT=oT: e.matmul(po[:], lhsT=oT[:, k, :], rhs=wo[:, k, half * 512:(half + 1) * 512],
                                                                          start=(k == 0), stop=(k == 7)),
                          r=[r_oT, r_wo], w=[r_po])
                kb.dve(lambda e, half=half, po=po, tmp=tmp, s=s: e.tensor_tensor(out=tmp[:, half * 512:(half + 1) * 512], in0=po[:],
                                                                                 in1=mod["g"][:, s, 0, half * 512:(half + 1) * 512], op=ALU.mult),
                       r=[r_po, mod["r_g"]], w=[r_tmp])
            kb.pool(lambda e, tmp=tmp, ht=ht: e.tensor_tensor(out=tmp[:], in0=tmp[:], in1=ht[:], op=ALU.add), r=[r_tmp, r_h], w=[r_tmp])
            kb.store(h_dst[t * 128:(t + 1) * 128, :], tmp[:], r_tmp)


def ffn_dense_pass(kb, h_src, h_dst, mod, w13_dram, w2_dram, FF, blocks, tile_set):
    nf = FF // 128
    with kb.scope():
        fb = _front_bufs(kb, nh=2)
        maxnt = max(len(b) for b in blocks)
        tT, r_tT = kb.sb([128, 8, maxnt * 128], BF16, "tT")
        hid, r_hid = kb.sb([128, nf, maxnt * 128], BF16, "hid")
        w13rot = kb.sbrot(2, [128, 8, 2, 256], BF16, "w13c")
        w2, r_w2 = kb.sb([128, nf, 1024], BF16, "w2")
        sarot = kb.sbrot(2, [128, 512], BF16, "sa")
        hrot = kb.sbrot(2, [128, 1024], F32, "h2")
        trot = kb.sbrot(2, [128, 1024], F32, "tmp")
        w2_loaded = False
        for blk in blocks:
            nt = len(blk)
            TB = nt * 128
            for j, t in enumerate(blk):
                front(kb, fb, h_src[t * 128:(t + 1) * 128, :], mod, tile_set(t), 1, tT[:, :, j * 128:(j + 1) * 128], r_tT)
            if not w2_loaded:
                for f0 in range(0, nf, 2):
                    kb.P.dma("pool", w2[:, f0:f0 + 2, :], w2_dram[f0 * 128:(f0 + 2) * 128, :].rearrange("(k p) n -> p k n", p=128),
                             w=[r_w2], sem=r_w2)
                w2_loaded = True
            tgs = [(g0, min(512, TB - g0)) for g0 in range(0, TB, 512)]
            for fc in range(0, nf, 2):
                wc, r_wc = w13rot.next()
                for ab in range(2):
                    kb.P.dma("pool", wc[:, :, ab, :], w13_dram[:, ab * FF + fc * 128: ab * FF + (fc + 2) * 128].rearrange("(k p) n -> p k n", p=128),
                             w=[r_wc], sem=r_wc)
                for sub in range(2):
                    f = fc + sub
                    for gi, (g0, gn) in enumerate(tgs):
                        pa, r_pa = kb.pb[(gi % 2) * 2]
                        pbk, r_pbk = kb.pb[(gi % 2) * 2 + 1]
                        for ab, (pp, r_pp) in enumerate(((pa, r_pa), (pbk, r_pbk))):
                            for k in range(8):
                                kb.pe(lambda e, pp=pp, wc=wc, k=k, ab=ab, sub=sub, g0=g0, gn=gn:
                                      e.matmul(pp[:, 0:gn], lhsT=wc[:, k, ab, sub * 128:(sub + 1) * 128], rhs=tT[:, k, g0:g0 + gn],
                                               start=(k == 0), stop=(k == 7)), r=[r_wc, r_tT], w=[r_pp])
                        sa, r_sa = sarot.next()
                        kb.act(lambda e, sa=sa, pa=pa, gn=gn: e.activation(out=sa[:, 0:gn], in_=pa[:, 0:gn], func=AF.Silu), r=[r_pa], w=[r_sa])
                        kb.dve(lambda e, sa=sa, pbk=pbk, f=f, g0=g0, gn=gn: e.tensor_tensor(out=hid[:, f, g0:g0 + gn], in0=pbk[:, 0:gn],
                                                                                             in1=sa[:, 0:gn], op=ALU.mult),
                               r=[r_pbk, r_sa], w=[r_hid])
            for j, t in enumerate(blk):
                s = tile_set(t)
                ht, r_h = hrot.next()
                kb.load(ht[:], r_h, h_src[t * 128:(t + 1) * 128, :])
                tmp, r_tmp = trot.next()
                for half in range(2):
                    po, r_po = kb.pb[4 + (j % 2) * 2 + half]
                    for f in range(nf):
                        kb.pe(lambda e, po=po, f=f, j=j, half=half: e.matmul(po[:], lhsT=hid[:, f, j * 128:(j + 1) * 128],
                                                                            rhs=w2[:, f, half * 512:(half + 1) * 512],
                                                                            start=(f == 0), stop=(f == nf - 1)),
                              r=[r_hid, r_w2], w=[r_po])
                    kb.dve(lambda e, half=half, po=po, tmp=tmp, s=s: e.tensor_tensor(out=tmp[:, half * 512:(half + 1) * 512], in0=po[:],
                                                                                     in1=mod["g"][:, s, 1, half * 512:(half + 1) * 512], op=ALU.mult),
                           r=[r_po, mod["r_g"]], w=[r_tmp])
                kb.pool(lambda e, tmp=tmp, ht=ht: e.tensor_tensor(out=tmp[:], in0=tmp[:], in1=ht[:], op=ALU.add), r=[r_tmp, r_h], w=[r_tmp])
                kb.store(h_dst[t * 128:(t + 1) * 128, :], tmp[:], r_tmp)


NT_Q = 34
NT_ALL = 66


def tile_set(t):
    return 1 if t < 2 else 0


def std_groups(nq_tiles, nk_tiles):
    groups = [(0, 256, [0, 1])]
    for g0 in range(2, nq_tiles, 4):
        groups.append((g0 * 128, min(4, nq_tiles - g0) * 128, list(range(nk_tiles))))
    return groups


def layer0_mixer(kb, xcat, rope, mod, wqkv_d, qn_d, kn_d, wo_d, hm_dst, nq_tiles=NT_Q, nk_tiles=NT_ALL):
    NQ, NK = nq_tiles * 128, nk_tiles * 128
    KT_s = kb.dram("a_KT", [2, 128, NK], BF16)
    V_s = kb.dram("a_V", [2, NK, 129], BF16)
    QT_s = kb.dram("a_QT", [8, 128, NQ], BF16)
    O_s = kb.dram("a_O", [NQ, 1024], BF16)
    with kb.scope():
        wqkv, r_wqkv = kb.sb([128, 8, 1536], BF16, "wqkv")
        load_weight(kb, wqkv[:], r_wqkv, wqkv_d)
        gq, r_gq = kb.sb([128, 128], F32, "gq")
        gk, r_gk = kb.sb([128, 128], F32, "gk")
        kb.load(gq[:], r_gq, qn_d.broadcast_to([128, 128]))
        kb.load(gk[:], r_gk, kn_d.broadcast_to([128, 128]))
        kb.dve(lambda e: e.tensor_scalar(out=gq[:], in0=gq[:], scalar1=float(128 ** -0.5), scalar2=None, op0=ALU.mult), r=[r_gq], w=[r_gq])
        fb = _front_bufs(kb, nh=2)
        aTrot = kb.sbrot(2, [128, 8, 128], BF16, "aT")
        csrot = kb.sbrot(2, [128, 2, 128], F32, "cs")
        vtrot = kb.sbrot(2, [128, 2, 129], BF16, "vt")
        for vt, r_vt in vtrot.items:
            kb.pool(lambda e, vt=vt: e.memset(vt[:], 1.0), w=[r_vt])
        kfrot = kb.sbrot(2, [128, 2, 128], F32, "kf")
        qfrot = kb.sbrot(2, [128, 8, 128], F32, "qf")
        tb = norm_bufs(kb)
        kbrot = kb.sbrot(2, [128, 2, 128], BF16, "kb16")
        qbrot = kb.sbrot(2, [128, 8, 128], BF16, "qb16")
        kTrot = kb.sbrot(2, [128, 2, 128], BF16, "kT")
        qTrot = kb.sbrot(2, [128, 8, 128], BF16, "qT")
        for t in range(nk_tiles):
            rows = slice(t * 128, (t + 1) * 128)
            aT, r_aT = aTrot.next()
            front(kb, fb, xcat[rows, :], mod, tile_set(t), 0, aT[:], r_aT)
            cs, r_cs = csrot.next()
            kb.load(cs[:], r_cs, rope[rows, :, :])
            pkv, r_pkv = kb.pb[0]
            for k in range(8):
                kb.pe(lambda e, k=k, aT=aT: e.matmul(pkv[:], lhsT=aT[:, k, :], rhs=wqkv[:, k, 1024:1536], start=(k == 0), stop=(k == 7)),
                      r=[r_aT, r_wqkv], w=[r_pkv])
            vt, r_vt = vtrot.next()
            kb.act(lambda e, vt=vt: e.activation(out=vt[:, :, 0:128], in_=pkv[:, 256:512].rearrange("p (h d) -> p h d", d=128), func=AF.Copy),
                   r=[r_pkv], w=[r_vt])
            kb.store(V_s[:, rows, :].rearrange("h p d -> p h d"), vt[:], r_vt)
            kf, r_kf = kfrot.next()
            kb.act(lambda e, kf=kf: e.activation(out=kf[:], in_=pkv[:, 0:256].rearrange("p (h d) -> p h d", d=128), func=AF.Copy),
                   r=[r_pkv], w=[r_kf])
            k16, r_k16 = kbrot.next()
            qk_norm_rope(kb, kf[:], r_kf, 2, 128, (gk, r_gk), (cs, r_cs), k16[:], r_k16, tb)
            pt, r_pt = kb.pb[6]
            ptb = pt[:].bitcast(BF16).rearrange("p (k j) -> p k j", j=128)
            for hh in range(2):
                kb.pe(lambda e, hh=hh, k16=k16, ptb=ptb: e.transpose(out=ptb[:, hh, :], in_=k16[:, hh, :], identity=kb.identb[:]),
                      r=[r_k16, kb.r_identb], w=[r_pt])
            kT, r_kT = kTrot.next()
            kb.act(lambda e, kT=kT, ptb=ptb: e.activation(out=kT[:], in_=ptb[:, 0:2, :], func=AF.Copy), r=[r_pt], w=[r_kT])
            kb.store(KT_s[:, :, rows].rearrange("h d t -> d h t"), kT[:], r_kT)
            if t < nq_tiles:
                qf, r_qf = qfrot.next()
                for half in range(2):
                    pq, r_pq = kb.pb[1 + half]
                    for k in range(8):
                        kb.pe(lambda e, k=k, aT=aT, pq=pq, half=half: e.matmul(pq[:], lhsT=aT[:, k, :], rhs=wqkv[:, k, half * 512:(half + 1) * 512],
                                                                              start=(k == 0), stop=(k == 7)),
                              r=[r_aT, r_wqkv], w=[r_pq])
                    kb.act(lambda e, qf=qf, pq=pq, half=half: e.activation(out=qf[:, half * 4:(half + 1) * 4, :],
                                                                           in_=pq[:].rearrange("p (h d) -> p h d", d=128), func=AF.Copy),
                           r=[r_pq], w=[r_qf])
                q16, r_q16 = qbrot.next()
                qk_norm_rope(kb, qf[:], r_qf, 8, 128, (gq, r_gq), (cs, r_cs), q16[:], r_q16, tb)
                pt2, r_pt2 = kb.pb[5]
                ptb2 = pt2[:].bitcast(BF16).rearrange("p (k j) -> p k j", j=128)
                for hh in range(8):
                    kb.pe(lambda e, hh=hh, q16=q16, ptb2=ptb2: e.transpose(out=ptb2[:, hh, :], in_=q16[:, hh, :], identity=kb.identb[:]),
                          r=[r_q16, kb.r_identb], w=[r_pt2])
                qT, r_qT = qTrot.next()
                kb.act(lambda e, qT=qT, ptb2=ptb2: e.activation(out=qT[:], in_=ptb2, func=AF.Copy), r=[r_pt2], w=[r_qT])
                kb.store(QT_s[:, :, rows].rearrange("h d t -> d h t"), qT[:], r_qT)
    with kb.scope():
        ktrot = kb.sbrot(1, [128, NK], BF16, "KT")
        vrot = kb.sbrot(1, [128, nk_tiles, 129], BF16, "V")
        qrot = kb.sbrot(2, [128, NQ], BF16, "QT")

        def load_kv(kvh):
            kt_t, r_kt = ktrot.next()
            kb.load(kt_t[:], r_kt, KT_s[kvh])
            v_t, r_v = vrot.next()
            kb.load(v_t[:], r_v, V_s[kvh].rearrange("(kt p) d -> p kt d", p=128))
            return [(kt_t, r_kt, 128, 0)], (v_t, r_v)

        def load_q(h):
            q_t, r_q = qrot.next()
            kb.load(q_t[:], r_q, QT_s[h])
            return [(q_t, r_q, 128, 0)]

        heads = [(h, h // 4) for h in range(8)]
        attention_core(kb, heads, {"kv": load_kv, "q": load_q}, std_groups(nq_tiles, nk_tiles), 128, O_s, 1.0)
    outproj_pass(kb, O_s, wo_d, xcat, hm_dst, mod, nq_tiles, tile_set)


def ffn_blocks(nq_tiles):
    blocks = [[0, 1]]
    for b0 in range(2, nq_tiles, 8):
        blocks.append(list(range(b0, min(nq_tiles, b0 + 8))))
    return blocks


def build_A():
    nc = bass.Bass("TRN2", target_bir_lowering=False)
    d = lambda name, shape: nc.dram_tensor(name, list(shape), F32, kind="ExternalInput").ap()
    xcat = d("xcat", [NT_ALL * 128, 1024])
    rope = d("rope128", [NT_ALL * 128, 2, 128])
    c = d("c", [1, 1024]); cctx = d("cctx", [1, 1024])
    ada_w = d("ada_w", [1024, 6144]); ada_b = d("ada_b", [1, 6144])
    gmix = d("gmix", [1, 1024]); gffn = d("gffn", [1, 1024])
    wqkv = d("wqkv", [1024, 1536]); qn = d("qn", [1, 128]); kn = d("kn", [1, 128]); wo = d("wo", [1024, 1024])
    w13 = d("w13", [1024, 5632]); w2 = d("w2", [2816, 1024]); ident = d("ident", [128, 128])
    hout = nc.dram_tensor("hout", [NT_Q * 128, 1024], F32, kind="ExternalOutput").ap()
    kb = KB(nc)
    with kb.gst:
        kb.setup_globals(ident)
        mod = kb.modulation(c, cctx, ada_w, ada_b, gmix, gffn)
        hm = kb.dram("hm0", [NT_Q * 128, 1024], F32)
        layer0_mixer(kb, xcat, rope, mod, wqkv, qn, kn, wo, hm)
        ffn_dense_pass(kb, hm, hout, mod, w13, w2, 2816, ffn_blocks(NT_Q), tile_set)
        kb.P.emit(nc)
    return nc


def rope_table(tokens, dim):
    tokens = np.asarray(tokens)
    q = dim // 4
    inv = (10000.0 ** (-np.arange(q, dtype=np.float32) / np.float32(q))).astype(np.float32)
    r = (np.maximum(tokens, 0) // 64).astype(np.float32)[:, None] * inv
    c = (np.maximum(tokens, 0) % 64).astype(np.float32)[:, None] * inv
    ang = np.concatenate([r, r, c, c], axis=-1).astype(np.float32)
    cos = np.cos(ang).astype(np.float32)
    sin = np.sin(ang).astype(np.float32)
    sgn = np.concatenate([-np.ones(q), np.ones(q), -np.ones(q), np.ones(q)]).astype(np.float32)
    out = np.stack([cos, sin * sgn], axis=1)
    nopos = tokens < 0
    out[nopos, 0, :] = 1.0
    out[nopos, 1, :] = 0.0
    return np.ascontiguousarray(out.astype(np.float32))


def core_tokens(hf):
    own = np.arange(hf * 4096, (hf + 1) * 4096)
    oth = np.arange((1 - hf) * 4096, (2 - hf) * 4096)
    return own, oth


_NC_CACHE = {}


def run_A(inp):
    if "A" not in _NC_CACHE:
        _NC_CACHE["A"] = build_A()
    nc = _NC_CACHE["A"]
    f = lambda a: np.ascontiguousarray(np.asarray(a, dtype=np.float32))
    maps = []
    for core in range(8):
        b, hf = core // 2, core % 2
        own, oth = core_tokens(hf)
        toks = np.concatenate([-np.ones(256, dtype=np.int64), own, oth])
        xcat = np.concatenate([inp["ctx"][b], inp["x"][b][own], inp["x"][b][oth]], axis=0)
        maps.append({
            "xcat": f(xcat), "rope128": rope_table(toks, 128),
            "c": f(inp["c"][b:b + 1]), "cctx": f(inp["c_ctx"][None, :]),
            "ada_w": f(inp["ada_w"][0]), "ada_b": f(inp["ada_b"][0:1]),
            "gmix": f(inp["norm_mix"][0:1]), "gffn": f(inp["norm_ffn"][0:1]),
            "wqkv": f(inp["a_wqkv"][0]), "qn": f(inp["a_q_norm"][0:1]), "kn": f(inp["a_k_norm"][0:1]), "wo": f(inp["a_wo"][0]),
            "w13": f(inp["ffn_w13"][0]), "w2": f(inp["ffn_w2"][0]), "ident": np.eye(128, dtype=np.float32),
        })
    res = run_bass_kernel_spmd(nc, maps, core_ids=list(range(8)))
    h1 = np.zeros((4, 8192, 1024), np.float32)
    hc1 = np.zeros((4, 256, 1024), np.float32)
    for core in range(8):
        b, hf = core // 2, core % 2
        o = res.results[core]["hout"]
        hc1[b] = o[0:256]
        h1[b, hf * 4096:(hf + 1) * 4096] = o[256:]
    return h1, hc1


def layer1_mixer(kb, hcat, rope64, mod, wdown_d, qln_d, kvln_d, wuq_d, wukv_d, wo_d, hm_dst, nq_tiles=NT_Q, nk_tiles=NT_ALL, qtiles=None):
    if qtiles is None:
        qtiles = list(range(nq_tiles))
    nq_tiles = len(qtiles)
    qpos = {t: i for i, t in enumerate(qtiles)}
    NQ, NK = nq_tiles * 128, nk_tiles * 128
    KT_s = kb.dram("b_KT", [8, 128, NK], BF16)
    KR_s = kb.dram("b_KR", [128, NK], BF16)
    V_s = kb.dram("b_V", [8, NK, 129], BF16)
    QT_s = kb.dram("b_QT", [8, 128, NQ], BF16)
    QR_s = kb.dram("b_QR", [4, 128, NQ], BF16)
    O_s = kb.dram("b_O", [NQ, 1024], BF16)
    with kb.scope():
        wdown, r_wdown = kb.sb([128, 8, 704], BF16, "wdown")
        load_weight(kb, wdown[:], r_wdown, wdown_d)
        wuq, r_wuq = kb.sb([128, 3, 1536], BF16, "wuq")
        load_weight(kb, wuq[:], r_wuq, wuq_d)
        wukv, r_wukv = kb.sb([128, 2, 2048], BF16, "wukv")
        load_weight(kb, wukv[:], r_wukv, wukv_d)
        gql, r_gql = kb.sb([128, 384], F32, "gql")
        gkv, r_gkv = kb.sb([128, 256], F32, "gkv")
        kb.load(gql[:], r_gql, qln_d.broadcast_to([128, 384]))
        kb.load(gkv[:], r_gkv, kvln_d.broadcast_to([128, 256]))
        fb = _front_bufs(kb, nh=2)
        tb = norm_bufs(kb)
        aTrot = kb.sbrot(2, [128, 8, 128], BF16, "aT")
        csrot = kb.sbrot(2, [128, 2, 64], F32, "cs")
        ckfrot = kb.sbrot(2, [128, 320], F32, "ckf")
        cknrot = kb.sbrot(2, [128, 256], BF16, "ckn")
        cknTrot = kb.sbrot(2, [128, 2, 128], BF16, "cknT")
        krrot = kb.sbrot(2, [128, 2, 64], BF16, "kr")
        krTrot = kb.sbrot(2, [128, 128], BF16, "krT")
        kTrot = kb.sbrot(2, [128, 8, 128], BF16, "kT")
        vtrot = kb.sbrot(2, [128, 8, 129], BF16, "vt")
        for vt, r_vt in vtrot.items:
            kb.pool(lambda e, vt=vt: e.memset(vt[:], 1.0), w=[r_vt])
        dqfrot = kb.sbrot(2, [128, 384], F32, "dqf")
        dqnrot = kb.sbrot(2, [128, 384], BF16, "dqn")
        dqnTrot = kb.sbrot(2, [128, 3, 128], BF16, "dqnT")
        qTrot = kb.sbrot(2, [128, 8, 128], BF16, "qT")
        qrfrot = kb.sbrot(2, [128, 8, 64], F32, "qrf")
        qr16rot = kb.sbrot(2, [128, 8, 64], BF16, "qr16")
        qrTrot = kb.sbrot(2, [128, 4, 128], BF16, "qrT")
        wukv_v = wukv[:].rearrange("p k (h x) -> p k h x", x=256)
        wuq_v = wuq[:].rearrange("p k (h x) -> p k h x", x=192)

        def bfview(bank):
            pt, r_pt = kb.pb[bank]
            return pt[:].bitcast(BF16).rearrange("p (k j) -> p k j", j=128), r_pt

        for t in range(nk_tiles):
            rows = slice(t * 128, (t + 1) * 128)
            aT, r_aT = aTrot.next()
            front(kb, fb, hcat[rows, :], mod, tile_set(t), 0, aT[:], r_aT)
            cs, r_cs = csrot.next()
            kb.load(cs[:], r_cs, rope64[rows, :, :])
            pkv, r_pkv = kb.pb[0]
            for k in range(8):
                kb.pe(lambda e, k=k, aT=aT: e.matmul(pkv[:, 0:320], lhsT=aT[:, k, :], rhs=wdown[:, k, 384:704], start=(k == 0), stop=(k == 7)),
                      r=[r_aT, r_wdown], w=[r_pkv])
            ckf, r_ckf = ckfrot.next()
            kb.act(lambda e, ckf=ckf: e.activation(out=ckf[:], in_=pkv[:, 0:320], func=AF.Copy), r=[r_pkv], w=[r_ckf])
            ckn, r_ckn = cknrot.next()
            qk_norm_rope(kb, ckf[:, 0:256].unsqueeze(1), r_ckf, 1, 256, (gkv, r_gkv), None, ckn[:].unsqueeze(1), r_ckn, tb)
            ptb, r_pt = bfview(6)
            for kc in range(2):
                kb.pe(lambda e, kc=kc, ckn=ckn, ptb=ptb: e.transpose(out=ptb[:, kc, :], in_=ckn[:, kc * 128:(kc + 1) * 128], identity=kb.identb[:]),
                      r=[r_ckn, kb.r_identb], w=[r_pt])
            cknT, r_cknT = cknTrot.next()
            kb.act(lambda e, cknT=cknT, ptb=ptb: e.activation(out=cknT[:], in_=ptb[:, 0:2, :], func=AF.Copy), r=[r_pt], w=[r_cknT])
            kr, r_kr = krrot.next()
            qk_norm_rope(kb, ckf[:, 256:320].unsqueeze(1), r_ckf, 1, 64, None, (cs, r_cs), kr[:, 0:1, :], r_kr, tb)
            kb.dve(lambda e, kr=kr: e.tensor_copy(out=kr[:, 1, :], in_=kr[:, 0, :]), r=[r_kr], w=[r_kr])
            ptb, r_pt = bfview(6)
            kb.pe(lambda e, kr=kr, ptb=ptb: e.transpose(out=ptb[:, 2, :], in_=kr[:].rearrange("p a d -> p (a d)"), identity=kb.identb[:]),
                  r=[r_kr, kb.r_identb], w=[r_pt])
            krT, r_krT = krTrot.next()
            kb.act(lambda e, krT=krT, ptb=ptb: e.activation(out=krT[:], in_=ptb[:, 2, :], func=AF.Copy), r=[r_pt], w=[r_krT])
            kb.store(KR_s[:, rows], krT[:], r_krT)
            kT, r_kT = kTrot.next()
            for hg in range(2):
                pk, r_pk = kb.pb[1 + hg]
                for hh in range(4):
                    h = hg * 4 + hh
                    for kc in range(2):
                        kb.pe(lambda e, pk=pk, hh=hh, h=h, kc=kc, cknT=cknT: e.matmul(pk[:, hh * 128:(hh + 1) * 128], lhsT=wukv_v[:, kc, h, 0:128],
                                                                                     rhs=cknT[:, kc, :], start=(kc == 0 and hh == 0), stop=(kc == 1),
                                                                                     skip_group_check=True),
                              r=[r_wukv, r_cknT], w=[r_pk])
                kb.act(lambda e, pk=pk, hg=hg, kT=kT: e.activation(out=kT[:, hg * 4:(hg + 1) * 4, :], in_=pk[:].rearrange("p (h t) -> p h t", t=128),
                                                                  func=AF.Copy), r=[r_pk], w=[r_kT])
            kb.store(KT_s[:, :, rows].rearrange("h d t -> d h t"), kT[:], r_kT)
            vt, r_vt = vtrot.next()
            for hg in range(2):
                pv, r_pv = kb.pb[3 + hg]
                for kc in range(2):
                    kb.pe(lambda e, pv=pv, hg=hg, kc=kc, cknT=cknT: e.matmul(pv[:].rearrange("p (h d) -> p h d", d=128), lhsT=cknT[:, kc, :],
                                                                           rhs=wukv_v[:, kc, hg * 4:(hg + 1) * 4, 128:256],
                                                                           start=(kc == 0), stop=(kc == 1)),
                          r=[r_wukv, r_cknT], w=[r_pv])
                kb.dve(lambda e, pv=pv, hg=hg, vt=vt: e.tensor_copy(out=vt[:, hg * 4:(hg + 1) * 4, 0:128], in_=pv[:].rearrange("p (h d) -> p h d", d=128)),
                       r=[r_pv], w=[r_vt])
            kb.store(V_s[:, rows, :].rearrange("h p d -> p h d"), vt[:], r_vt)
            if t not in qpos:
                continue
            qrows = slice(qpos[t] * 128, (qpos[t] + 1) * 128)
            pdq, r_pdq = kb.pb[0]
            for k in range(8):
                kb.pe(lambda e, k=k, aT=aT: e.matmul(pdq[:, 0:384], lhsT=aT[:, k, :], rhs=wdown[:, k, 0:384], start=(k == 0), stop=(k == 7)),
                      r=[r_aT, r_wdown], w=[r_pdq])
            dqf, r_dqf = dqfrot.next()
            kb.act(lambda e, dqf=dqf: e.activation(out=dqf[:], in_=pdq[:, 0:384], func=AF.Copy), r=[r_pdq], w=[r_dqf])
            dqn, r_dqn = dqnrot.next()
            qk_norm_rope(kb, dqf[:].unsqueeze(1), r_dqf, 1, 384, (gql, r_gql), None, dqn[:].unsqueeze(1), r_dqn, tb)
            ptb, r_pt = bfview(6)
            for kc in range(3):
                kb.pe(lambda e, kc=kc, dqn=dqn, ptb=ptb: e.transpose(out=ptb[:, 3 + kc, :], in_=dqn[:, kc * 128:(kc + 1) * 128], identity=kb.identb[:]),
                      r=[r_dqn, kb.r_identb], w=[r_pt])
            dqnT, r_dqnT = dqnTrot.next()
            kb.act(lambda e, dqnT=dqnT, ptb=ptb: e.activation(out=dqnT[:], in_=ptb[:, 3:6, :], func=AF.Copy), r=[r_pt], w=[r_dqnT])
            qT, r_qT = qTrot.next()
            for hg in range(2):
                pk, r_pk = kb.pb[1 + hg]
                for hh in range(4):
                    h = hg * 4 + hh
                    for kc in range(3):
                        kb.pe(lambda e, pk=pk, hh=hh, h=h, kc=kc, dqnT=dqnT: e.matmul(pk[:, hh * 128:(hh + 1) * 128], lhsT=wuq_v[:, kc, h, 0:128],
                                                                                     rhs=dqnT[:, kc, :], start=(kc == 0 and hh == 0), stop=(kc == 2),
                                                                                     skip_group_check=True),
                              r=[r_wuq, r_dqnT], w=[r_pk])
                kb.act(lambda e, pk=pk, hg=hg, qT=qT: e.activation(out=qT[:, hg * 4:(hg + 1) * 4, :], in_=pk[:].rearrange("p (h t) -> p h t", t=128),
                                                                  func=AF.Copy), r=[r_pk], w=[r_qT])
            kb.store(QT_s[:, :, qrows].rearrange("h d t -> d h t"), qT[:], r_qT)
            pqr, r_pqr = kb.pb[3]
            for kc in range(3):
                kb.pe(lambda e, kc=kc, dqnT=dqnT: e.matmul(pqr[:].rearrange("p (h d) -> p h d", d=64), lhsT=dqnT[:, kc, :],
                                                           rhs=wuq_v[:, kc, :, 128:192], start=(kc == 0), stop=(kc == 2)),
                      r=[r_wuq, r_dqnT], w=[r_pqr])
            qrf, r_qrf = qrfrot.next()
            kb.act(lambda e, qrf=qrf: e.activation(out=qrf[:], in_=pqr[:].rearrange("p (h d) -> p h d", d=64), func=AF.Copy), r=[r_pqr], w=[r_qrf])
            qr16, r_qr16 = qr16rot.next()
            qk_norm_rope(kb, qrf[:], r_qrf, 8, 64, None, (cs, r_cs), qr16[:], r_qr16, tb)
            ptb5, r_pt5 = bfview(5)
            for pr in range(4):
                kb.pe(lambda e, pr=pr, qr16=qr16, ptb5=ptb5: e.transpose(out=ptb5[:, pr, :], in_=qr16[:, 2 * pr:2 * pr + 2, :].rearrange("p a d -> p (a d)"),
                                                                       identity=kb.identb[:]), r=[r_qr16, kb.r_identb], w=[r_pt5])
            qrT, r_qrT = qrTrot.next()
            kb.act(lambda e, qrT=qrT, ptb5=ptb5: e.activation(out=qrT[:], in_=ptb5[:, 0:4, :], func=AF.Copy), r=[r_pt5], w=[r_qrT])
            kb.store(QR_s[:, :, qrows].rearrange("h d t -> d h t"), qrT[:], r_qrT)
    with kb.scope():
        krt, r_krt = kb.sb([128, NK], BF16, "KR")
        kb.load(krt[:], r_krt, KR_s)
        ktrot = kb.sbrot(2, [128, NK], BF16, "KT")
        vrot = kb.sbrot(2, [128, nk_tiles, 129], BF16, "V")
        qrot = kb.sbrot(2, [128, NQ], BF16, "QT")
        qrrot = kb.sbrot(2, [128, NQ], BF16, "QR")
        state = {}

        def load_kv(h):
            kt_t, r_kt = ktrot.next()
            kb.load(kt_t[:], r_kt, KT_s[h])
            v_t, r_v = vrot.next()
            kb.load(v_t[:], r_v, V_s[h].rearrange("(kt p) d -> p kt d", p=128))
            return [(kt_t, r_kt, 128, 0), (krt, r_krt, 64, (h % 2) * 64)], (v_t, r_v)

        def load_q(h):
            q_t, r_q = qrot.next()
            kb.load(q_t[:], r_q, QT_s[h])
            if h % 2 == 0:
                state["qr"] = qrrot.next()
                kb.load(state["qr"][0][:], state["qr"][1], QR_s[h // 2])
            qr_t, r_qr = state["qr"]
            return [(q_t, r_q, 128, 0), (qr_t, r_qr, 64, (h % 2) * 64)]

        heads = [(h, h) for h in range(8)]
        attention_core(kb, heads, {"kv": load_kv, "q": load_q}, std_groups(nq_tiles, nk_tiles), 128, O_s, float(192 ** -0.5))
    outproj_pass(kb, O_s, wo_d, hcat, hm_dst, mod, [(i, t) for i, t in enumerate(qtiles)], tile_set)


def moe_pass(kb, h_src, h_dst, mod, router_d, w13_d, w2_d, blocks, tile_set, n_exp=8, FF=3584, final=None, dst_row0=0, dbg=None, exp_loop=8):
    for blk in blocks:
        _moe_block(kb, blk, h_src, h_dst, mod, router_d, w13_d, w2_d, tile_set, n_exp, FF, final, dst_row0, dbg, exp_loop)


def _moe_block(kb, blk, h_src, h_dst, mod, router_d, w13_d, w2_d, tile_set, n_exp, FF, final, dst_row0, dbg, exp_loop):
    UF = FF // 2
    nfu = UF // 128
    if True:
        nt = len(blk)
        TB = nt * 128
        s = tile_set(blk[0])
        assert all(tile_set(t) == s for t in blk)
        with kb.scope():
            tT, r_tT = kb.sb([128, 8, TB], BF16, "tT")
            acc, r_acc = kb.sb([128, nt, 1024], F32, "acc")
            gates, r_gates = kb.sb([128, nt, 8], F32, "gates")
            with kb.scope():
                fb = _front_bufs(kb, nh=2)
                rcol, r_rcol = kb.sb([128, 8, 8], F32, "rcol")
                kb.load(rcol[:], r_rcol, router_d.rearrange("(k p) e -> p k e", p=128))
                Rbc, r_Rbc = kb.sb([128, 8, 1024], F32, "Rbc")
                dex, r_dex = kb.sb([128, 8, 128], F32, "dex")
                constc, r_constc = kb.sb([128, 8], F32, "constc")
                jf, r_jf = kb.sb([128, 1024], F32, "junkf")
                for e_ in range(n_exp):
                    kb.dve(lambda e, e_=e_: e.tensor_tensor(out=dex[:], in0=kb.identf[:].unsqueeze(1).broadcast_to([128, 8, 128]),
                                                            in1=rcol[:, :, e_:e_ + 1].broadcast_to([128, 8, 128]), op=ALU.mult),
                           r=[kb.r_identf, r_rcol], w=[r_dex])
                    for half in range(2):
                        pr, r_pr = kb.pb[half]
                        kb.pe(lambda e, half=half, pr=pr: e.matmul(pr[:], lhsT=kb.onesf[:], rhs=dex[:, half * 4:(half + 1) * 4, :],
                                                                   start=True, stop=True), r=[kb.r_onesf, r_dex], w=[r_pr])
                        kb.act(lambda e, half=half, pr=pr, e_=e_: e.activation(out=Rbc[:, e_, half * 512:(half + 1) * 512], in_=pr[:], func=AF.Copy),
                               r=[r_pr], w=[r_Rbc])
                    kb.dve(lambda e, e_=e_: e.scalar_tensor_tensor(out=jf[:], in0=Rbc[:, e_, :], scalar=1.0, in1=mod["ab2"][:, s, 1, :],
                                                                    op0=ALU.mult, op1=ALU.mult, accum_out=constc[:, e_:e_ + 1]),
                           r=[r_Rbc, mod["r_ab2"]], w=[r_jf, r_constc])
                    kb.dve(lambda e, e_=e_: e.tensor_tensor(out=Rbc[:, e_, :], in0=Rbc[:, e_, :], in1=mod["ab2"][:, s, 0, :], op=ALU.mult),
                           r=[r_Rbc, mod["r_ab2"]], w=[r_Rbc])
                lgrot = kb.sbrot(2, [128, 64], F32, "lg")
                for j, t in enumerate(blk):
                    ht, r_h, stt, r_st = front(kb, fb, h_src[t * 128:(t + 1) * 128, :], mod, s, 1, tT[:, :, j * 128:(j + 1) * 128], r_tT)
                    lg, r_lg = lgrot.next()
                    for e_ in range(n_exp):
                        kb.dve(lambda e, e_=e_, ht=ht, stt=stt, lg=lg: e.scalar_tensor_tensor(out=jf[:], in0=ht[:], scalar=stt[:, 2:3], in1=Rbc[:, e_, :],
                                                                                               op0=ALU.mult, op1=ALU.mult, accum_out=lg[:, e_:e_ + 1]),
                               r=[r_h, r_st, r_Rbc], w=[r_jf, r_lg])
                    L = lg[:, 0:8]
                    kb.dve(lambda e, lg=lg: e.tensor_tensor(out=lg[:, 0:8], in0=lg[:, 0:8], in1=constc[:], op=ALU.add), r=[r_lg, r_constc], w=[r_lg])
                    kb.dve(lambda e, lg=lg: e.tensor_reduce(out=lg[:, 8:9], in_=lg[:, 0:8], axis=AX.X, op=ALU.max), r=[r_lg], w=[r_lg])
                    kb.dve(lambda e, lg=lg: e.tensor_scalar(out=lg[:, 16:24], in0=lg[:, 0:8], scalar1=lg[:, 8:9], scalar2=None, op0=ALU.is_equal),
                           r=[r_lg], w=[r_lg])
                    kb.dve(lambda e, lg=lg: e.scalar_tensor_tensor(out=lg[:, 24:32], in0=lg[:, 16:24], scalar=-1e30, in1=lg[:, 0:8],
                                                                   op0=ALU.mult, op1=ALU.add), r=[r_lg], w=[r_lg])
                    kb.dve(lambda e, lg=lg: e.tensor_reduce(out=lg[:, 9:10], in_=lg[:, 24:32], axis=AX.X, op=ALU.max), r=[r_lg], w=[r_lg])
                    kb.dve(lambda e, lg=lg: e.tensor_scalar(out=lg[:, 32:40], in0=lg[:, 24:32], scalar1=lg[:, 9:10], scalar2=None, op0=ALU.is_equal),
                           r=[r_lg], w=[r_lg])
                    kb.dve(lambda e, lg=lg: e.tensor_tensor(out=lg[:, 10:11], in0=lg[:, 9:10], in1=lg[:, 8:9], op=ALU.subtract), r=[r_lg], w=[r_lg])
                    kb.act(lambda e, lg=lg: e.activation(out=lg[:, 11:12], in_=lg[:, 10:11], func=AF.Exp), r=[r_lg], w=[r_lg])
                    kb.dve(lambda e, lg=lg: e.tensor_scalar(out=lg[:, 12:13], in0=lg[:, 11:12], scalar1=1.0, scalar2=None, op0=ALU.add), r=[r_lg], w=[r_lg])
                    kb.dve(lambda e, lg=lg: e.reciprocal(out=lg[:, 13:14], in_=lg[:, 12:13]), r=[r_lg], w=[r_lg])
                    kb.dve(lambda e, lg=lg: e.tensor_tensor(out=lg[:, 14:15], in0=lg[:, 11:12], in1=lg[:, 13:14], op=ALU.mult), r=[r_lg], w=[r_lg])
                    kb.dve(lambda e, lg=lg: e.tensor_scalar(out=lg[:, 40:48], in0=lg[:, 16:24], scalar1=lg[:, 13:14], scalar2=None, op0=ALU.mult),
                           r=[r_lg], w=[r_lg])
                    kb.dve(lambda e, lg=lg, j=j: e.scalar_tensor_tensor(out=gates[:, j, :], in0=lg[:, 32:40], scalar=lg[:, 14:15], in1=lg[:, 40:48],
                                                                        op0=ALU.mult, op1=ALU.add), r=[r_lg], w=[r_gates])
                    if dbg is not None:
                        kb.store(dbg[t * 128:(t + 1) * 128, 0:64], lg[:], r_lg)
                        kb.store(dbg[t * 128:(t + 1) * 128, 64:72], gates[:, j, :], r_gates)
            with kb.scope():
                hid, r_hid = kb.sb([128, nfu, TB], BF16, "hid")
                w13rot = kb.sbrot(2, [128, 8, 2, 256], BF16, "w13c")
                w2t, r_w2t = kb.sb([128, nfu, 1024], BF16, "w2")
                sarot = kb.sbrot(2, [128, 512], BF16, "sa")
                hrot = kb.sbrot(2, [128, 1024], F32, "h2")
                fstrot = kb.sbrot(2, [128, 4], F32, "fst")
                tgs = [(g0, min(512, TB - g0)) for g0 in range(0, TB, 512)]
                first = True
                for e_ in range(exp_loop):
                    for uh in range(2):
                        base = uh * UF
                        for fc in range(0, nfu, 2):
                            wc, r_wc = w13rot.next()
                            for ab in range(2):
                                c0 = ab * FF + base + fc * 128
                                kb.P.dma("pool", wc[:, :, ab, :], w13_d[e_, :, c0:c0 + 256].rearrange("(k p) n -> p k n", p=128), w=[r_wc], sem=r_wc)
                            for sub in range(2):
                                f = fc + sub
                                for gi, (g0, gn) in enumerate(tgs):
                                    pa, r_pa = kb.pb[(gi % 2) * 2]
                                    pbk, r_pbk = kb.pb[(gi % 2) * 2 + 1]
                                    for ab, (pp, r_pp) in enumerate(((pa, r_pa), (pbk, r_pbk))):
                                        for k in range(8):
                                            kb.pe(lambda e, pp=pp, wc=wc, k=k, ab=ab, sub=sub, g0=g0, gn=gn:
                                                  e.matmul(pp[:, 0:gn], lhsT=wc[:, k, ab, sub * 128:(sub + 1) * 128], rhs=tT[:, k, g0:g0 + gn],
                                                           start=(k == 0), stop=(k == 7)), r=[r_wc, r_tT], w=[r_pp])
                                    sa, r_sa = sarot.next()
                                    kb.act(lambda e, sa=sa, pa=pa, gn=gn: e.activation(out=sa[:, 0:gn], in_=pa[:, 0:gn], func=AF.Silu), r=[r_pa], w=[r_sa])
                                    kb.dve(lambda e, sa=sa, pbk=pbk, f=f, g0=g0, gn=gn: e.tensor_tensor(out=hid[:, f, g0:g0 + gn], in0=pbk[:, 0:gn],
                                                                                                         in1=sa[:, 0:gn], op=ALU.mult),
                                           r=[r_pbk, r_sa], w=[r_hid])
                        for f0 in range(0, nfu, 2):
                            kb.P.dma("pool", w2t[:, f0:f0 + 2, :], w2_d[e_, base + f0 * 128:base + (f0 + 2) * 128, :].rearrange("(k p) n -> p k n", p=128),
                                     w=[r_w2t], sem=r_w2t)
                        for j in range(nt):
                            for half in range(2):
                                po, r_po = kb.pb[4 + (j % 2) * 2 + half]
                                for f in range(nfu):
                                    kb.pe(lambda e, po=po, f=f, j=j, half=half: e.matmul(po[:], lhsT=hid[:, f, j * 128:(j + 1) * 128],
                                                                                        rhs=w2t[:, f, half * 512:(half + 1) * 512],
                                                                                        start=(f == 0), stop=(f == nfu - 1)),
                                          r=[r_hid, r_w2t], w=[r_po])
                                if first:
                                    kb.dve(lambda e, po=po, j=j, half=half, e_=e_: e.tensor_scalar(out=acc[:, j, half * 512:(half + 1) * 512], in0=po[:],
                                                                                                   scalar1=gates[:, j, e_:e_ + 1], scalar2=None, op0=ALU.mult),
                                           r=[r_po, r_gates], w=[r_acc])
                                else:
                                    kb.dve(lambda e, po=po, j=j, half=half, e_=e_: e.scalar_tensor_tensor(out=acc[:, j, half * 512:(half + 1) * 512], in0=po[:],
                                                                                                          scalar=gates[:, j, e_:e_ + 1],
                                                                                                          in1=acc[:, j, half * 512:(half + 1) * 512],
                                                                                                          op0=ALU.mult, op1=ALU.add),
                                           r=[r_po, r_gates, r_acc], w=[r_acc])
                        first = False
                for j, t in enumerate(blk):
                    ht, r_h = hrot.next()
                    kb.load(ht[:], r_h, h_src[t * 128:(t + 1) * 128, :])
                    kb.dve(lambda e, j=j: e.tensor_tensor(out=acc[:, j, :], in0=acc[:, j, :], in1=mod["g"][:, s, 1, :], op=ALU.mult),
                           r=[r_acc, mod["r_g"]], w=[r_acc])
                    kb.pool(lambda e, j=j, ht=ht: e.tensor_tensor(out=ht[:], in0=ht[:], in1=acc[:, j, :], op=ALU.add), r=[r_acc, r_h], w=[r_h])
                    if final is not None:
                        gfin, r_gfin = final
                        fst, r_fst = fstrot.next()
                        kb.dve(lambda e, j=j, ht=ht, fst=fst: e.scalar_tensor_tensor(out=acc[:, j, :], in0=ht[:], scalar=1.0, in1=ht[:], op0=ALU.mult,
                                                                                     op1=ALU.mult, accum_out=fst[:, 0:1]), r=[r_h], w=[r_acc, r_fst])
                        kb.rsqrt_cols(fst[:, 2:3], r_fst, fst[:, 0:1], r_fst, 1, 1.0 / 1024, fst[:, 1:2], r_fst)
                        kb.dve(lambda e, ht=ht, fst=fst: e.tensor_scalar(out=ht[:], in0=ht[:], scalar1=fst[:, 2:3], scalar2=None, op0=ALU.mult),
                               r=[r_h, r_fst], w=[r_h])
                        kb.pool(lambda e, ht=ht: e.tensor_tensor(out=ht[:], in0=ht[:], in1=gfin[:], op=ALU.mult), r=[r_h, r_gfin], w=[r_h])
                    kb.store(h_dst[t * 128 - dst_row0:(t + 1) * 128 - dst_row0, :], ht[:], r_h)


def build_B(stage='all'):
    nc = bass.Bass("TRN2", target_bir_lowering=False)
    d = lambda name, shape: nc.dram_tensor(name, list(shape), F32, kind="ExternalInput").ap()
    hcat = d("hcat", [NT_ALL * 128, 1024])
    rope = d("rope64", [NT_ALL * 128, 2, 64])
    c = d("c", [1, 1024]); cctx = d("cctx", [1, 1024])
    ada_w = d("ada_w", [1024, 6144]); ada_b = d("ada_b", [1, 6144])
    gmix = d("gmix", [1, 1024]); gffn = d("gffn", [1, 1024])
    wdown = d("wdown", [1024, 704]); qln = d("qln", [1, 384]); kvln = d("kvln", [1, 256])
    wuq = d("wuq", [384, 1536]); wukv = d("wukv", [256, 2048]); wo = d("wo", [1024, 1024])
    router = d("router", [1024, 8]); w13 = d("mw13", [8, 1024, 7168]); w2 = d("mw2", [8, 3584, 1024]); ident = d("ident", [128, 128])
    hout = nc.dram_tensor("hout", [NT_Q * 128, 1024], F32, kind="ExternalOutput").ap()
    kb = KB(nc)
    with kb.gst:
        kb.setup_globals(ident)
        mod = kb.modulation(c, cctx, ada_w, ada_b, gmix, gffn, need_ab2=True)
        hm = kb.dram("hm1", [NT_Q * 128, 1024], F32)
        if stage == 'mixer':
            layer1_mixer(kb, hcat, rope, mod, wdown, qln, kvln, wuq, wukv, wo, hout)
        elif stage == 'moe':
            moe_pass(kb, hcat, hout, mod, router, w13, w2, ffn_blocks(NT_Q), tile_set)
        else:
            layer1_mixer(kb, hcat, rope, mod, wdown, qln, kvln, wuq, wukv, wo, hm)
            moe_pass(kb, hm, hout, mod, router, w13, w2, ffn_blocks(NT_Q), tile_set)
        kb.P.emit(nc)
    return nc


def run_B(inp, h1, hc1, layer=1, stage='all'):
    if "B" + stage not in _NC_CACHE:
        _NC_CACHE["B" + stage] = build_B(stage)
    nc = _NC_CACHE["B" + stage]
    f = lambda a: np.ascontiguousarray(np.asarray(a, dtype=np.float32))
    p = layer // 2
    maps = []
    for core in range(8):
        b, hf = core // 2, core % 2
        own, oth = core_tokens(hf)
        toks = np.concatenate([-np.ones(256, dtype=np.int64), own, oth])
        hcat = np.concatenate([hc1[b], h1[b][own], h1[b][oth]], axis=0)
        maps.append({
            "hcat": f(hcat), "rope64": rope_table(toks, 64),
            "c": f(inp["c"][b:b + 1]), "cctx": f(inp["c_ctx"][None, :]),
            "ada_w": f(inp["ada_w"][layer]), "ada_b": f(inp["ada_b"][layer:layer + 1]),
            "gmix": f(inp["norm_mix"][layer:layer + 1]), "gffn": f(inp["norm_ffn"][layer:layer + 1]),
            "wdown": f(inp["b_w_down"][0]), "qln": f(inp["b_q_lora_norm"][0:1]), "kvln": f(inp["b_kv_lora_norm"][0:1]),
            "wuq": f(inp["b_w_uq"][0]), "wukv": f(inp["b_w_ukv"][0]), "wo": f(inp["b_wo"][0]),
            "router": f(inp["moe_router"][p]), "mw13": f(inp["moe_w13"][p]), "mw2": f(inp["moe_w2"][p]),
            "ident": np.eye(128, dtype=np.float32),
        })
    res = run_bass_kernel_spmd(nc, maps, core_ids=list(range(8)))
    h2 = np.zeros((4, 8192, 1024), np.float32)
    hc2 = np.zeros((4, 256, 1024), np.float32)
    for core in range(8):
        b, hf = core // 2, core % 2
        o = res.results[core]["hout"]
        hc2[b] = o[0:256]
        h2[b, hf * 4096:(hf + 1) * 4096] = o[256:]
    return h2, hc2


def layer2_mixer(kb, h_src, mod, win_d, lng_d, lnb_d, wsp_d, bsp_d, wout_d, hm_dst, tiles, tile_set):
    NT = max(tiles) + 1
    O_s = kb.dram("c_O", [NT * 128, 1024], BF16)
    with kb.scope():
        win, r_win = kb.sb([128, 8, 2048], BF16, "win")
        load_weight(kb, win[:], r_win, win_d)
        lng, r_lng = kb.sb([128, 1024], F32, "lng")
        lnb, r_lnb = kb.sb([128, 1024], F32, "lnb")
        kb.load(lng[:], r_lng, lng_d.broadcast_to([128, 1024]))
        kb.load(lnb[:], r_lnb, lnb_d.broadcast_to([128, 1024]))
        bsbc, r_bsbc = kb.sb([128, 1024], F32, "bsbc")
        kb.load(bsbc[:], r_bsbc, bsp_d.broadcast_to([128, 1024]))
        bscol, r_bscol = kb.sb([128, 8], F32, "bscol")
        dtmp, r_dtmp = kb.sb([128, 8, 128], F32, "dtmp")
        kb.diag_extract(bscol[:], r_bscol, bsbc[:], r_bsbc, 8, dtmp, r_dtmp)
        wsp, r_wsp = kb.sb([128, 8, 128], BF16, "wsp")
        kb.P.dma("pool", wsp[:], wsp_d.rearrange("g p q -> p g q"), w=[r_wsp], sem=r_wsp)
        wsT, r_wsT = kb.sb([128, 8, 128], BF16, "wsT")
        pt, r_pt = kb.pb[6]
        ptb = pt[:].bitcast(BF16).rearrange("p (k j) -> p k j", j=128)
        for g in range(8):
            kb.pe(lambda e, g=g: e.transpose(out=ptb[:, g, :], in_=wsp[:, g, :], identity=kb.identb[:]), r=[r_wsp, kb.r_identb], w=[r_pt])
        kb.act(lambda e: e.activation(out=wsT[:], in_=ptb, func=AF.Copy), r=[r_pt], w=[r_wsT])
        fb = _front_bufs(kb, nh=2)
        aTrot = kb.sbrot(2, [128, 8, 128], BF16, "aT")
        urot = kb.sbrot(2, [128, 1024], BF16, "u")
        vrot = kb.sbrot(2, [128, 1024], F32, "v")
        vnrot = kb.sbrot(2, [128, 1024], BF16, "vn")
        strot = kb.sbrot(2, [128, 32], F32, "lnst")
        gtrot = kb.sbrot(2, [128, 1024], BF16, "gt")
        for t in tiles:
            rows = slice(t * 128, (t + 1) * 128)
            aT, r_aT = aTrot.next()
            front(kb, fb, h_src[rows, :], mod, tile_set(t), 0, aT[:], r_aT)
            u, r_u = urot.next()
            v, r_v = vrot.next()
            for cb in range(4):
                pu, r_pu = kb.pb[cb]
                for k in range(8):
                    kb.pe(lambda e, k=k, cb=cb, pu=pu, aT=aT: e.matmul(pu[:], lhsT=aT[:, k, :], rhs=win[:, k, cb * 512:(cb + 1) * 512],
                                                                      start=(k == 0), stop=(k == 7)), r=[r_aT, r_win], w=[r_pu])
                if cb < 2:
                    kb.act(lambda e, cb=cb, pu=pu, u=u: e.activation(out=u[:, cb * 512:(cb + 1) * 512], in_=pu[:], func=AF.Gelu), r=[r_pu], w=[r_u])
                else:
                    kb.act(lambda e, cb=cb, pu=pu, v=v: e.activation(out=v[:, (cb - 2) * 512:(cb - 1) * 512], in_=pu[:], func=AF.Gelu), r=[r_pu], w=[r_v])
            st, r_st = strot.next()
            for hb in range(2):
                kb.dve(lambda e, hb=hb, st=st, v=v: e.bn_stats(out=st[:, hb * 6:(hb + 1) * 6], in_=v[:, hb * 512:(hb + 1) * 512]), r=[r_v], w=[r_st])
            kb.dve(lambda e, st=st: e.bn_aggr(out=st[:, 12:14], in_=st[:, 0:12]), r=[r_st], w=[r_st])
            kb.rsqrt_cols(st[:, 16:17], r_st, st[:, 13:14], r_st, 1, 1.0, st[:, 15:16], r_st)
            kb.dve(lambda e, st=st, v=v: e.tensor_scalar(out=v[:], in0=v[:], scalar1=st[:, 12:13], scalar2=st[:, 16:17], op0=ALU.subtract, op1=ALU.mult),
                   r=[r_v, r_st], w=[r_v])
            kb.pool(lambda e, v=v: e.tensor_tensor(out=v[:], in0=v[:], in1=lng[:], op=ALU.mult), r=[r_v, r_lng], w=[r_v])
            vn, r_vn = vnrot.next()
            kb.dve(lambda e, v=v, vn=vn: e.tensor_tensor(out=vn[:], in0=v[:], in1=lnb[:], op=ALU.add), r=[r_v, r_lnb], w=[r_vn])
            gt, r_gt = gtrot.next()
            for hb in range(2):
                pm, r_pm = kb.pb[4 + hb]
                for gg in range(4):
                    g = hb * 4 + gg
                    kb.pe(lambda e, g=g, gg=gg, pm=pm, vn=vn: e.matmul(pm[:, gg * 128:(gg + 1) * 128], lhsT=wsT[:, g, :], rhs=vn[:, g * 128:(g + 1) * 128],
                                                                      start=(gg == 0), stop=True, skip_group_check=True), r=[r_wsT, r_vn], w=[r_pm])
                for gg in range(4):
                    g = hb * 4 + gg
                    kb.dve(lambda e, g=g, gg=gg, pm=pm, gt=gt, u=u: e.scalar_tensor_tensor(out=gt[:, g * 128:(g + 1) * 128], in0=pm[:, gg * 128:(gg + 1) * 128],
                                                                                          scalar=bscol[:, g:g + 1], in1=u[:, g * 128:(g + 1) * 128],
                                                                                          op0=ALU.add, op1=ALU.mult), r=[r_pm, r_bscol, r_u], w=[r_gt])
            kb.store(O_s[rows, :], gt[:], r_gt)
    outproj_pass(kb, O_s, wout_d, h_src, hm_dst, mod, tiles, tile_set)


def layer3_mixer(kb, h_src, rope64, mod, wqkv_d, sinks_d, wo_d, masks_d, hm_dst, ktile_src=None):
    NTK = 36
    if ktile_src is None:
        ktile_src = list(range(36))
    qtiles = list(range(2, 34))
    O_s = kb.dram("d_O", [34 * 128, 1024], BF16)
    with kb.scope():
        wqkv, r_wqkv = kb.sb([128, 8, 1280], BF16, "wqkv")
        load_weight(kb, wqkv[:], r_wqkv, wqkv_d)
        esink, r_esink = kb.sb([128, 16], F32, "esink")
        kb.load(esink[:], r_esink, sinks_d.broadcast_to([128, 16]))
        kb.act(lambda e: e.activation(out=esink[:], in_=esink[:], func=AF.Exp), r=[r_esink], w=[r_esink])
        mkf, r_mkf = kb.sb([128, 4, 128], F32, "mkf")
        kb.load(mkf[:], r_mkf, masks_d.rearrange("m k q -> k m q"))
        mk, r_mk = kb.sb([128, 4, 128], BF16, "mk")
        kb.dve(lambda e: e.tensor_copy(out=mk[:], in_=mkf[:]), r=[r_mkf], w=[r_mk])
        KT, r_KT = kb.sb([128, 2, NTK * 128], BF16, "KT")
        VV, r_VV = kb.sb([128, NTK, 2, 65], BF16, "VV")
        kb.pool(lambda e: e.memset(VV[:], 1.0), w=[r_VV])
        fb = _front_bufs(kb, nh=2)
        tb = norm_bufs(kb)
        aTrot = kb.sbrot(2, [128, 8, 128], BF16, "aT")
        csrot = kb.sbrot(2, [128, 2, 64], F32, "cs")
        kfrot = kb.sbrot(2, [128, 2, 64], F32, "kf")
        k16rot = kb.sbrot(2, [128, 2, 2, 64], BF16, "k16")
        qfrot = kb.sbrot(2, [128, 16, 64], F32, "qf")
        q16rot = kb.sbrot(2, [128, 16, 64], BF16, "q16")
        qTrot = kb.sbrot(2, [128, 8, 128], BF16, "qT")
        ptrot = kb.sbrot(3, [128, 640], BF16, "pT")
        otrot = kb.sbrot(2, [128, 16, 64], BF16, "ot")
        denrot = kb.sbrot(2, [128, 32], F32, "den")

        def bfview(bank):
            pt, r_pt = kb.pb[bank]
            return pt[:].bitcast(BF16).rearrange("p (k j) -> p k j", j=128), r_pt

        for t in range(NTK):
            rows = slice(ktile_src[t] * 128, (ktile_src[t] + 1) * 128)
            aT, r_aT = aTrot.next()
            front(kb, fb, h_src[rows, :], mod, tile_set(t), 0, aT[:], r_aT)
            cs, r_cs = csrot.next()
            kb.load(cs[:], r_cs, rope64[rows, :, :])
            pkv, r_pkv = kb.pb[0]
            for k in range(8):
                kb.pe(lambda e, k=k, aT=aT: e.matmul(pkv[:, 0:256], lhsT=aT[:, k, :], rhs=wqkv[:, k, 1024:1280], start=(k == 0), stop=(k == 7)),
                      r=[r_aT, r_wqkv], w=[r_pkv])
            kb.act(lambda e, t=t: e.activation(out=VV[:, t, :, 0:64], in_=pkv[:, 128:256].rearrange("p (h d) -> p h d", d=64), func=AF.Copy),
                   r=[r_pkv], w=[r_VV])
            kf, r_kf = kfrot.next()
            kb.act(lambda e, kf=kf: e.activation(out=kf[:], in_=pkv[:, 0:128].rearrange("p (h d) -> p h d", d=64), func=AF.Copy), r=[r_pkv], w=[r_kf])
            k16, r_k16 = k16rot.next()
            qk_norm_rope(kb, kf[:], r_kf, 2, 64, None, (cs, r_cs), k16[:, :, 0, :], r_k16, tb)
            kb.dve(lambda e, k16=k16: e.tensor_copy(out=k16[:, :, 1, :], in_=k16[:, :, 0, :]), r=[r_k16], w=[r_k16])
            ptb, r_pt = bfview(6)
            for kvh in range(2):
                kb.pe(lambda e, kvh=kvh, k16=k16, ptb=ptb: e.transpose(out=ptb[:, kvh, :], in_=k16[:, kvh, :, :].rearrange("p a d -> p (a d)"),
                                                                     identity=kb.identb[:]), r=[r_k16, kb.r_identb], w=[r_pt])
            kb.act(lambda e, t=t, ptb=ptb: e.activation(out=KT[:, :, t * 128:(t + 1) * 128], in_=ptb[:, 0:2, :], func=AF.Copy), r=[r_pt], w=[r_KT])
        for t in qtiles:
            rows = slice(t * 128, (t + 1) * 128)
            aT, r_aT = aTrot.next()
            front(kb, fb, h_src[rows, :], mod, 0, 0, aT[:], r_aT)
            cs, r_cs = csrot.next()
            kb.load(cs[:], r_cs, rope64[rows, :, :])
            qf, r_qf = qfrot.next()
            for half in range(2):
                pq, r_pq = kb.pb[half]
                for k in range(8):
                    kb.pe(lambda e, k=k, aT=aT, pq=pq, half=half: e.matmul(pq[:], lhsT=aT[:, k, :], rhs=wqkv[:, k, half * 512:(half + 1) * 512],
                                                                          start=(k == 0), stop=(k == 7)), r=[r_aT, r_wqkv], w=[r_pq])
                kb.act(lambda e, qf=qf, pq=pq, half=half: e.activation(out=qf[:, half * 8:(half + 1) * 8, :],
                                                                       in_=pq[:].rearrange("p (h d) -> p h d", d=64), func=AF.Copy), r=[r_pq], w=[r_qf])
            q16, r_q16 = q16rot.next()
            qk_norm_rope(kb, qf[:], r_qf, 16, 64, None, (cs, r_cs), q16[:], r_q16, tb)
            ptb, r_pt = bfview(6)
            for pr in range(8):
                kb.pe(lambda e, pr=pr, q16=q16, ptb=ptb: e.transpose(out=ptb[:, pr, :], in_=q16[:, 2 * pr:2 * pr + 2, :].rearrange("p a d -> p (a d)"),
                                                                   identity=kb.identb[:]), r=[r_q16, kb.r_identb], w=[r_pt])
            qT, r_qT = qTrot.next()
            kb.act(lambda e, qT=qT, ptb=ptb: e.activation(out=qT[:], in_=ptb, func=AF.Copy), r=[r_pt], w=[r_qT])
            left = t - 1 if t > 2 else 34
            right = t + 1 if t < 33 else 35
            ktl = [(0, None), (1, None), (left, 0 if t > 2 else 2), (t, None), (right, 1 if t < 33 else 3)]
            obanks = (2, 3, 4)
            for h in range(16):
                kvh, base = h // 8, (h % 2) * 64
                psA, r_psA = kb.pb[5]
                psB, r_psB = kb.pb[7]
                for ki, (kt, mi) in enumerate(ktl):
                    ps, r_ps, c0 = (psA, r_psA, ki * 128) if ki < 4 else (psB, r_psB, 0)
                    kb.pe(lambda e, ps=ps, c0=c0, kvh=kvh, base=base, kt=kt, qT=qT, h=h, ki=ki, mi=mi:
                          e.matmul(ps[:, c0:c0 + 128], lhsT=KT[base:base + 64, kvh, kt * 128:(kt + 1) * 128], rhs=qT[base:base + 64, h // 2, :],
                                   start=(ki == 0 or ki == 4), stop=(mi is None), skip_group_check=True),
                          r=[r_KT, r_qT], w=[r_ps])
                    if mi is not None:
                        kb.pe(lambda e, ps=ps, c0=c0, mi=mi: e.matmul(ps[:, c0:c0 + 128], lhsT=kb.identb[:], rhs=mk[:, mi, :], start=False, stop=True,
                                                                      skip_group_check=True), r=[kb.r_identb, r_mk], w=[r_ps])
                pT, r_pT = ptrot.next()
                kb.act(lambda e, pT=pT, psA=psA: e.activation(out=pT[:, 0:512], in_=psA[:], func=AF.Exp, scale=0.125), r=[r_psA], w=[r_pT])
                kb.act(lambda e, pT=pT, psB=psB: e.activation(out=pT[:, 512:640], in_=psB[:, 0:128], func=AF.Exp, scale=0.125), r=[r_psB], w=[r_pT])
                ob, r_ob = kb.pb[obanks[h // 7]]
                oc = (h % 7) * 65
                for ki, (kt, mi) in enumerate(ktl):
                    kb.pe(lambda e, ob=ob, oc=oc, pT=pT, ki=ki, kt=kt, kvh=kvh, h=h:
                          e.matmul(ob[:, oc:oc + 65], lhsT=pT[:, ki * 128:(ki + 1) * 128], rhs=VV[:, kt, kvh, :],
                                   start=(ki == 0 and h % 7 == 0), stop=(ki == 4), skip_group_check=True),
                          r=[r_pT, r_VV], w=[r_ob])
            ot, r_ot = otrot.next()
            den, r_den = denrot.next()
            for bi, (h0, nh_) in enumerate(((0, 7), (7, 7), (14, 2))):
                ob, r_ob = kb.pb[obanks[bi]]
                ov = ob[:, 0:nh_ * 65].rearrange("p (h d) -> p h d", d=65)
                kb.dve(lambda e, ov=ov, h0=h0, nh_=nh_, den=den: e.tensor_tensor(out=den[:, h0:h0 + nh_], in0=ov[:, :, 64], in1=esink[:, h0:h0 + nh_], op=ALU.add),
                       r=[r_ob, r_esink], w=[r_den])
                kb.dve(lambda e, h0=h0, nh_=nh_, den=den: e.reciprocal(out=den[:, 16 + h0:16 + h0 + nh_], in_=den[:, h0:h0 + nh_]), r=[r_den], w=[r_den])
                kb.dve(lambda e, ov=ov, h0=h0, nh_=nh_, den=den, ot=ot: e.tensor_tensor(out=ot[:, h0:h0 + nh_, :], in0=ov[:, :, 0:64],
                                                                                       in1=den[:, 16 + h0:16 + h0 + nh_].unsqueeze(2).broadcast_to([128, nh_, 64]),
                                                                                       op=ALU.mult), r=[r_ob, r_den], w=[r_ot])
            kb.store(O_s[rows, :], ot[:].rearrange("p h d -> p (h d)"), r_ot)
    outproj_pass(kb, O_s, wo_d, h_src, hm_dst, mod, qtiles, tile_set)


def tile_set_C(t):
    return 1 if t < 2 else 0


def build_C(debug=False):
    nc = bass.Bass("TRN2", target_bir_lowering=False)
    d = lambda name, shape: nc.dram_tensor(name, list(shape), F32, kind="ExternalInput").ap()
    hcat = d("hcat", [36 * 128, 1024])
    rope = d("rope64", [36 * 128, 2, 64])
    c = d("c", [1, 1024]); cctx = d("cctx", [1, 1024])
    ada_w2 = d("ada_w2", [1024, 6144]); ada_b2 = d("ada_b2", [1, 6144]); gmix2 = d("gmix2", [1, 1024]); gffn2 = d("gffn2", [1, 1024])
    ada_w3 = d("ada_w3", [1024, 6144]); ada_b3 = d("ada_b3", [1, 6144]); gmix3 = d("gmix3", [1, 1024]); gffn3 = d("gffn3", [1, 1024])
    win = d("c_win", [1024, 2048]); lng = d("c_lng", [1, 1024]); lnb = d("c_lnb", [1, 1024])
    wsp = d("c_wsp", [8, 128, 128]); bsp = d("c_bsp", [1, 1024]); wout = d("c_wout", [1024, 1024])
    w13 = d("w13", [1024, 5632]); w2 = d("w2", [2816, 1024])
    dwqkv = d("d_wqkv", [1024, 1280]); sinks = d("d_sinks", [1, 16]); dwo = d("d_wo", [1024, 1024]); masks = d("masks", [4, 128, 128])
    router = d("router", [1024, 8]); mw13 = d("mw13", [8, 1024, 7168]); mw2 = d("mw2", [8, 3584, 1024])
    gfin_d = d("gfin", [1, 1024]); ident = d("ident", [128, 128])
    out = nc.dram_tensor("out", [4096, 1024], F32, kind="ExternalOutput").ap()
    kb = KB(nc)
    with kb.gst:
        kb.setup_globals(ident)
        hm2 = kb.dram("hm2", [36 * 128, 1024], F32)
        h3s = kb.dram("h3s", [36 * 128, 1024], F32, kind=("ExternalOutput" if debug else "Internal"))
        hm3 = kb.dram("hm3", [34 * 128, 1024], F32, kind=("ExternalOutput" if debug else "Internal"))
        with kb.scope():
            mod2 = kb.modulation(c, cctx, ada_w2, ada_b2, gmix2, gffn2)
            layer2_mixer(kb, hcat, mod2, win, lng, lnb, wsp, bsp, wout, hm2, list(range(36)), tile_set_C)
            blocks = [[0, 1, 34, 35]] + [list(range(b0, b0 + 8)) for b0 in range(2, 34, 8)]
            ffn_dense_pass(kb, hm2, h3s, mod2, w13, w2, 2816, blocks, tile_set_C)
        with kb.scope():
            mod3 = kb.modulation(c, cctx, ada_w3, ada_b3, gmix3, gffn3, need_ab2=True)
            layer3_mixer(kb, h3s, rope, mod3, dwqkv, sinks, dwo, masks, hm3)
            gfin, r_gfin = kb.sb([128, 1024], F32, "gfin")
            kb.load(gfin[:], r_gfin, gfin_d.broadcast_to([128, 1024]))
            blocks = [list(range(b0, b0 + 8)) for b0 in range(2, 34, 8)]
            moe_pass(kb, hm3, out, mod3, router, mw13, mw2, blocks, tile_set_C, final=(gfin, r_gfin), dst_row0=256)
        kb.P.emit(nc)
    return nc


def band_masks(hf):
    k = np.arange(128)[:, None]
    q = np.arange(128)[None, :]
    NEG = np.float32(-30000.0)
    left = np.where(k >= q, 0.0, NEG).astype(np.float32)
    right = np.where(k <= q, 0.0, NEG).astype(np.float32)
    allm = np.full((128, 128), NEG, np.float32)
    return np.ascontiguousarray(np.stack([left, right, allm if hf == 0 else left, allm if hf == 1 else right]))


def run_C(inp, h2, hc2, debug=False):
    if ("C", debug) not in _NC_CACHE:
        _NC_CACHE[("C", debug)] = build_C(debug)
    nc = _NC_CACHE[("C", debug)]
    f = lambda a: np.ascontiguousarray(np.asarray(a, dtype=np.float32))
    maps = []
    for core in range(8):
        b, hf = core // 2, core % 2
        own = np.arange(hf * 4096, (hf + 1) * 4096)
        lh = np.arange(hf * 4096 - 128, hf * 4096)
        rh = np.arange((hf + 1) * 4096, (hf + 1) * 4096 + 128)
        lh_ok, rh_ok = lh[0] >= 0, rh[-1] < 8192
        hl = h2[b][lh] if lh_ok else np.zeros((128, 1024), np.float32)
        hr = h2[b][rh] if rh_ok else np.zeros((128, 1024), np.float32)
        toks = np.concatenate([-np.ones(256, dtype=np.int64), own, lh if lh_ok else -np.ones(128, dtype=np.int64),
                               rh if rh_ok else -np.ones(128, dtype=np.int64)])
        hcat = np.concatenate([hc2[b], h2[b][own], hl, hr], axis=0)
        maps.append({
            "hcat": f(hcat), "rope64": rope_table(toks, 64),
            "c": f(inp["c"][b:b + 1]), "cctx": f(inp["c_ctx"][None, :]),
            "ada_w2": f(inp["ada_w"][2]), "ada_b2": f(inp["ada_b"][2:3]), "gmix2": f(inp["norm_mix"][2:3]), "gffn2": f(inp["norm_ffn"][2:3]),
            "ada_w3": f(inp["ada_w"][3]), "ada_b3": f(inp["ada_b"][3:4]), "gmix3": f(inp["norm_mix"][3:4]), "gffn3": f(inp["norm_ffn"][3:4]),
            "c_win": f(inp["c_w_in"][0]), "c_lng": f(inp["c_ln_g"][0:1]), "c_lnb": f(inp["c_ln_b"][0:1]),
            "c_wsp": f(inp["c_w_spatial"][0]), "c_bsp": f(inp["c_b_spatial"][0].reshape(1, 1024)), "c_wout": f(inp["c_w_out"][0]),
            "w13": f(inp["ffn_w13"][1]), "w2": f(inp["ffn_w2"][1]),
            "d_wqkv": f(inp["d_wqkv"][0]), "d_sinks": f(inp["d_sinks"][0:1]), "d_wo": f(inp["d_wo"][0]), "masks": band_masks(hf),
            "router": f(inp["moe_router"][1]), "mw13": f(inp["moe_w13"][1]), "mw2": f(inp["moe_w2"][1]),
            "gfin": f(inp["final_norm"][None, :]), "ident": np.eye(128, dtype=np.float32),
        })
    res = run_bass_kernel_spmd(nc, maps, core_ids=list(range(8)))
    out = np.zeros((4, 8192, 1024), np.float32)
    for core in range(8):
        b, hf = core // 2, core % 2
        out[b, hf * 4096:(hf + 1) * 4096] = res.results[core]["out"]
    if debug:
        return out, res.results
    return out


def kernel(**inputs):
    inp = {k: np.asarray(v) for k, v in inputs.items()}
    return run_fused(inp)


def tile_set_F(t):
    return 1 if t < 2 else 0


def build_fused():
    nc = bass.Bass("TRN2", target_bir_lowering=False)
    d = lambda name, shape: nc.dram_tensor(name, list(shape), F32, kind="ExternalInput").ap()
    xcat = d("xcat", [66 * 128, 1024])
    rope128 = d("rope128", [66 * 128, 2, 128])
    rope64 = d("rope64", [66 * 128, 2, 64])
    c = d("c", [1, 1024]); cctx = d("cctx", [1, 1024])
    ada_w = d("ada_w", [4, 1024, 6144]); ada_b = d("ada_b", [4, 6144]); gmix = d("gmix", [4, 1024]); gffn = d("gffn", [4, 1024])
    a_wqkv = d("a_wqkv", [1024, 1536]); a_qn = d("a_qn", [1, 128]); a_kn = d("a_kn", [1, 128]); a_wo = d("a_wo", [1024, 1024])
    b_wdown = d("b_wdown", [1024, 704]); b_qln = d("b_qln", [1, 384]); b_kvln = d("b_kvln", [1, 256])
    b_wuq = d("b_wuq", [384, 1536]); b_wukv = d("b_wukv", [256, 2048]); b_wo = d("b_wo", [1024, 1024])
    c_win = d("c_win", [1024, 2048]); c_lng = d("c_lng", [1, 1024]); c_lnb = d("c_lnb", [1, 1024])
    c_wsp = d("c_wsp", [8, 128, 128]); c_bsp = d("c_bsp", [1, 1024]); c_wout = d("c_wout", [1024, 1024])
    d_wqkv = d("d_wqkv", [1024, 1280]); d_sinks = d("d_sinks", [1, 16]); d_wo = d("d_wo", [1024, 1024]); masks = d("masks", [4, 128, 128])
    ffn_w13 = d("ffn_w13", [2, 1024, 5632]); ffn_w2 = d("ffn_w2", [2, 2816, 1024])
    router = d("router", [2, 1024, 8]); mw13 = d("mw13", [2, 8, 1024, 7168]); mw2 = d("mw2", [2, 8, 3584, 1024])
    gfin_d = d("gfin", [1, 1024]); ident = d("ident", [128, 128])
    out = nc.dram_tensor("out", [4096, 1024], F32, kind="ExternalOutput").ap()
    kb = KB(nc)
    halo = [34, 65]
    lat36 = list(range(2, 34)) + halo
    with kb.gst:
        kb.setup_globals(ident)
        hm0 = kb.dram("hm0", [66 * 128, 1024], F32)
        h1 = kb.dram("h1", [66 * 128, 1024], F32)
        hm1 = kb.dram("hm1", [66 * 128, 1024], F32)
        h2 = kb.dram("h2", [66 * 128, 1024], F32)
        hm2 = kb.dram("hm2", [66 * 128, 1024], F32)
        h3 = kb.dram("h3", [66 * 128, 1024], F32)
        hm3 = kb.dram("hm3", [34 * 128, 1024], F32)
        mk = lambda l, ab2: kb.modulation(c, cctx, ada_w[l], ada_b[l:l + 1, :], gmix[l:l + 1, :], gffn[l:l + 1, :], need_ab2=ab2)
        with kb.scope():
            mod = mk(0, False)
            layer0_mixer(kb, xcat, rope128, mod, a_wqkv, a_qn, a_kn, a_wo, hm0, nq_tiles=66, nk_tiles=66)
            blocks = [[0, 1]] + [list(range(b0, b0 + 8)) for b0 in range(2, 66, 8)]
            ffn_dense_pass(kb, hm0, h1, mod, ffn_w13[0], ffn_w2[0], 2816, blocks, tile_set_F)
        with kb.scope():
            mod = mk(1, True)
            layer1_mixer(kb, h1, rope64, mod, b_wdown, b_qln, b_kvln, b_wuq, b_wukv, b_wo, hm1, nk_tiles=66, qtiles=[0, 1] + lat36)
            blocks = [[0, 1], lat36[0:9], lat36[9:18], lat36[18:26], lat36[26:34]]
            moe_pass(kb, hm1, h2, mod, router[0], mw13[0], mw2[0], blocks, tile_set_F)
        with kb.scope():
            mod = mk(2, False)
            layer2_mixer(kb, h2, mod, c_win, c_lng, c_lnb, c_wsp, c_bsp, c_wout, hm2, [0, 1] + lat36, tile_set_F)
            blocks = [[0, 1] + halo] + [list(range(b0, b0 + 8)) for b0 in range(2, 34, 8)]
            ffn_dense_pass(kb, hm2, h3, mod, ffn_w13[1], ffn_w2[1], 2816, blocks, tile_set_F)
        with kb.scope():
            mod = mk(3, True)
            layer3_mixer(kb, h3, rope64, mod, d_wqkv, d_sinks, d_wo, masks, hm3, ktile_src=list(range(34)) + [65, 34])
            gfin, r_gfin = kb.sb([128, 1024], F32, "gfin")
            kb.load(gfin[:], r_gfin, gfin_d.broadcast_to([128, 1024]))
            blocks = [list(range(b0, b0 + 8)) for b0 in range(2, 34, 8)]
            moe_pass(kb, hm3, out, mod, router[1], mw13[1], mw2[1], blocks, tile_set_F, final=(gfin, r_gfin), dst_row0=256)
        kb.P.emit(nc)
    return nc


def run_fused(inp):
    if "F" not in _NC_CACHE:
        _NC_CACHE["F"] = build_fused()
    nc = _NC_CACHE["F"]
    f = lambda a: np.ascontiguousarray(np.asarray(a, dtype=np.float32))
    shared = {
        "cctx": f(inp["c_ctx"][None, :]), "ada_w": f(inp["ada_w"]), "ada_b": f(inp["ada_b"]), "gmix": f(inp["norm_mix"]), "gffn": f(inp["norm_ffn"]),
        "a_wqkv": f(inp["a_wqkv"][0]), "a_qn": f(inp["a_q_norm"][0:1]), "a_kn": f(inp["a_k_norm"][0:1]), "a_wo": f(inp["a_wo"][0]),
        "b_wdown": f(inp["b_w_down"][0]), "b_qln": f(inp["b_q_lora_norm"][0:1]), "b_kvln": f(inp["b_kv_lora_norm"][0:1]),
        "b_wuq": f(inp["b_w_uq"][0]), "b_wukv": f(inp["b_w_ukv"][0]), "b_wo": f(inp["b_wo"][0]),
        "c_win": f(inp["c_w_in"][0]), "c_lng": f(inp["c_ln_g"][0:1]), "c_lnb": f(inp["c_ln_b"][0:1]),
        "c_wsp": f(inp["c_w_spatial"][0]), "c_bsp": f(inp["c_b_spatial"][0].reshape(1, 1024)), "c_wout": f(inp["c_w_out"][0]),
        "d_wqkv": f(inp["d_wqkv"][0]), "d_sinks": f(inp["d_sinks"][0:1]), "d_wo": f(inp["d_wo"][0]),
        "ffn_w13": f(inp["ffn_w13"]), "ffn_w2": f(inp["ffn_w2"]),
        "router": f(inp["moe_router"]), "mw13": f(inp["moe_w13"]), "mw2": f(inp["moe_w2"]),
        "gfin": f(inp["final_norm"][None, :]), "ident": np.eye(128, dtype=np.float32),
    }
    maps = []
    for core in range(8):
        b, hf = core // 2, core % 2
        own, oth = core_tokens(hf)
        toks = np.concatenate([-np.ones(256, dtype=np.int64), own, oth])
        xcat = np.concatenate([inp["ctx"][b], inp["x"][b][own], inp["x"][b][oth]], axis=0)
        m = dict(shared)
        m.update({"xcat": f(xcat), "rope128": rope_table(toks, 128), "rope64": rope_table(toks, 64),
                  "c": f(inp["c"][b:b + 1]), "masks": band_masks(hf)})
        maps.append(m)
    res = run_bass_kernel_spmd(nc, maps, core_ids=list(range(8)))
    out = np.zeros((4, 8192, 1024), np.float32)
    for core in range(8):
        b, hf = core // 2, core % 2
        out[b, hf * 4096:(hf + 1) * 4096] = res.results[core]["out"]
    return out


def kernel_unfused(**inputs):
    inp = {k: np.asarray(v) for k, v in inputs.items()}
    h1, hc1 = run_A(inp)
    h2, hc2 = run_B(inp, h1, hc1)
    return run_C(inp, h2, hc2)
```

```python
import contextlib
import numpy as np
import concourse.bass as bass
import concourse.mybir as mybir
from concourse.bass_utils import run_bass_kernel_spmd

F32 = mybir.dt.float32
BF16 = mybir.dt.bfloat16
AF = mybir.ActivationFunctionType
ALU = mybir.AluOpType
AX = mybir.AxisListType

COMPUTE = ("pe", "act", "dve", "pool")
QUEUES = ("sp", "act", "pool")
STREAMS = ("pe", "act", "dve", "pool", "sp")
EPS = 1e-6


class Res:
    __slots__ = ("name", "last_w", "readers", "dma_sem", "dma_cnt")

    def __init__(self, name):
        self.name = name
        self.last_w = None
        self.readers = []
        self.dma_sem = None
        self.dma_cnt = 0


class Op:
    __slots__ = ("idx", "eng", "fn", "is_dma", "waits", "inc", "count", "res", "dma_val", "deps", "dma_sem")

    def __init__(self, idx, eng, fn, is_dma):
        self.idx = idx
        self.eng = eng
        self.fn = fn
        self.is_dma = is_dma
        self.waits = {}
        self.inc = False
        self.count = None
        self.res = None
        self.dma_val = None
        self.deps = ()


class Prog:
    def __init__(self):
        self.ops = []
        self.n_dma_sems = 0
        self.last_compute = {}
        self.last_dma = {}
        self.free_sems = []

    def _add(self, eng, fn, r, w, is_dma, sem_res=None):
        op = Op(len(self.ops), eng, fn, is_dma)
        deps = set()
        for x in r:
            if x.last_w is not None:
                deps.add(x.last_w)
        soft = set()
        for x in w:
            if x.last_w is not None:
                soft.add(x.last_w)
            soft.update(x.readers)
        for dd in soft:
            dop = self.ops[dd]
            if (not is_dma) and (not dop.is_dma) and dop.eng == eng:
                continue
            deps.add(dd)
        if eng == "pe" and not is_dma:
            deps = {dd for dd in deps if self.ops[dd].is_dma or self.ops[dd].eng != "pe"}
        op.deps = deps
        for x in r:
            if not is_dma:
                x.readers = [i for i in x.readers if self.ops[i].is_dma or self.ops[i].eng != eng]
            x.readers.append(op.idx)
        for x in w:
            x.last_w = op.idx
            x.readers = []
        if is_dma:
            if sem_res.dma_sem is None:
                if self.free_sems:
                    sem_res.dma_sem, sem_res.dma_cnt = self.free_sems.pop()
                else:
                    sem_res.dma_sem = self.n_dma_sems
                    self.n_dma_sems += 1
            sem_res.dma_cnt += 16
            op.res = sem_res
            op.dma_sem = sem_res.dma_sem
            op.dma_val = sem_res.dma_cnt
            self.last_dma[sem_res.dma_sem] = op.idx
        else:
            self.last_compute[eng] = op.idx
        self.ops.append(op)
        return op

    def op(self, eng, fn, r=(), w=()):
        return self._add(eng, fn, list(r), list(w), False)

    def dma(self, q, out, in_, r=(), w=(), sem=None):
        return self._add(q, (out, in_), list(r), list(w), True, sem_res=sem)

    def barrier(self):
        deps = set(self.last_compute.values()) | set(self.last_dma.values())
        for s in STREAMS:
            op = Op(len(self.ops), s, None, False)
            op.deps = set(deps)
            self.ops.append(op)

    def plan(self):
        ops = self.ops
        for op in ops:
            for d in op.deps:
                if not ops[d].is_dma:
                    ops[d].inc = True
        cnt = {e: 0 for e in COMPUTE}
        for op in ops:
            if not op.is_dma and op.fn is not None and op.inc:
                cnt[op.eng] += 1
                op.count = cnt[op.eng]
        known = {s: {} for s in STREAMS}
        for op in ops:
            kn = known[op.eng]
            for d in op.deps:
                dop = ops[d]
                if dop.is_dma:
                    key, val = ("d", dop.dma_sem), dop.dma_val
                else:
                    key, val = ("c", dop.eng), dop.count
                if kn.get(key, 0) >= val:
                    continue
                if op.waits.get(key, 0) < val:
                    op.waits[key] = val
            for k, v in op.waits.items():
                kn[k] = max(kn.get(k, 0), v)
        self.final_counts = cnt

    def emit(self, nc):
        self.plan()
        ops = self.ops
        with contextlib.ExitStack() as st:
            csem = {e: st.enter_context(nc.semaphore("c_" + e)) for e in COMPUTE}
            dsem = [st.enter_context(nc.semaphore(f"d{i}")) for i in range(self.n_dma_sems)]
            block = st.enter_context(nc.Block())

            def semof(key):
                return csem[key[1]] if key[0] == "c" else dsem[key[1]]

            streams = {s: [] for s in STREAMS}
            for op in ops:
                streams[op.eng].append(op)
            seen = {}
            for op in ops:
                if op.is_dma:
                    seen[op.dma_sem] = max(seen.get(op.dma_sem, 0), op.dma_val)

            def run(engname, eng):
                for op in streams[engname]:
                    for key, val in op.waits.items():
                        eng.wait_ge(semof(key), val)
                    if op.fn is None:
                        continue
                    if op.is_dma:
                        out, in_ = op.fn
                        eng.dma_start(out=out, in_=in_).then_inc(dsem[op.dma_sem], 16)
                    else:
                        ins = op.fn(eng)
                        if op.inc:
                            ins.then_inc(csem[engname], 1)

            @block.tensor
            def _(e):
                run("pe", e)

            @block.scalar
            def _(e):
                run("act", e)

            @block.vector
            def _(e):
                run("dve", e)

            @block.gpsimd
            def _(e):
                run("pool", e)

            @block.sync
            def _(e):
                run("sp", e)
                for k, v in seen.items():
                    e.wait_ge(dsem[k], v)
                for en in COMPUTE:
                    if self.final_counts[en]:
                        e.wait_ge(csem[en], self.final_counts[en])


class Rot:
    def __init__(self, items):
        self.items = items
        self.i = 0

    def next(self):
        it = self.items[self.i % len(self.items)]
        self.i += 1
        return it


class KB:
    def __init__(self, nc):
        self.nc = nc
        self.P = Prog()
        self.gst = contextlib.ExitStack()
        self.cur = self.gst
        self._n = 0
        self.scope_res = [[]]

    def sb(self, shape, dt=F32, name=None):
        self._n += 1
        name = f"{name or 't'}_{self._n}"
        t = self.cur.enter_context(self.nc.sbuf_tensor(name, list(shape), dt))
        r = Res(name)
        self.scope_res[-1].append(r)
        return t, r

    def sbrot(self, n, shape, dt=F32, name=None):
        return Rot([self.sb(shape, dt, name) for _ in range(n)])

    @contextlib.contextmanager
    def scope(self):
        prev = self.cur
        with contextlib.ExitStack() as st:
            self.cur = st
            self.scope_res.append([])
            yield
            self.P.barrier()
            for r in self.scope_res.pop():
                if r.dma_sem is not None:
                    self.P.free_sems.append((r.dma_sem, r.dma_cnt))
                    r.dma_sem = None
        self.cur = prev

    def dram(self, name, shape, dt, kind="Internal"):
        return self.nc.dram_tensor(name, list(shape), dt, kind=kind).ap()

    def dve(self, fn, r=(), w=()):
        return self.P.op("dve", fn, r, w)

    def act(self, fn, r=(), w=()):
        return self.P.op("act", fn, r, w)

    def pe(self, fn, r=(), w=()):
        return self.P.op("pe", fn, r, w)

    def pool(self, fn, r=(), w=()):
        return self.P.op("pool", fn, r, w)

    def load(self, tile_ap, res, dram_ap, q="sp"):
        return self.P.dma(q, tile_ap, dram_ap, w=[res], sem=res)

    def store(self, dram_ap, tile_ap, res, q="sp"):
        return self.P.dma(q, dram_ap, tile_ap, r=[res], sem=res)

    def setup_globals(self, ident_dram):
        nc = self.nc
        self.pb = []
        for i in range(8):
            t = self.gst.enter_context(nc.psum_tensor(f"pb{i}", [128, 512], F32))
            self.pb.append((t, Res(f"pb{i}")))
        self.identf, self.r_identf = self.sb([128, 128], F32, "identf")
        self.identb, self.r_identb = self.sb([128, 128], BF16, "identb")
        self.load(self.identf[:], self.r_identf, ident_dram)
        self.dve(lambda e: e.tensor_copy(out=self.identb[:], in_=self.identf[:]), r=[self.r_identf], w=[self.r_identb])
        self.nhalf, self.r_nhalf = self.sb([128, 1], F32, "nhalf")
        self.pool(lambda e: e.memset(self.nhalf[:], -0.5), w=[self.r_nhalf])
        self.onesf, self.r_onesf = self.sb([128, 128], F32, "onesf")
        self.pool(lambda e: e.memset(self.onesf[:], 1.0), w=[self.r_onesf])

    def rsqrt_cols(self, out_t, out_r, in_t, in_r, n, scale, tmp_t, tmp_r):
        self.dve(lambda e: e.tensor_scalar(out=tmp_t[:, 0:n], in0=in_t[:, 0:n], scalar1=scale, scalar2=EPS,
                                           op0=ALU.mult, op1=ALU.add), r=[in_r], w=[tmp_r])
        self.pool(lambda e: e.tensor_tensor(out=out_t[:, 0:n], in0=tmp_t[:, 0:n],
                                            in1=self.nhalf[:].broadcast_to([128, n]), op=ALU.pow),
                  r=[tmp_r, self.r_nhalf], w=[out_r])

    def diag_extract(self, col_ap, col_r, bc_ap, bc_r, n, tmp_t, tmp_r):
        self.dve(lambda e: e.tensor_tensor(out=tmp_t[:, 0:n, :], in0=bc_ap.rearrange("p (k j) -> p k j", j=128),
                                           in1=self.identf[:].unsqueeze(1).broadcast_to([128, n, 128]), op=ALU.mult),
                 r=[bc_r, self.r_identf], w=[tmp_r])
        self.dve(lambda e: e.tensor_reduce(out=col_ap, in_=tmp_t[:, 0:n, :], axis=AX.X, op=ALU.add), r=[tmp_r], w=[col_r])

    def modulation(self, c_row, cctx_row, ada_w, ada_b, gmix_row, gffn_row, need_ab2=False):
        m = {}
        m["cols"], m["r_cols"] = self.sb([128, 2, 4, 8], F32, "modcols")
        m["g"], m["r_g"] = self.sb([128, 2, 2, 1024], F32, "modg")
        if need_ab2:
            m["ab2"], m["r_ab2"] = self.sb([128, 2, 2, 1024], F32, "modab2")
        with self.scope():
            if not need_ab2:
                m["ab2"], m["r_ab2"] = self.sb([128, 2, 2, 1024], F32, "modab2")
            cbc, r_cbc = self.sb([128, 2, 1024], F32, "cbc")
            self.load(cbc[:, 0, :], r_cbc, c_row.broadcast_to([128, 1024]))
            self.load(cbc[:, 1, :], r_cbc, cctx_row.broadcast_to([128, 1024]))
            tmp, r_tmp = self.sb([128, 8, 128], F32, "dtmp")
            ccol, r_ccol = self.sb([128, 2, 8], F32, "ccol")
            for s in range(2):
                self.diag_extract(ccol[:, s, :], r_ccol, cbc[:, s, :], r_cbc, 8, tmp, r_tmp)
            scol, r_scol = self.sb([128, 2, 8], F32, "scol")
            self.act(lambda e: e.activation(out=scol[:], in_=ccol[:], func=AF.Silu), r=[r_ccol], w=[r_scol])
            srep, r_srep = self.sb([128, 2, 8, 128], F32, "srep")
            for s in range(2):
                for k in range(8):
                    self.dve(lambda e, s=s, k=k: e.tensor_copy(out=srep[:, s, k, :],
                                                               in_=scol[:, s, k:k + 1].broadcast_to([128, 128])),
                             r=[r_scol], w=[r_srep])
            adab, r_adab = self.sb([128, 6144], F32, "adab")
            self.load(adab[:], r_adab, ada_b.broadcast_to([128, 6144]))
            gn, r_gn = self.sb([128, 2, 1024], F32, "gn")
            self.load(gn[:, 0, :], r_gn, gmix_row.broadcast_to([128, 1024]))
            self.load(gn[:, 1, :], r_gn, gffn_row.broadcast_to([128, 1024]))
            modbc, r_modbc = self.sb([128, 2, 6144], F32, "modbc")
            wrot = self.sbrot(2, [128, 8, 512], F32, "adaw")
            for cg in range(12):
                wt, r_wt = wrot.next()
                self.load(wt[:], r_wt, ada_w[:, cg * 512:(cg + 1) * 512].rearrange("(k p) n -> p k n", p=128))
                for s in range(2):
                    pt, r_pt = self.pb[(cg * 2 + s) % 8]
                    for k in range(8):
                        self.pe(lambda e, s=s, k=k, wt=wt, pt=pt: e.matmul(pt[:], lhsT=srep[:, s, k, :], rhs=wt[:, k, :],
                                                                            start=(k == 0), stop=(k == 7)),
                                r=[r_srep, r_wt], w=[r_pt])
                    self.dve(lambda e, s=s, cg=cg, pt=pt: e.tensor_tensor(out=modbc[:, s, cg * 512:(cg + 1) * 512], in0=pt[:],
                                                                          in1=adab[:, cg * 512:(cg + 1) * 512], op=ALU.add),
                             r=[r_pt, r_adab], w=[r_modbc])
            abc, r_abc = self.sb([128, 1024], F32, "abc")
            for s in range(2):
                for which in range(2):
                    o = which * 3072
                    self.dve(lambda e, s=s, o=o, which=which: e.scalar_tensor_tensor(
                        out=(abc[:] if which == 0 else m["ab2"][:, s, 0, :]), in0=modbc[:, s, o + 1024:o + 2048], scalar=1.0,
                        in1=gn[:, which, :], op0=ALU.add, op1=ALU.mult), r=[r_modbc, r_gn], w=[r_abc if which == 0 else m["r_ab2"]])
                    src_ap = abc[:] if which == 0 else m["ab2"][:, s, 0, :]
                    src_r = r_abc if which == 0 else m["r_ab2"]
                    self.diag_extract(m["cols"][:, s, which * 2, :], m["r_cols"], src_ap, src_r, 8, tmp, r_tmp)
                    self.diag_extract(m["cols"][:, s, which * 2 + 1, :], m["r_cols"], modbc[:, s, o:o + 1024], r_modbc, 8, tmp, r_tmp)
                    self.dve(lambda e, s=s, o=o, which=which: e.tensor_copy(out=m["g"][:, s, which, :], in_=modbc[:, s, o + 2048:o + 3072]),
                             r=[r_modbc], w=[m["r_g"]])
                self.dve(lambda e, s=s: e.tensor_copy(out=m["ab2"][:, s, 1, :], in_=modbc[:, s, 3072:4096]), r=[r_modbc], w=[m["r_ab2"]])
        return m


def _front_bufs(kb, nh=3):
    fb = {}
    fb["h"] = kb.sbrot(nh, [128, 1024], F32, "h")
    fb["junk"] = kb.sb([128, 1024], BF16, "junk")
    fb["st"] = kb.sbrot(2, [128, 4], F32, "st")
    fb["hn"] = kb.sbrot(2, [128, 1024], BF16, "hn")
    return fb


def front(kb, fb, h_dram, mod, s, which, aT_ap, r_aT, pbank=7):
    ht, r_h = fb["h"].next()
    kb.load(ht[:], r_h, h_dram)
    stt, r_st = fb["st"].next()
    junk, r_junk = fb["junk"]
    kb.dve(lambda e: e.scalar_tensor_tensor(out=junk[:], in0=ht[:], scalar=1.0, in1=ht[:], op0=ALU.mult, op1=ALU.mult,
                                            accum_out=stt[:, 0:1]), r=[r_h], w=[r_junk, r_st])
    kb.rsqrt_cols(stt[:, 2:3], r_st, stt[:, 0:1], r_st, 1, 1.0 / 1024, stt[:, 1:2], r_st)
    hn, r_hn = fb["hn"].next()
    kb.act(lambda e: e.activation(out=hn[:], in_=ht[:], func=AF.Copy, scale=stt[:, 2:3]), r=[r_h, r_st], w=[r_hn])
    pt, r_pt = kb.pb[pbank]
    ptb = pt[:].bitcast(BF16).rearrange("p (k j) -> p k j", j=128)
    for k in range(8):
        kb.pe(lambda e, k=k: e.transpose(out=ptb[:, k, :], in_=hn[:, k * 128:(k + 1) * 128], identity=kb.identb[:]),
              r=[r_hn, kb.r_identb], w=[r_pt])
    cols = mod["cols"]
    for k in range(8):
        kb.dve(lambda e, k=k: e.tensor_scalar(out=aT_ap[:, k, :], in0=ptb[:, k, :], scalar1=cols[:, s, which * 2, k:k + 1],
                                              scalar2=cols[:, s, which * 2 + 1, k:k + 1], op0=ALU.mult, op1=ALU.add),
               r=[r_pt, mod["r_cols"]], w=[r_aT])
    return ht, r_h, stt, r_st


def load_weight(kb, dst_ap, res, w_dram, q="pool"):
    K, N = w_dram.shape
    step = 2048
    for c0 in range(0, N, step):
        c1 = min(N, c0 + step)
        kb.P.dma(q, dst_ap[:, :, c0:c1], w_dram[:, c0:c1].rearrange("(k p) n -> p k n", p=128), w=[res], sem=res)


def qk_norm_rope(kb, xv, r_xf, H, D, gain, cs, out_ap, r_out, tb):
    fl = lambda t: t[:, 0:H * D].rearrange("p (h d) -> p h d", d=D)
    t1, r_t1 = tb["t1"]
    t2, r_t2 = tb["t2"]
    t3, r_t3 = tb["t3"]
    st, r_st = tb["st"].next()
    cur, r_cur = xv, r_xf
    if gain is not None:
        g_t, r_g = gain
        kb.dve(lambda e: e.tensor_tensor(out=fl(t1), in0=xv, in1=xv, op=ALU.mult), r=[r_xf], w=[r_t1])
        kb.dve(lambda e: e.tensor_reduce(out=st[:, 0:H], in_=fl(t1), axis=AX.X, op=ALU.add), r=[r_t1], w=[r_st])
        kb.rsqrt_cols(st[:, 32:32 + H], r_st, st[:, 0:H], r_st, H, 1.0 / D, st[:, 16:16 + H], r_st)
        kb.dve(lambda e: e.tensor_tensor(out=fl(t1), in0=xv, in1=st[:, 32:32 + H].unsqueeze(2).broadcast_to([128, H, D]), op=ALU.mult),
               r=[r_xf, r_st], w=[r_t1])
        kb.pool(lambda e: e.tensor_tensor(out=fl(t1), in0=fl(t1), in1=g_t[:, 0:D].unsqueeze(1).broadcast_to([128, H, D]), op=ALU.mult),
                r=[r_t1, r_g], w=[r_t1])
        cur, r_cur = fl(t1), r_t1
    if cs is None:
        kb.dve(lambda e: e.tensor_copy(out=out_ap, in_=cur), r=[r_cur], w=[r_out])
        return
    cs_t, r_cs = cs
    q4 = D // 4
    cv = cur.rearrange("p h (a r i) -> p h a r i", a=2, r=2, i=q4)
    mv = fl(t2).rearrange("p h (a r i) -> p h a r i", a=2, r=2, i=q4)
    sv = cs_t[:, 1, 0:D].rearrange("p (a r i) -> p a r i", a=2, r=2, i=q4)
    for r_ in range(2):
        kb.pool(lambda e, r_=r_: e.tensor_tensor(out=mv[:, :, :, r_, :], in0=cv[:, :, :, 1 - r_, :],
                                                 in1=sv[:, :, r_, :].unsqueeze(1).broadcast_to([128, H, 2, q4]), op=ALU.mult),
                r=[r_cur, r_cs], w=[r_t2])
    kb.dve(lambda e: e.tensor_tensor(out=fl(t3), in0=cur, in1=cs_t[:, 0, 0:D].unsqueeze(1).broadcast_to([128, H, D]), op=ALU.mult),
           r=[r_cur, r_cs], w=[r_t3])
    kb.dve(lambda e: e.tensor_tensor(out=out_ap, in0=fl(t3), in1=fl(t2), op=ALU.add), r=[r_t3, r_t2], w=[r_out])


def norm_bufs(kb):
    return {"t1": kb.sb([128, 1024], F32, "t1"), "t2": kb.sb([128, 1024], F32, "t2"), "t3": kb.sb([128, 1024], F32, "t3"),
            "st": kb.sbrot(2, [128, 48], F32, "qst")}


def attention_core(kb, heads, loaders, groups, dv, O_s, exp_scale):
    ptrot = kb.sbrot(3, [128, 512], BF16, "pT")
    otrot = kb.sbrot(2, [128, 4, dv], BF16, "ot")
    rdrot = kb.sbrot(2, [128, 4], F32, "rden")

    def iters():
        cur_kv = None
        accsel = 0
        kpieces = vv = None
        for (h, kvkey) in heads:
            if kvkey != cur_kv:
                kpieces, vv = loaders["kv"](kvkey)
                cur_kv = kvkey
            qpieces = loaders["q"](h)
            for (q0, nq, kts) in groups:
                banks = (1, 2) if accsel == 0 else (3, 4)
                accsel ^= 1
                for ki, kt in enumerate(kts):
                    yield dict(h=h, q0=q0, nq=nq, kt=kt, ki=ki, nkt=len(kts), kp=kpieces, qp=qpieces, vv=vv, banks=banks)

    cnt = [0]

    def emit_qk(it):
        ps, r_ps = kb.pb[5 + (cnt[0] % 2)]
        cnt[0] += 1
        it["ps"] = (ps, r_ps)
        npz = len(it["kp"])
        kt, q0, nq = it["kt"], it["q0"], it["nq"]
        for pi in range(npz):
            kt_t, r_kt, nrows, base = it["kp"][pi]
            qt_t, r_qt, _, _ = it["qp"][pi]
            kb.pe(lambda e, kt_t=kt_t, qt_t=qt_t, nrows=nrows, base=base, kt=kt, q0=q0, nq=nq, ps=ps, pi=pi, npz=npz:
                  e.matmul(ps[:, 0:nq], lhsT=kt_t[base:base + nrows, kt * 128:(kt + 1) * 128],
                           rhs=qt_t[base:base + nrows, q0:q0 + nq], start=(pi == 0), stop=(pi == npz - 1)),
                  r=[r_kt, r_qt], w=[r_ps])

    def emit_rest(it):
        ps, r_ps = it["ps"]
        vt, r_vt = it["vv"]
        kt, q0, nq, ki, nkt, banks, h = it["kt"], it["q0"], it["nq"], it["ki"], it["nkt"], it["banks"], it["h"]
        nqs = nq // 128
        pT, r_pT = ptrot.next()
        kb.act(lambda e, pT=pT, ps=ps, nq=nq: e.activation(out=pT[:, 0:nq], in_=ps[:, 0:nq], func=AF.Exp, scale=exp_scale),
               r=[r_ps], w=[r_pT])
        for qs in range(nqs):
            at, r_at = kb.pb[banks[qs // 2]]
            c0 = (qs % 2) * (dv + 1)
            kb.pe(lambda e, at=at, c0=c0, pT=pT, qs=qs, kt=kt, ki=ki, vt=vt, nkt=nkt:
                  e.matmul(at[:, c0:c0 + dv + 1], lhsT=pT[:, qs * 128:(qs + 1) * 128], rhs=vt[:, kt, :],
                           start=(ki == 0 and qs % 2 == 0), stop=(ki == nkt - 1), skip_group_check=True),
                  r=[r_pT, r_vt], w=[r_at])
        if ki != nkt - 1:
            return
        ot, r_ot = otrot.next()
        rd, r_rd = rdrot.next()
        for qs in range(nqs):
            at, r_at = kb.pb[banks[qs // 2]]
            c0 = (qs % 2) * (dv + 1)
            kb.dve(lambda e, at=at, c0=c0, rd=rd, qs=qs: e.reciprocal(out=rd[:, qs:qs + 1], in_=at[:, c0 + dv:c0 + dv + 1]),
                   r=[r_at], w=[r_rd])
            kb.act(lambda e, at=at, c0=c0, rd=rd, qs=qs, ot=ot: e.activation(out=ot[:, qs, :], in_=at[:, c0:c0 + dv], func=AF.Copy,
                                                                              scale=rd[:, qs:qs + 1]),
                   r=[r_at, r_rd], w=[r_ot])
        kb.store(O_s[q0:q0 + nq, h * dv:(h + 1) * dv].rearrange("(qs p) d -> p qs d", p=128), ot[:, 0:nqs, :], r_ot)

    prev = None
    for it in iters():
        emit_qk(it)
        if prev is not None:
            emit_rest(prev)
        prev = it
    if prev is not None:
        emit_rest(prev)


def outproj_pass(kb, O_s, wo_dram, h_src, h_dst, mod, tiles, tile_set):
    with kb.scope():
        wo, r_wo = kb.sb([128, 8, 1024], BF16, "wo")
        load_weight(kb, wo[:], r_wo, wo_dram)
        orot = kb.sbrot(2, [128, 1024], BF16, "o")
        oTrot = kb.sbrot(2, [128, 8, 128], BF16, "oT")
        hrot = kb.sbrot(2, [128, 1024], F32, "h")
        trot = kb.sbrot(2, [128, 1024], F32, "tmp")
        if isinstance(tiles, int):
            tiles = list(range(tiles))
        for tt in tiles:
            oi, t = tt if isinstance(tt, tuple) else (tt, tt)
            s = tile_set(t)
            o, r_o = orot.next()
            kb.load(o[:], r_o, O_s[oi * 128:(oi + 1) * 128, :])
            ht, r_h = hrot.next()
            kb.load(ht[:], r_h, h_src[t * 128:(t + 1) * 128, :])
            pt, r_pt = kb.pb[7]
            ptb = pt[:].bitcast(BF16).rearrange("p (k j) -> p k j", j=128)
            for k in range(8):
                kb.pe(lambda e, k=k, o=o, ptb=ptb: e.transpose(out=ptb[:, k, :], in_=o[:, k * 128:(k + 1) * 128], identity=kb.identb[:]),
                      r=[r_o, kb.r_identb], w=[r_pt])
            oT, r_oT = oTrot.next()
            kb.act(lambda e, oT=oT, ptb=ptb: e.activation(out=oT[:], in_=ptb, func=AF.Copy), r=[r_pt], w=[r_oT])
            tmp, r_tmp = trot.next()
            for half in range(2):
                po, r_po = kb.pb[(t % 2) * 2 + half]
                for k in range(8):
                    kb.pe(lambda e, k=k, half=half, po=po, oT=oT: e.matmul(po[:], lhsT=oT[:, k, :], rhs=wo[:, k, half * 512:(half + 1) * 512],
                                                                          start=(k == 0), stop=(k == 7)),
                          r=[r_oT, r_wo], w=[r_po])
                kb.dve(lambda e, half=half, po=po, tmp=tmp, s=s: e.tensor_tensor(out=tmp[:, half * 512:(half + 1) * 512], in0=po[:],
                                                                                 in1=mod["g"][:, s, 0, half * 512:(half + 1) * 512], op=ALU.mult),
                       r=[r_po, mod["r_g"]], w=[r_tmp])
            kb.pool(lambda e, tmp=tmp, ht=ht: e.tensor_tensor(out=tmp[:], in0=tmp[:], in1=ht[:], op=ALU.add), r=[r_tmp, r_h], w=[r_tmp])
            kb.store(h_dst[t * 128:(t + 1) * 128, :], tmp[:], r_tmp)


def ffn_dense_pass(kb, h_src, h_dst, mod, w13_dram, w2_dram, FF, blocks, tile_set):
    nf = FF // 128
    with kb.scope():
        fb = _front_bufs(kb, nh=2)
        maxnt = max(len(b) for b in blocks)
        tT, r_tT = kb.sb([128, 8, maxnt * 128], BF16, "tT")
        hid, r_hid = kb.sb([128, nf, maxnt * 128], BF16, "hid")
        w13rot = kb.sbrot(2, [128, 8, 2, 256], BF16, "w13c")
        w2, r_w2 = kb.sb([128, nf, 1024], BF16, "w2")
        sarot = kb.sbrot(2, [128, 512], BF16, "sa")
        hrot = kb.sbrot(2, [128, 1024], F32, "h2")
        trot = kb.sbrot(2, [128, 1024], F32, "tmp")
        w2_loaded = False
        for blk in blocks:
            nt = len(blk)
            TB = nt * 128
            for j, t in enumerate(blk):
                front(kb, fb, h_src[t * 128:(t + 1) * 128, :], mod, tile_set(t), 1, tT[:, :, j * 128:(j + 1) * 128], r_tT)
            if not w2_loaded:
                for f0 in range(0, nf, 2):
                    kb.P.dma("pool", w2[:, f0:f0 + 2, :], w2_dram[f0 * 128:(f0 + 2) * 128, :].rearrange("(k p) n -> p k n", p=128),
                             w=[r_w2], sem=r_w2)
                w2_loaded = True
            tgs = [(g0, min(512, TB - g0)) for g0 in range(0, TB, 512)]
            for fc in range(0, nf, 2):
                wc, r_wc = w13rot.next()
                for ab in range(2):
                    kb.P.dma("pool", wc[:, :, ab, :], w13_dram[:, ab * FF + fc * 128: ab * FF + (fc + 2) * 128].rearrange("(k p) n -> p k n", p=128),
                             w=[r_wc], sem=r_wc)
                for sub in range(2):
                    f = fc + sub
                    for gi, (g0, gn) in enumerate(tgs):
                        pa, r_pa = kb.pb[(gi % 2) * 2]
                        pbk, r_pbk = kb.pb[(gi % 2) * 2 + 1]
                        for ab, (pp, r_pp) in enumerate(((pa, r_pa), (pbk, r_pbk))):
                            for k in range(8):
                                kb.pe(lambda e, pp=pp, wc=wc, k=k, ab=ab, sub=sub, g0=g0, gn=gn:
                                      e.matmul(pp[:, 0:gn], lhsT=wc[:, k, ab, sub * 128:(sub + 1) * 128], rhs=tT[:, k, g0:g0 + gn],
                                               start=(k == 0), stop=(k == 7)), r=[r_wc, r_tT], w=[r_pp])
                        sa, r_sa = sarot.next()
                        kb.act(lambda e, sa=sa, pa=pa, gn=gn: e.activation(out=sa[:, 0:gn], in_=pa[:, 0:gn], func=AF.Silu), r=[r_pa], w=[r_sa])
                        kb.dve(lambda e, sa=sa, pbk=pbk, f=f, g0=g0, gn=gn: e.tensor_tensor(out=hid[:, f, g0:g0 + gn], in0=pbk[:, 0:gn],
                                                                                             in1=sa[:, 0:gn], op=ALU.mult),
                               r=[r_pbk, r_sa], w=[r_hid])
            for j, t in enumerate(blk):
                s = tile_set(t)
                ht, r_h = hrot.next()
                kb.load(ht[:], r_h, h_src[t * 128:(t + 1) * 128, :])
                tmp, r_tmp = trot.next()
                for half in range(2):
                    po, r_po = kb.pb[4 + (j % 2) * 2 + half]
                    for f in range(nf):
                        kb.pe(lambda e, po=po, f=f, j=j, half=half: e.matmul(po[:], lhsT=hid[:, f, j * 128:(j + 1) * 128],
                                                                            rhs=w2[:, f, half * 512:(half + 1) * 512],
                                                                            start=(f == 0), stop=(f == nf - 1)),
                              r=[r_hid, r_w2], w=[r_po])
                    kb.dve(lambda e, half=half, po=po, tmp=tmp, s=s: e.tensor_tensor(out=tmp[:, half * 512:(half + 1) * 512], in0=po[:],
                                                                                     in1=mod["g"][:, s, 1, half * 512:(half + 1) * 512], op=ALU.mult),
                           r=[r_po, mod["r_g"]], w=[r_tmp])
                kb.pool(lambda e, tmp=tmp, ht=ht: e.tensor_tensor(out=tmp[:], in0=tmp[:], in1=ht[:], op=ALU.add), r=[r_tmp, r_h], w=[r_tmp])
                kb.store(h_dst[t * 128:(t + 1) * 128, :], tmp[:], r_tmp)


NT_Q = 34
NT_ALL = 66


def tile_set(t):
    return 1 if t < 2 else 0


def std_groups(nq_tiles, nk_tiles):
    groups = [(0, 256, [0, 1])]
    for g0 in range(2, nq_tiles, 4):
        groups.append((g0 * 128, min(4, nq_tiles - g0) * 128, list(range(nk_tiles))))
    return groups


def layer0_mixer(kb, xcat, rope, mod, wqkv_d, qn_d, kn_d, wo_d, hm_dst, nq_tiles=NT_Q, nk_tiles=NT_ALL):
    NQ, NK = nq_tiles * 128, nk_tiles * 128
    KT_s = kb.dram("a_KT", [2, 128, NK], BF16)
    V_s = kb.dram("a_V", [2, NK, 129], BF16)
    QT_s = kb.dram("a_QT", [8, 128, NQ], BF16)
    O_s = kb.dram("a_O", [NQ, 1024], BF16)
    with kb.scope():
        wqkv, r_wqkv = kb.sb([128, 8, 1536], BF16, "wqkv")
        load_weight(kb, wqkv[:], r_wqkv, wqkv_d)
        gq, r_gq = kb.sb([128, 128], F32, "gq")
        gk, r_gk = kb.sb([128, 128], F32, "gk")
        kb.load(gq[:], r_gq, qn_d.broadcast_to([128, 128]))
        kb.load(gk[:], r_gk, kn_d.broadcast_to([128, 128]))
        kb.dve(lambda e: e.tensor_scalar(out=gq[:], in0=gq[:], scalar1=float(128 ** -0.5), scalar2=None, op0=ALU.mult), r=[r_gq], w=[r_gq])
        fb = _front_bufs(kb, nh=2)
        aTrot = kb.sbrot(2, [128, 8, 128], BF16, "aT")
        csrot = kb.sbrot(2, [128, 2, 128], F32, "cs")
        vtrot = kb.sbrot(2, [128, 2, 129], BF16, "vt")
        for vt, r_vt in vtrot.items:
            kb.pool(lambda e, vt=vt: e.memset(vt[:], 1.0), w=[r_vt])
        kfrot = kb.sbrot(2, [128, 2, 128], F32, "kf")
        qfrot = kb.sbrot(2, [128, 8, 128], F32, "qf")
        tb = norm_bufs(kb)
        kbrot = kb.sbrot(2, [128, 2, 128], BF16, "kb16")
        qbrot = kb.sbrot(2, [128, 8, 128], BF16, "qb16")
        kTrot = kb.sbrot(2, [128, 2, 128], BF16, "kT")
        qTrot = kb.sbrot(2, [128, 8, 128], BF16, "qT")
        for t in range(nk_tiles):
            rows = slice(t * 128, (t + 1) * 128)
            aT, r_aT = aTrot.next()
            front(kb, fb, xcat[rows, :], mod, tile_set(t), 0, aT[:], r_aT)
            cs, r_cs = csrot.next()
            kb.load(cs[:], r_cs, rope[rows, :, :])
            pkv, r_pkv = kb.pb[0]
            for k in range(8):
                kb.pe(lambda e, k=k, aT=aT: e.matmul(pkv[:], lhsT=aT[:, k, :], rhs=wqkv[:, k, 1024:1536], start=(k == 0), stop=(k == 7)),
                      r=[r_aT, r_wqkv], w=[r_pkv])
            vt, r_vt = vtrot.next()
            kb.act(lambda e, vt=vt: e.activation(out=vt[:, :, 0:128], in_=pkv[:, 256:512].rearrange("p (h d) -> p h d", d=128), func=AF.Copy),
                   r=[r_pkv], w=[r_vt])
            kb.store(V_s[:, rows, :].rearrange("h p d -> p h d"), vt[:], r_vt)
            kf, r_kf = kfrot.next()
            kb.act(lambda e, kf=kf: e.activation(out=kf[:], in_=pkv[:, 0:256].rearrange("p (h d) -> p h d", d=128), func=AF.Copy),
                   r=[r_pkv], w=[r_kf])
            k16, r_k16 = kbrot.next()
            qk_norm_rope(kb, kf[:], r_kf, 2, 128, (gk, r_gk), (cs, r_cs), k16[:], r_k16, tb)
            pt, r_pt = kb.pb[6]
            ptb = pt[:].bitcast(BF16).rearrange("p (k j) -> p k j", j=128)
            for hh in range(2):
                kb.pe(lambda e, hh=hh, k16=k16, ptb=ptb: e.transpose(out=ptb[:, hh, :], in_=k16[:, hh, :], identity=kb.identb[:]),
                      r=[r_k16, kb.r_identb], w=[r_pt])
            kT, r_kT = kTrot.next()
            kb.act(lambda e, kT=kT, ptb=ptb: e.activation(out=kT[:], in_=ptb[:, 0:2, :], func=AF.Copy), r=[r_pt], w=[r_kT])
            kb.store(KT_s[:, :, rows].rearrange("h d t -> d h t"), kT[:], r_kT)
            if t < nq_tiles:
                qf, r_qf = qfrot.next()
                for half in range(2):
                    pq, r_pq = kb.pb[1 + half]
                    for k in range(8):
                        kb.pe(lambda e, k=k, aT=aT, pq=pq, half=half: e.matmul(pq[:], lhsT=aT[:, k, :], rhs=wqkv[:, k, half * 512:(half + 1) * 512],
                                                                              start=(k == 0), stop=(k == 7)),
                              r=[r_aT, r_wqkv], w=[r_pq])
                    kb.act(lambda e, qf=qf, pq=pq, half=half: e.activation(out=qf[:, half * 4:(half + 1) * 4, :],
                                                                           in_=pq[:].rearrange("p (h d) -> p h d", d=128), func=AF.Copy),
                           r=[r_pq], w=[r_qf])
                q16, r_q16 = qbrot.next()
                qk_norm_rope(kb, qf[:], r_qf, 8, 128, (gq, r_gq), (cs, r_cs), q16[:], r_q16, tb)
                pt2, r_pt2 = kb.pb[5]
                ptb2 = pt2[:].bitcast(BF16).rearrange("p (k j) -> p k j", j=128)
                for hh in range(8):
                    kb.pe(lambda e, hh=hh, q16=q16, ptb2=ptb2: e.transpose(out=ptb2[:, hh, :], in_=q16[:, hh, :], identity=kb.identb[:]),
                          r=[r_q16, kb.r_identb], w=[r_pt2])
                qT, r_qT = qTrot.next()
                kb.act(lambda e, qT=qT, ptb2=ptb2: e.activation(out=qT[:], in_=ptb2, func=AF.Copy), r=[r_pt2], w=[r_qT])
                kb.store(QT_s[:, :, rows].rearrange("h d t -> d h t"), qT[:], r_qT)
    with kb.scope():
        ktrot = kb.sbrot(2, [128, NK], BF16, "KT")
        vrot = kb.sbrot(2, [128, nk_tiles, 129], BF16, "V")
        qrot = kb.sbrot(2, [128, NQ], BF16, "QT")

        def load_kv(kvh):
            kt_t, r_kt = ktrot.next()
            kb.load(kt_t[:], r_kt, KT_s[kvh])
            v_t, r_v = vrot.next()
            kb.load(v_t[:], r_v, V_s[kvh].rearrange("(kt p) d -> p kt d", p=128))
            return [(kt_t, r_kt, 128, 0)], (v_t, r_v)

        def load_q(h):
            q_t, r_q = qrot.next()
            kb.load(q_t[:], r_q, QT_s[h])
            return [(q_t, r_q, 128, 0)]

        heads = [(h, h // 4) for h in range(8)]
        attention_core(kb, heads, {"kv": load_kv, "q": load_q}, std_groups(nq_tiles, nk_tiles), 128, O_s, 1.0)
    outproj_pass(kb, O_s, wo_d, xcat, hm_dst, mod, nq_tiles, tile_set)


def ffn_blocks(nq_tiles):
    blocks = [[0, 1]]
    for b0 in range(2, nq_tiles, 8):
        blocks.append(list(range(b0, min(nq_tiles, b0 + 8))))
    return blocks


def build_A():
    nc = bass.Bass("TRN2", target_bir_lowering=False)
    d = lambda name, shape: nc.dram_tensor(name, list(shape), F32, kind="ExternalInput").ap()
    xcat = d("xcat", [NT_ALL * 128, 1024])
    rope = d("rope128", [NT_ALL * 128, 2, 128])
    c = d("c", [1, 1024]); cctx = d("cctx", [1, 1024])
    ada_w = d("ada_w", [1024, 6144]); ada_b = d("ada_b", [1, 6144])
    gmix = d("gmix", [1, 1024]); gffn = d("gffn", [1, 1024])
    wqkv = d("wqkv", [1024, 1536]); qn = d("qn", [1, 128]); kn = d("kn", [1, 128]); wo = d("wo", [1024, 1024])
    w13 = d("w13", [1024, 5632]); w2 = d("w2", [2816, 1024]); ident = d("ident", [128, 128])
    hout = nc.dram_tensor("hout", [NT_Q * 128, 1024], F32, kind="ExternalOutput").ap()
    kb = KB(nc)
    with kb.gst:
        kb.setup_globals(ident)
        mod = kb.modulation(c, cctx, ada_w, ada_b, gmix, gffn)
        hm = kb.dram("hm0", [NT_Q * 128, 1024], F32)
        layer0_mixer(kb, xcat, rope, mod, wqkv, qn, kn, wo, hm)
        ffn_dense_pass(kb, hm, hout, mod, w13, w2, 2816, ffn_blocks(NT_Q), tile_set)
        kb.P.emit(nc)
    return nc


def rope_table(tokens, dim):
    tokens = np.asarray(tokens)
    q = dim // 4
    inv = (10000.0 ** (-np.arange(q, dtype=np.float32) / np.float32(q))).astype(np.float32)
    r = (np.maximum(tokens, 0) // 64).astype(np.float32)[:, None] * inv
    c = (np.maximum(tokens, 0) % 64).astype(np.float32)[:, None] * inv
    ang = np.concatenate([r, r, c, c], axis=-1).astype(np.float32)
    cos = np.cos(ang).astype(np.float32)
    sin = np.sin(ang).astype(np.float32)
    sgn = np.concatenate([-np.ones(q), np.ones(q), -np.ones(q), np.ones(q)]).astype(np.float32)
    out = np.stack([cos, sin * sgn], axis=1)
    nopos = tokens < 0
    out[nopos, 0, :] = 1.0
    out[nopos, 1, :] = 0.0
    return np.ascontiguousarray(out.astype(np.float32))


def core_tokens(hf):
    own = np.arange(hf * 4096, (hf + 1) * 4096)
    oth = np.arange((1 - hf) * 4096, (2 - hf) * 4096)
    return own, oth


_NC_CACHE = {}


def run_A(inp):
    if "A" not in _NC_CACHE:
        _NC_CACHE["A"] = build_A()
    nc = _NC_CACHE["A"]
    f = lambda a: np.ascontiguousarray(np.asarray(a, dtype=np.float32))
    maps = []
    for core in range(8):
        b, hf = core // 2, core % 2
        own, oth = core_tokens(hf)
        toks = np.concatenate([-np.ones(256, dtype=np.int64), own, oth])
        xcat = np.concatenate([inp["ctx"][b], inp["x"][b][own], inp["x"][b][oth]], axis=0)
        maps.append({
            "xcat": f(xcat), "rope128": rope_table(toks, 128),
            "c": f(inp["c"][b:b + 1]), "cctx": f(inp["c_ctx"][None, :]),
            "ada_w": f(inp["ada_w"][0]), "ada_b": f(inp["ada_b"][0:1]),
            "gmix": f(inp["norm_mix"][0:1]), "gffn": f(inp["norm_ffn"][0:1]),
            "wqkv": f(inp["a_wqkv"][0]), "qn": f(inp["a_q_norm"][0:1]), "kn": f(inp["a_k_norm"][0:1]), "wo": f(inp["a_wo"][0]),
            "w13": f(inp["ffn_w13"][0]), "w2": f(inp["ffn_w2"][0]), "ident": np.eye(128, dtype=np.float32),
        })
    res = run_bass_kernel_spmd(nc, maps, core_ids=list(range(8)))
    h1 = np.zeros((4, 8192, 1024), np.float32)
    hc1 = np.zeros((4, 256, 1024), np.float32)
    for core in range(8):
        b, hf = core // 2, core % 2
        o = res.results[core]["hout"]
        hc1[b] = o[0:256]
        h1[b, hf * 4096:(hf + 1) * 4096] = o[256:]
    return h1, hc1


def layer1_mixer(kb, hcat, rope64, mod, wdown_d, qln_d, kvln_d, wuq_d, wukv_d, wo_d, hm_dst, nq_tiles=NT_Q, nk_tiles=NT_ALL, qtiles=None):
    if qtiles is None:
        qtiles = list(range(nq_tiles))
    nq_tiles = len(qtiles)
    qpos = {t: i for i, t in enumerate(qtiles)}
    NQ, NK = nq_tiles * 128, nk_tiles * 128
    KT_s = kb.dram("b_KT", [8, 128, NK], BF16)
    KR_s = kb.dram("b_KR", [128, NK], BF16)
    V_s = kb.dram("b_V", [8, NK, 129], BF16)
    QT_s = kb.dram("b_QT", [8, 128, NQ], BF16)
    QR_s = kb.dram("b_QR", [4, 128, NQ], BF16)
    O_s = kb.dram("b_O", [NQ, 1024], BF16)
    with kb.scope():
        wdown, r_wdown = kb.sb([128, 8, 704], BF16, "wdown")
        load_weight(kb, wdown[:], r_wdown, wdown_d)
        wuq, r_wuq = kb.sb([128, 3, 1536], BF16, "wuq")
        load_weight(kb, wuq[:], r_wuq, wuq_d)
        wukv, r_wukv = kb.sb([128, 2, 2048], BF16, "wukv")
        load_weight(kb, wukv[:], r_wukv, wukv_d)
        gql, r_gql = kb.sb([128, 384], F32, "gql")
        gkv, r_gkv = kb.sb([128, 256], F32, "gkv")
        kb.load(gql[:], r_gql, qln_d.broadcast_to([128, 384]))
        kb.load(gkv[:], r_gkv, kvln_d.broadcast_to([128, 256]))
        fb = _front_bufs(kb, nh=2)
        tb = norm_bufs(kb)
        aTrot = kb.sbrot(2, [128, 8, 128], BF16, "aT")
        csrot = kb.sbrot(2, [128, 2, 64], F32, "cs")
        ckfrot = kb.sbrot(2, [128, 320], F32, "ckf")
        cknrot = kb.sbrot(2, [128, 256], BF16, "ckn")
        cknTrot = kb.sbrot(2, [128, 2, 128], BF16, "cknT")
        krrot = kb.sbrot(2, [128, 2, 64], BF16, "kr")
        krTrot = kb.sbrot(2, [128, 128], BF16, "krT")
        kTrot = kb.sbrot(2, [128, 8, 128], BF16, "kT")
        vtrot = kb.sbrot(2, [128, 8, 129], BF16, "vt")
        for vt, r_vt in vtrot.items:
            kb.pool(lambda e, vt=vt: e.memset(vt[:], 1.0), w=[r_vt])
        dqfrot = kb.sbrot(2, [128, 384], F32, "dqf")
        dqnrot = kb.sbrot(2, [128, 384], BF16, "dqn")
        dqnTrot = kb.sbrot(2, [128, 3, 128], BF16, "dqnT")
        qTrot = kb.sbrot(2, [128, 8, 128], BF16, "qT")
        qrfrot = kb.sbrot(2, [128, 8, 64], F32, "qrf")
        qr16rot = kb.sbrot(2, [128, 8, 64], BF16, "qr16")
        qrTrot = kb.sbrot(2, [128, 4, 128], BF16, "qrT")
        wukv_v = wukv[:].rearrange("p k (h x) -> p k h x", x=256)
        wuq_v = wuq[:].rearrange("p k (h x) -> p k h x", x=192)

        def bfview(bank):
            pt, r_pt = kb.pb[bank]
            return pt[:].bitcast(BF16).rearrange("p (k j) -> p k j", j=128), r_pt

        for t in range(nk_tiles):
            rows = slice(t * 128, (t + 1) * 128)
            aT, r_aT = aTrot.next()
            front(kb, fb, hcat[rows, :], mod, tile_set(t), 0, aT[:], r_aT)
            cs, r_cs = csrot.next()
            kb.load(cs[:], r_cs, rope64[rows, :, :])
            pkv, r_pkv = kb.pb[0]
            for k in range(8):
                kb.pe(lambda e, k=k, aT=aT: e.matmul(pkv[:, 0:320], lhsT=aT[:, k, :], rhs=wdown[:, k, 384:704], start=(k == 0), stop=(k == 7)),
                      r=[r_aT, r_wdown], w=[r_pkv])
            ckf, r_ckf = ckfrot.next()
            kb.act(lambda e, ckf=ckf: e.activation(out=ckf[:], in_=pkv[:, 0:320], func=AF.Copy), r=[r_pkv], w=[r_ckf])
            ckn, r_ckn = cknrot.next()
            qk_norm_rope(kb, ckf[:, 0:256].unsqueeze(1), r_ckf, 1, 256, (gkv, r_gkv), None, ckn[:].unsqueeze(1), r_ckn, tb)
            ptb, r_pt = bfview(6)
            for kc in range(2):
                kb.pe(lambda e, kc=kc, ckn=ckn, ptb=ptb: e.transpose(out=ptb[:, kc, :], in_=ckn[:, kc * 128:(kc + 1) * 128], identity=kb.identb[:]),
                      r=[r_ckn, kb.r_identb], w=[r_pt])
            cknT, r_cknT = cknTrot.next()
            kb.act(lambda e, cknT=cknT, ptb=ptb: e.activation(out=cknT[:], in_=ptb[:, 0:2, :], func=AF.Copy), r=[r_pt], w=[r_cknT])
            kr, r_kr = krrot.next()
            qk_norm_rope(kb, ckf[:, 256:320].unsqueeze(1), r_ckf, 1, 64, None, (cs, r_cs), kr[:, 0:1, :], r_kr, tb)
            kb.dve(lambda e, kr=kr: e.tensor_copy(out=kr[:, 1, :], in_=kr[:, 0, :]), r=[r_kr], w=[r_kr])
            ptb, r_pt = bfview(6)
            kb.pe(lambda e, kr=kr, ptb=ptb: e.transpose(out=ptb[:, 2, :], in_=kr[:].rearrange("p a d -> p (a d)"), identity=kb.identb[:]),
                  r=[r_kr, kb.r_identb], w=[r_pt])
            krT, r_krT = krTrot.next()
            kb.act(lambda e, krT=krT, ptb=ptb: e.activation(out=krT[:], in_=ptb[:, 2, :], func=AF.Copy), r=[r_pt], w=[r_krT])
            kb.store(KR_s[:, rows], krT[:], r_krT)
            kT, r_kT = kTrot.next()
            for hg in range(2):
                pk, r_pk = kb.pb[1 + hg]
                for hh in range(4):
                    h = hg * 4 + hh
                    for kc in range(2):
                        kb.pe(lambda e, pk=pk, hh=hh, h=h, kc=kc, cknT=cknT: e.matmul(pk[:, hh * 128:(hh + 1) * 128], lhsT=wukv_v[:, kc, h, 0:128],
                                                                                     rhs=cknT[:, kc, :], start=(kc == 0 and hh == 0), stop=(kc == 1),
                                                                                     skip_group_check=True),
                              r=[r_wukv, r_cknT], w=[r_pk])
                kb.act(lambda e, pk=pk, hg=hg, kT=kT: e.activation(out=kT[:, hg * 4:(hg + 1) * 4, :], in_=pk[:].rearrange("p (h t) -> p h t", t=128),
                                                                  func=AF.Copy), r=[r_pk], w=[r_kT])
            kb.store(KT_s[:, :, rows].rearrange("h d t -> d h t"), kT[:], r_kT)
            vt, r_vt = vtrot.next()
            for hg in range(2):
                pv, r_pv = kb.pb[3 + hg]
                for kc in range(2):
                    kb.pe(lambda e, pv=pv, hg=hg, kc=kc, cknT=cknT: e.matmul(pv[:].rearrange("p (h d) -> p h d", d=128), lhsT=cknT[:, kc, :],
                                                                           rhs=wukv_v[:, kc, hg * 4:(hg + 1) * 4, 128:256],
                                                                           start=(kc == 0), stop=(kc == 1)),
                          r=[r_wukv, r_cknT], w=[r_pv])
                kb.dve(lambda e, pv=pv, hg=hg, vt=vt: e.tensor_copy(out=vt[:, hg * 4:(hg + 1) * 4, 0:128], in_=pv[:].rearrange("p (h d) -> p h d", d=128)),
                       r=[r_pv], w=[r_vt])
            kb.store(V_s[:, rows, :].rearrange("h p d -> p h d"), vt[:], r_vt)
            if t not in qpos:
                continue
            qrows = slice(qpos[t] * 128, (qpos[t] + 1) * 128)
            pdq, r_pdq = kb.pb[0]
            for k in range(8):
                kb.pe(lambda e, k=k, aT=aT: e.matmul(pdq[:, 0:384], lhsT=aT[:, k, :], rhs=wdown[:, k, 0:384], start=(k == 0), stop=(k == 7)),
                      r=[r_aT, r_wdown], w=[r_pdq])
            dqf, r_dqf = dqfrot.next()
            kb.act(lambda e, dqf=dqf: e.activation(out=dqf[:], in_=pdq[:, 0:384], func=AF.Copy), r=[r_pdq], w=[r_dqf])
            dqn, r_dqn = dqnrot.next()
            qk_norm_rope(kb, dqf[:].unsqueeze(1), r_dqf, 1, 384, (gql, r_gql), None, dqn[:].unsqueeze(1), r_dqn, tb)
            ptb, r_pt = bfview(6)
            for kc in range(3):
                kb.pe(lambda e, kc=kc, dqn=dqn, ptb=ptb: e.transpose(out=ptb[:, 3 + kc, :], in_=dqn[:, kc * 128:(kc + 1) * 128], identity=kb.identb[:]),
                      r=[r_dqn, kb.r_identb], w=[r_pt])
            dqnT, r_dqnT = dqnTrot.next()
            kb.act(lambda e, dqnT=dqnT, ptb=ptb: e.activation(out=dqnT[:], in_=ptb[:, 3:6, :], func=AF.Copy), r=[r_pt], w=[r_dqnT])
            qT, r_qT = qTrot.next()
            for hg in range(2):
                pk, r_pk = kb.pb[1 + hg]
                for hh in range(4):
                    h = hg * 4 + hh
                    for kc in range(3):
                        kb.pe(lambda e, pk=pk, hh=hh, h=h, kc=kc, dqnT=dqnT: e.matmul(pk[:, hh * 128:(hh + 1) * 128], lhsT=wuq_v[:, kc, h, 0:128],
                                                                                     rhs=dqnT[:, kc, :], start=(kc == 0 and hh == 0), stop=(kc == 2),
                                                                                     skip_group_check=True),
                              r=[r_wuq, r_dqnT], w=[r_pk])
                kb.act(lambda e, pk=pk, hg=hg, qT=qT: e.activation(out=qT[:, hg * 4:(hg + 1) * 4, :], in_=pk[:].rearrange("p (h t) -> p h t", t=128),
                                                                  func=AF.Copy), r=[r_pk], w=[r_qT])
            kb.store(QT_s[:, :, qrows].rearrange("h d t -> d h t"), qT[:], r_qT)
            pqr, r_pqr = kb.pb[3]
            for kc in range(3):
                kb.pe(lambda e, kc=kc, dqnT=dqnT: e.matmul(pqr[:].rearrange("p (h d) -> p h d", d=64), lhsT=dqnT[:, kc, :],
                                                           rhs=wuq_v[:, kc, :, 128:192], start=(kc == 0), stop=(kc == 2)),
                      r=[r_wuq, r_dqnT], w=[r_pqr])
            qrf, r_qrf = qrfrot.next()
            kb.act(lambda e, qrf=qrf: e.activation(out=qrf[:], in_=pqr[:].rearrange("p (h d) -> p h d", d=64), func=AF.Copy), r=[r_pqr], w=[r_qrf])
            qr16, r_qr16 = qr16rot.next()
            qk_norm_rope(kb, qrf[:], r_qrf, 8, 64, None, (cs, r_cs), qr16[:], r_qr16, tb)
            ptb5, r_pt5 = bfview(5)
            for pr in range(4):
                kb.pe(lambda e, pr=pr, qr16=qr16, ptb5=ptb5: e.transpose(out=ptb5[:, pr, :], in_=qr16[:, 2 * pr:2 * pr + 2, :].rearrange("p a d -> p (a d)"),
                                                                       identity=kb.identb[:]), r=[r_qr16, kb.r_identb], w=[r_pt5])
            qrT, r_qrT = qrTrot.next()
            kb.act(lambda e, qrT=qrT, ptb5=ptb5: e.activation(out=qrT[:], in_=ptb5[:, 0:4, :], func=AF.Copy), r=[r_pt5], w=[r_qrT])
            kb.store(QR_s[:, :, qrows].rearrange("h d t -> d h t"), qrT[:], r_qrT)
    with kb.scope():
        krt, r_krt = kb.sb([128, NK], BF16, "KR")
        kb.load(krt[:], r_krt, KR_s)
        ktrot = kb.sbrot(2, [128, NK], BF16, "KT")
        vrot = kb.sbrot(2, [128, nk_tiles, 129], BF16, "V")
        qrot = kb.sbrot(2, [128, NQ], BF16, "QT")
        qrrot = kb.sbrot(2, [128, NQ], BF16, "QR")
        state = {}

        def load_kv(h):
            kt_t, r_kt = ktrot.next()
            kb.load(kt_t[:], r_kt, KT_s[h])
            v_t, r_v = vrot.next()
            kb.load(v_t[:], r_v, V_s[h].rearrange("(kt p) d -> p kt d", p=128))
            return [(kt_t, r_kt, 128, 0), (krt, r_krt, 64, (h % 2) * 64)], (v_t, r_v)

        def load_q(h):
            q_t, r_q = qrot.next()
            kb.load(q_t[:], r_q, QT_s[h])
            if h % 2 == 0:
                state["qr"] = qrrot.next()
                kb.load(state["qr"][0][:], state["qr"][1], QR_s[h // 2])
            qr_t, r_qr = state["qr"]
            return [(q_t, r_q, 128, 0), (qr_t, r_qr, 64, (h % 2) * 64)]

        heads = [(h, h) for h in range(8)]
        attention_core(kb, heads, {"kv": load_kv, "q": load_q}, std_groups(nq_tiles, nk_tiles), 128, O_s, float(192 ** -0.5))
    outproj_pass(kb, O_s, wo_d, hcat, hm_dst, mod, [(i, t) for i, t in enumerate(qtiles)], tile_set)


def moe_pass(kb, h_src, h_dst, mod, router_d, w13_d, w2_d, blocks, tile_set, n_exp=8, FF=3584, final=None, dst_row0=0, dbg=None, exp_loop=8):
    for blk in blocks:
        _moe_block(kb, blk, h_src, h_dst, mod, router_d, w13_d, w2_d, tile_set, n_exp, FF, final, dst_row0, dbg, exp_loop)


def _moe_block(kb, blk, h_src, h_dst, mod, router_d, w13_d, w2_d, tile_set, n_exp, FF, final, dst_row0, dbg, exp_loop):
    UF = FF // 2
    nfu = UF // 128
    if True:
        nt = len(blk)
        TB = nt * 128
        s = tile_set(blk[0])
        assert all(tile_set(t) == s for t in blk)
        with kb.scope():
            tT, r_tT = kb.sb([128, 8, TB], BF16, "tT")
            acc, r_acc = kb.sb([128, nt, 1024], F32, "acc")
            gates, r_gates = kb.sb([128, nt, 8], F32, "gates")
            with kb.scope():
                fb = _front_bufs(kb, nh=2)
                rcol, r_rcol = kb.sb([128, 8, 8], F32, "rcol")
                kb.load(rcol[:], r_rcol, router_d.rearrange("(k p) e -> p k e", p=128))
                Rbc, r_Rbc = kb.sb([128, 8, 1024], F32, "Rbc")
                dex, r_dex = kb.sb([128, 8, 128], F32, "dex")
                constc, r_constc = kb.sb([128, 8], F32, "constc")
                jf, r_jf = kb.sb([128, 1024], F32, "junkf")
                for e_ in range(n_exp):
                    kb.dve(lambda e, e_=e_: e.tensor_tensor(out=dex[:], in0=kb.identf[:].unsqueeze(1).broadcast_to([128, 8, 128]),
                                                            in1=rcol[:, :, e_:e_ + 1].broadcast_to([128, 8, 128]), op=ALU.mult),
                           r=[kb.r_identf, r_rcol], w=[r_dex])
                    for half in range(2):
                        pr, r_pr = kb.pb[half]
                        kb.pe(lambda e, half=half, pr=pr: e.matmul(pr[:], lhsT=kb.onesf[:], rhs=dex[:, half * 4:(half + 1) * 4, :],
                                                                   start=True, stop=True), r=[kb.r_onesf, r_dex], w=[r_pr])
                        kb.act(lambda e, half=half, pr=pr, e_=e_: e.activation(out=Rbc[:, e_, half * 512:(half + 1) * 512], in_=pr[:], func=AF.Copy),
                               r=[r_pr], w=[r_Rbc])
                    kb.dve(lambda e, e_=e_: e.scalar_tensor_tensor(out=jf[:], in0=Rbc[:, e_, :], scalar=1.0, in1=mod["ab2"][:, s, 1, :],
                                                                    op0=ALU.mult, op1=ALU.mult, accum_out=constc[:, e_:e_ + 1]),
                           r=[r_Rbc, mod["r_ab2"]], w=[r_jf, r_constc])
                    kb.dve(lambda e, e_=e_: e.tensor_tensor(out=Rbc[:, e_, :], in0=Rbc[:, e_, :], in1=mod["ab2"][:, s, 0, :], op=ALU.mult),
                           r=[r_Rbc, mod["r_ab2"]], w=[r_Rbc])
                lgrot = kb.sbrot(2, [128, 64], F32, "lg")
                for j, t in enumerate(blk):
                    ht, r_h, stt, r_st = front(kb, fb, h_src[t * 128:(t + 1) * 128, :], mod, s, 1, tT[:, :, j * 128:(j + 1) * 128], r_tT)
                    lg, r_lg = lgrot.next()
                    for e_ in range(n_exp):
                        kb.dve(lambda e, e_=e_, ht=ht, stt=stt, lg=lg: e.scalar_tensor_tensor(out=jf[:], in0=ht[:], scalar=stt[:, 2:3], in1=Rbc[:, e_, :],
                                                                                               op0=ALU.mult, op1=ALU.mult, accum_out=lg[:, e_:e_ + 1]),
                               r=[r_h, r_st, r_Rbc], w=[r_jf, r_lg])
                    L = lg[:, 0:8]
                    kb.dve(lambda e, lg=lg: e.tensor_tensor(out=lg[:, 0:8], in0=lg[:, 0:8], in1=constc[:], op=ALU.add), r=[r_lg, r_constc], w=[r_lg])
                    kb.dve(lambda e, lg=lg: e.tensor_reduce(out=lg[:, 8:9], in_=lg[:, 0:8], axis=AX.X, op=ALU.max), r=[r_lg], w=[r_lg])
                    kb.dve(lambda e, lg=lg: e.tensor_scalar(out=lg[:, 16:24], in0=lg[:, 0:8], scalar1=lg[:, 8:9], scalar2=None, op0=ALU.is_equal),
                           r=[r_lg], w=[r_lg])
                    kb.dve(lambda e, lg=lg: e.scalar_tensor_tensor(out=lg[:, 24:32], in0=lg[:, 16:24], scalar=-1e30, in1=lg[:, 0:8],
                                                                   op0=ALU.mult, op1=ALU.add), r=[r_lg], w=[r_lg])
                    kb.dve(lambda e, lg=lg: e.tensor_reduce(out=lg[:, 9:10], in_=lg[:, 24:32], axis=AX.X, op=ALU.max), r=[r_lg], w=[r_lg])
                    kb.dve(lambda e, lg=lg: e.tensor_scalar(out=lg[:, 32:40], in0=lg[:, 24:32], scalar1=lg[:, 9:10], scalar2=None, op0=ALU.is_equal),
                           r=[r_lg], w=[r_lg])
                    kb.dve(lambda e, lg=lg: e.tensor_tensor(out=lg[:, 10:11], in0=lg[:, 9:10], in1=lg[:, 8:9], op=ALU.subtract), r=[r_lg], w=[r_lg])
                    kb.act(lambda e, lg=lg: e.activation(out=lg[:, 11:12], in_=lg[:, 10:11], func=AF.Exp), r=[r_lg], w=[r_lg])
                    kb.dve(lambda e, lg=lg: e.tensor_scalar(out=lg[:, 12:13], in0=lg[:, 11:12], scalar1=1.0, scalar2=None, op0=ALU.add), r=[r_lg], w=[r_lg])
                    kb.dve(lambda e, lg=lg: e.reciprocal(out=lg[:, 13:14], in_=lg[:, 12:13]), r=[r_lg], w=[r_lg])
                    kb.dve(lambda e, lg=lg: e.tensor_tensor(out=lg[:, 14:15], in0=lg[:, 11:12], in1=lg[:, 13:14], op=ALU.mult), r=[r_lg], w=[r_lg])
                    kb.dve(lambda e, lg=lg: e.tensor_scalar(out=lg[:, 40:48], in0=lg[:, 16:24], scalar1=lg[:, 13:14], scalar2=None, op0=ALU.mult),
                           r=[r_lg], w=[r_lg])
                    kb.dve(lambda e, lg=lg, j=j: e.scalar_tensor_tensor(out=gates[:, j, :], in0=lg[:, 32:40], scalar=lg[:, 14:15], in1=lg[:, 40:48],
                                                                        op0=ALU.mult, op1=ALU.add), r=[r_lg], w=[r_gates])
                    if dbg is not None:
                        kb.store(dbg[t * 128:(t + 1) * 128, 0:64], lg[:], r_lg)
                        kb.store(dbg[t * 128:(t + 1) * 128, 64:72], gates[:, j, :], r_gates)
            with kb.scope():
                hid, r_hid = kb.sb([128, nfu, TB], BF16, "hid")
                w13rot = kb.sbrot(2, [128, 8, 2, 256], BF16, "w13c")
                w2t, r_w2t = kb.sb([128, nfu, 1024], BF16, "w2")
                sarot = kb.sbrot(2, [128, 512], BF16, "sa")
                hrot = kb.sbrot(2, [128, 1024], F32, "h2")
                fstrot = kb.sbrot(2, [128, 4], F32, "fst")
                tgs = [(g0, min(512, TB - g0)) for g0 in range(0, TB, 512)]
                first = True
                for e_ in range(exp_loop):
                    for uh in range(2):
                        base = uh * UF
                        for fc in range(0, nfu, 2):
                            wc, r_wc = w13rot.next()
                            for ab in range(2):
                                c0 = ab * FF + base + fc * 128
                                kb.P.dma("pool", wc[:, :, ab, :], w13_d[e_, :, c0:c0 + 256].rearrange("(k p) n -> p k n", p=128), w=[r_wc], sem=r_wc)
                            for sub in range(2):
                                f = fc + sub
                                for gi, (g0, gn) in enumerate(tgs):
                                    pa, r_pa = kb.pb[(gi % 2) * 2]
                                    pbk, r_pbk = kb.pb[(gi % 2) * 2 + 1]
                                    for ab, (pp, r_pp) in enumerate(((pa, r_pa), (pbk, r_pbk))):
                                        for k in range(8):
                                            kb.pe(lambda e, pp=pp, wc=wc, k=k, ab=ab, sub=sub, g0=g0, gn=gn:
                                                  e.matmul(pp[:, 0:gn], lhsT=wc[:, k, ab, sub * 128:(sub + 1) * 128], rhs=tT[:, k, g0:g0 + gn],
                                                           start=(k == 0), stop=(k == 7)), r=[r_wc, r_tT], w=[r_pp])
                                    sa, r_sa = sarot.next()
                                    kb.act(lambda e, sa=sa, pa=pa, gn=gn: e.activation(out=sa[:, 0:gn], in_=pa[:, 0:gn], func=AF.Silu), r=[r_pa], w=[r_sa])
                                    kb.dve(lambda e, sa=sa, pbk=pbk, f=f, g0=g0, gn=gn: e.tensor_tensor(out=hid[:, f, g0:g0 + gn], in0=pbk[:, 0:gn],
                                                                                                         in1=sa[:, 0:gn], op=ALU.mult),
                                           r=[r_pbk, r_sa], w=[r_hid])
                        for f0 in range(0, nfu, 2):
                            kb.P.dma("pool", w2t[:, f0:f0 + 2, :], w2_d[e_, base + f0 * 128:base + (f0 + 2) * 128, :].rearrange("(k p) n -> p k n", p=128),
                                     w=[r_w2t], sem=r_w2t)
                        for j in range(nt):
                            for half in range(2):
                                po, r_po = kb.pb[4 + (j % 2) * 2 + half]
                                for f in range(nfu):
                                    kb.pe(lambda e, po=po, f=f, j=j, half=half: e.matmul(po[:], lhsT=hid[:, f, j * 128:(j + 1) * 128],
                                                                                        rhs=w2t[:, f, half * 512:(half + 1) * 512],
                                                                                        start=(f == 0), stop=(f == nfu - 1)),
                                          r=[r_hid, r_w2t], w=[r_po])
                                if first:
                                    kb.dve(lambda e, po=po, j=j, half=half, e_=e_: e.tensor_scalar(out=acc[:, j, half * 512:(half + 1) * 512], in0=po[:],
                                                                                                   scalar1=gates[:, j, e_:e_ + 1], scalar2=None, op0=ALU.mult),
                                           r=[r_po, r_gates], w=[r_acc])
                                else:
                                    kb.dve(lambda e, po=po, j=j, half=half, e_=e_: e.scalar_tensor_tensor(out=acc[:, j, half * 512:(half + 1) * 512], in0=po[:],
                                                                                                          scalar=gates[:, j, e_:e_ + 1],
                                                                                                          in1=acc[:, j, half * 512:(half + 1) * 512],
                                                                                                          op0=ALU.mult, op1=ALU.add),
                                           r=[r_po, r_gates, r_acc], w=[r_acc])
                        first = False
                for j, t in enumerate(blk):
                    ht, r_h = hrot.next()
                    kb.load(ht[:], r_h, h_src[t * 128:(t + 1) * 128, :])
                    kb.dve(lambda e, j=j: e.tensor_tensor(out=acc[:, j, :], in0=acc[:, j, :], in1=mod["g"][:, s, 1, :], op=ALU.mult),
                           r=[r_acc, mod["r_g"]], w=[r_acc])
                    kb.pool(lambda e, j=j, ht=ht: e.tensor_tensor(out=ht[:], in0=ht[:], in1=acc[:, j, :], op=ALU.add), r=[r_acc, r_h], w=[r_h])
                    if final is not None:
                        gfin, r_gfin = final
                        fst, r_fst = fstrot.next()
                        kb.dve(lambda e, j=j, ht=ht, fst=fst: e.scalar_tensor_tensor(out=acc[:, j, :], in0=ht[:], scalar=1.0, in1=ht[:], op0=ALU.mult,
                                                                                     op1=ALU.mult, accum_out=fst[:, 0:1]), r=[r_h], w=[r_acc, r_fst])
                        kb.rsqrt_cols(fst[:, 2:3], r_fst, fst[:, 0:1], r_fst, 1, 1.0 / 1024, fst[:, 1:2], r_fst)
                        kb.dve(lambda e, ht=ht, fst=fst: e.tensor_scalar(out=ht[:], in0=ht[:], scalar1=fst[:, 2:3], scalar2=None, op0=ALU.mult),
                               r=[r_h, r_fst], w=[r_h])
                        kb.pool(lambda e, ht=ht: e.tensor_tensor(out=ht[:], in0=ht[:], in1=gfin[:], op=ALU.mult), r=[r_h, r_gfin], w=[r_h])
                    kb.store(h_dst[t * 128 - dst_row0:(t + 1) * 128 - dst_row0, :], ht[:], r_h)


def build_B(stage='all'):
    nc = bass.Bass("TRN2", target_bir_lowering=False)
    d = lambda name, shape: nc.dram_tensor(name, list(shape), F32, kind="ExternalInput").ap()
    hcat = d("hcat", [NT_ALL * 128, 1024])
    rope = d("rope64", [NT_ALL * 128, 2, 64])
    c = d("c", [1, 1024]); cctx = d("cctx", [1, 1024])
    ada_w = d("ada_w", [1024, 6144]); ada_b = d("ada_b", [1, 6144])
    gmix = d("gmix", [1, 1024]); gffn = d("gffn", [1, 1024])
    wdown = d("wdown", [1024, 704]); qln = d("qln", [1, 384]); kvln = d("kvln", [1, 256])
    wuq = d("wuq", [384, 1536]); wukv = d("wukv", [256, 2048]); wo = d("wo", [1024, 1024])
    router = d("router", [1024, 8]); w13 = d("mw13", [8, 1024, 7168]); w2 = d("mw2", [8, 3584, 1024]); ident = d("ident", [128, 128])
    hout = nc.dram_tensor("hout", [NT_Q * 128, 1024], F32, kind="ExternalOutput").ap()
    kb = KB(nc)
    with kb.gst:
        kb.setup_globals(ident)
        mod = kb.modulation(c, cctx, ada_w, ada_b, gmix, gffn, need_ab2=True)
        hm = kb.dram("hm1", [NT_Q * 128, 1024], F32)
        if stage == 'mixer':
            layer1_mixer(kb, hcat, rope, mod, wdown, qln, kvln, wuq, wukv, wo, hout)
        elif stage == 'moe':
            moe_pass(kb, hcat, hout, mod, router, w13, w2, ffn_blocks(NT_Q), tile_set)
        else:
            layer1_mixer(kb, hcat, rope, mod, wdown, qln, kvln, wuq, wukv, wo, hm)
            moe_pass(kb, hm, hout, mod, router, w13, w2, ffn_blocks(NT_Q), tile_set)
        kb.P.emit(nc)
    return nc


def run_B(inp, h1, hc1, layer=1, stage='all'):
    if "B" + stage not in _NC_CACHE:
        _NC_CACHE["B" + stage] = build_B(stage)
    nc = _NC_CACHE["B" + stage]
    f = lambda a: np.ascontiguousarray(np.asarray(a, dtype=np.float32))
    p = layer // 2
    maps = []
    for core in range(8):
        b, hf = core // 2, core % 2
        own, oth = core_tokens(hf)
        toks = np.concatenate([-np.ones(256, dtype=np.int64), own, oth])
        hcat = np.concatenate([hc1[b], h1[b][own], h1[b][oth]], axis=0)
        maps.append({
            "hcat": f(hcat), "rope64": rope_table(toks, 64),
            "c": f(inp["c"][b:b + 1]), "cctx": f(inp["c_ctx"][None, :]),
            "ada_w": f(inp["ada_w"][layer]), "ada_b": f(inp["ada_b"][layer:layer + 1]),
            "gmix": f(inp["norm_mix"][layer:layer + 1]), "gffn": f(inp["norm_ffn"][layer:layer + 1]),
            "wdown": f(inp["b_w_down"][0]), "qln": f(inp["b_q_lora_norm"][0:1]), "kvln": f(inp["b_kv_lora_norm"][0:1]),
            "wuq": f(inp["b_w_uq"][0]), "wukv": f(inp["b_w_ukv"][0]), "wo": f(inp["b_wo"][0]),
            "router": f(inp["moe_router"][p]), "mw13": f(inp["moe_w13"][p]), "mw2": f(inp["moe_w2"][p]),
            "ident": np.eye(128, dtype=np.float32),
        })
    res = run_bass_kernel_spmd(nc, maps, core_ids=list(range(8)))
    h2 = np.zeros((4, 8192, 1024), np.float32)
    hc2 = np.zeros((4, 256, 1024), np.float32)
    for core in range(8):
        b, hf = core // 2, core % 2
        o = res.results[core]["hout"]
        hc2[b] = o[0:256]
        h2[b, hf * 4096:(hf + 1) * 4096] = o[256:]
    return h2, hc2


def layer2_mixer(kb, h_src, mod, win_d, lng_d, lnb_d, wsp_d, bsp_d, wout_d, hm_dst, tiles, tile_set):
    NT = max(tiles) + 1
    O_s = kb.dram("c_O", [NT * 128, 1024], BF16)
    with kb.scope():
        win, r_win = kb.sb([128, 8, 2048], BF16, "win")
        load_weight(kb, win[:], r_win, win_d)
        lng, r_lng = kb.sb([128, 1024], F32, "lng")
        lnb, r_lnb = kb.sb([128, 1024], F32, "lnb")
        kb.load(lng[:], r_lng, lng_d.broadcast_to([128, 1024]))
        kb.load(lnb[:], r_lnb, lnb_d.broadcast_to([128, 1024]))
        bsbc, r_bsbc = kb.sb([128, 1024], F32, "bsbc")
        kb.load(bsbc[:], r_bsbc, bsp_d.broadcast_to([128, 1024]))
        bscol, r_bscol = kb.sb([128, 8], F32, "bscol")
        dtmp, r_dtmp = kb.sb([128, 8, 128], F32, "dtmp")
        kb.diag_extract(bscol[:], r_bscol, bsbc[:], r_bsbc, 8, dtmp, r_dtmp)
        wsp, r_wsp = kb.sb([128, 8, 128], BF16, "wsp")
        kb.P.dma("pool", wsp[:], wsp_d.rearrange("g p q -> p g q"), w=[r_wsp], sem=r_wsp)
        wsT, r_wsT = kb.sb([128, 8, 128], BF16, "wsT")
        pt, r_pt = kb.pb[6]
        ptb = pt[:].bitcast(BF16).rearrange("p (k j) -> p k j", j=128)
        for g in range(8):
            kb.pe(lambda e, g=g: e.transpose(out=ptb[:, g, :], in_=wsp[:, g, :], identity=kb.identb[:]), r=[r_wsp, kb.r_identb], w=[r_pt])
        kb.act(lambda e: e.activation(out=wsT[:], in_=ptb, func=AF.Copy), r=[r_pt], w=[r_wsT])
        fb = _front_bufs(kb, nh=2)
        aTrot = kb.sbrot(2, [128, 8, 128], BF16, "aT")
        urot = kb.sbrot(2, [128, 1024], BF16, "u")
        vrot = kb.sbrot(2, [128, 1024], F32, "v")
        vnrot = kb.sbrot(2, [128, 1024], BF16, "vn")
        strot = kb.sbrot(2, [128, 32], F32, "lnst")
        gtrot = kb.sbrot(2, [128, 1024], BF16, "gt")
        for t in tiles:
            rows = slice(t * 128, (t + 1) * 128)
            aT, r_aT = aTrot.next()
            front(kb, fb, h_src[rows, :], mod, tile_set(t), 0, aT[:], r_aT)
            u, r_u = urot.next()
            v, r_v = vrot.next()
            for cb in range(4):
                pu, r_pu = kb.pb[cb]
                for k in range(8):
                    kb.pe(lambda e, k=k, cb=cb, pu=pu, aT=aT: e.matmul(pu[:], lhsT=aT[:, k, :], rhs=win[:, k, cb * 512:(cb + 1) * 512],
                                                                      start=(k == 0), stop=(k == 7)), r=[r_aT, r_win], w=[r_pu])
                if cb < 2:
                    kb.act(lambda e, cb=cb, pu=pu, u=u: e.activation(out=u[:, cb * 512:(cb + 1) * 512], in_=pu[:], func=AF.Gelu), r=[r_pu], w=[r_u])
                else:
                    kb.act(lambda e, cb=cb, pu=pu, v=v: e.activation(out=v[:, (cb - 2) * 512:(cb - 1) * 512], in_=pu[:], func=AF.Gelu), r=[r_pu], w=[r_v])
            st, r_st = strot.next()
            for hb in range(2):
                kb.dve(lambda e, hb=hb, st=st, v=v: e.bn_stats(out=st[:, hb * 6:(hb + 1) * 6], in_=v[:, hb * 512:(hb + 1) * 512]), r=[r_v], w=[r_st])
            kb.dve(lambda e, st=st: e.bn_aggr(out=st[:, 12:14], in_=st[:, 0:12]), r=[r_st], w=[r_st])
            kb.rsqrt_cols(st[:, 16:17], r_st, st[:, 13:14], r_st, 1, 1.0, st[:, 15:16], r_st)
            kb.dve(lambda e, st=st, v=v: e.tensor_scalar(out=v[:], in0=v[:], scalar1=st[:, 12:13], scalar2=st[:, 16:17], op0=ALU.subtract, op1=ALU.mult),
                   r=[r_v, r_st], w=[r_v])
            kb.pool(lambda e, v=v: e.tensor_tensor(out=v[:], in0=v[:], in1=lng[:], op=ALU.mult), r=[r_v, r_lng], w=[r_v])
            vn, r_vn = vnrot.next()
            kb.dve(lambda e, v=v, vn=vn: e.tensor_tensor(out=vn[:], in0=v[:], in1=lnb[:], op=ALU.add), r=[r_v, r_lnb], w=[r_vn])
            gt, r_gt = gtrot.next()
            for hb in range(2):
                pm, r_pm = kb.pb[4 + hb]
                for gg in range(4):
                    g = hb * 4 + gg
                    kb.pe(lambda e, g=g, gg=gg, pm=pm, vn=vn: e.matmul(pm[:, gg * 128:(gg + 1) * 128], lhsT=wsT[:, g, :], rhs=vn[:, g * 128:(g + 1) * 128],
                                                                      start=(gg == 0), stop=True, skip_group_check=True), r=[r_wsT, r_vn], w=[r_pm])
                for gg in range(4):
                    g = hb * 4 + gg
                    kb.dve(lambda e, g=g, gg=gg, pm=pm, gt=gt, u=u: e.scalar_tensor_tensor(out=gt[:, g * 128:(g + 1) * 128], in0=pm[:, gg * 128:(gg + 1) * 128],
                                                                                          scalar=bscol[:, g:g + 1], in1=u[:, g * 128:(g + 1) * 128],
                                                                                          op0=ALU.add, op1=ALU.mult), r=[r_pm, r_bscol, r_u], w=[r_gt])
            kb.store(O_s[rows, :], gt[:], r_gt)
    outproj_pass(kb, O_s, wout_d, h_src, hm_dst, mod, tiles, tile_set)


def layer3_mixer(kb, h_src, rope64, mod, wqkv_d, sinks_d, wo_d, masks_d, hm_dst, ktile_src=None):
    NTK = 36
    if ktile_src is None:
        ktile_src = list(range(36))
    qtiles = list(range(2, 34))
    O_s = kb.dram("d_O", [34 * 128, 1024], BF16)
    with kb.scope():
        wqkv, r_wqkv = kb.sb([128, 8, 1280], BF16, "wqkv")
        load_weight(kb, wqkv[:], r_wqkv, wqkv_d)
        esink, r_esink = kb.sb([128, 16], F32, "esink")
        kb.load(esink[:], r_esink, sinks_d.broadcast_to([128, 16]))
        kb.act(lambda e: e.activation(out=esink[:], in_=esink[:], func=AF.Exp), r=[r_esink], w=[r_esink])
        mkf, r_mkf = kb.sb([128, 4, 128], F32, "mkf")
        kb.load(mkf[:], r_mkf, masks_d.rearrange("m k q -> k m q"))
        mk, r_mk = kb.sb([128, 4, 128], BF16, "mk")
        kb.dve(lambda e: e.tensor_copy(out=mk[:], in_=mkf[:]), r=[r_mkf], w=[r_mk])
        KT, r_KT = kb.sb([128, 2, NTK * 128], BF16, "KT")
        VV, r_VV = kb.sb([128, NTK, 2, 65], BF16, "VV")
        kb.pool(lambda e: e.memset(VV[:], 1.0), w=[r_VV])
        fb = _front_bufs(kb, nh=2)
        tb = norm_bufs(kb)
        aTrot = kb.sbrot(2, [128, 8, 128], BF16, "aT")
        csrot = kb.sbrot(2, [128, 2, 64], F32, "cs")
        kfrot = kb.sbrot(2, [128, 2, 64], F32, "kf")
        k16rot = kb.sbrot(2, [128, 2, 2, 64], BF16, "k16")
        qfrot = kb.sbrot(2, [128, 16, 64], F32, "qf")
        q16rot = kb.sbrot(2, [128, 16, 64], BF16, "q16")
        qTrot = kb.sbrot(2, [128, 8, 128], BF16, "qT")
        ptrot = kb.sbrot(3, [128, 640], BF16, "pT")
        otrot = kb.sbrot(2, [128, 16, 64], BF16, "ot")
        denrot = kb.sbrot(2, [128, 32], F32, "den")

        def bfview(bank):
            pt, r_pt = kb.pb[bank]
            return pt[:].bitcast(BF16).rearrange("p (k j) -> p k j", j=128), r_pt

        for t in range(NTK):
            rows = slice(ktile_src[t] * 128, (ktile_src[t] + 1) * 128)
            aT, r_aT = aTrot.next()
            front(kb, fb, h_src[rows, :], mod, tile_set(t), 0, aT[:], r_aT)
            cs, r_cs = csrot.next()
            kb.load(cs[:], r_cs, rope64[rows, :, :])
            pkv, r_pkv = kb.pb[0]
            for k in range(8):
                kb.pe(lambda e, k=k, aT=aT: e.matmul(pkv[:, 0:256], lhsT=aT[:, k, :], rhs=wqkv[:, k, 1024:1280], start=(k == 0), stop=(k == 7)),
                      r=[r_aT, r_wqkv], w=[r_pkv])
            kb.act(lambda e, t=t: e.activation(out=VV[:, t, :, 0:64], in_=pkv[:, 128:256].rearrange("p (h d) -> p h d", d=64), func=AF.Copy),
                   r=[r_pkv], w=[r_VV])
            kf, r_kf = kfrot.next()
            kb.act(lambda e, kf=kf: e.activation(out=kf[:], in_=pkv[:, 0:128].rearrange("p (h d) -> p h d", d=64), func=AF.Copy), r=[r_pkv], w=[r_kf])
            k16, r_k16 = k16rot.next()
            qk_norm_rope(kb, kf[:], r_kf, 2, 64, None, (cs, r_cs), k16[:, :, 0, :], r_k16, tb)
            kb.dve(lambda e, k16=k16: e.tensor_copy(out=k16[:, :, 1, :], in_=k16[:, :, 0, :]), r=[r_k16], w=[r_k16])
            ptb, r_pt = bfview(6)
            for kvh in range(2):
                kb.pe(lambda e, kvh=kvh, k16=k16, ptb=ptb: e.transpose(out=ptb[:, kvh, :], in_=k16[:, kvh, :, :].rearrange("p a d -> p (a d)"),
                                                                     identity=kb.identb[:]), r=[r_k16, kb.r_identb], w=[r_pt])
            kb.act(lambda e, t=t, ptb=ptb: e.activation(out=KT[:, :, t * 128:(t + 1) * 128], in_=ptb[:, 0:2, :], func=AF.Copy), r=[r_pt], w=[r_KT])
        for t in qtiles:
            rows = slice(t * 128, (t + 1) * 128)
            aT, r_aT = aTrot.next()
            front(kb, fb, h_src[rows, :], mod, 0, 0, aT[:], r_aT)
            cs, r_cs = csrot.next()
            kb.load(cs[:], r_cs, rope64[rows, :, :])
            qf, r_qf = qfrot.next()
            for half in range(2):
                pq, r_pq = kb.pb[half]
                for k in range(8):
                    kb.pe(lambda e, k=k, aT=aT, pq=pq, half=half: e.matmul(pq[:], lhsT=aT[:, k, :], rhs=wqkv[:, k, half * 512:(half + 1) * 512],
                                                                          start=(k == 0), stop=(k == 7)), r=[r_aT, r_wqkv], w=[r_pq])
                kb.act(lambda e, qf=qf, pq=pq, half=half: e.activation(out=qf[:, half * 8:(half + 1) * 8, :],
                                                                       in_=pq[:].rearrange("p (h d) -> p h d", d=64), func=AF.Copy), r=[r_pq], w=[r_qf])
            q16, r_q16 = q16rot.next()
            qk_norm_rope(kb, qf[:], r_qf, 16, 64, None, (cs, r_cs), q16[:], r_q16, tb)
            ptb, r_pt = bfview(6)
            for pr in range(8):
                kb.pe(lambda e, pr=pr, q16=q16, ptb=ptb: e.transpose(out=ptb[:, pr, :], in_=q16[:, 2 * pr:2 * pr + 2, :].rearrange("p a d -> p (a d)"),
                                                                   identity=kb.identb[:]), r=[r_q16, kb.r_identb], w=[r_pt])
            qT, r_qT = qTrot.next()
            kb.act(lambda e, qT=qT, ptb=ptb: e.activation(out=qT[:], in_=ptb, func=AF.Copy), r=[r_pt], w=[r_qT])
            left = t - 1 if t > 2 else 34
            right = t + 1 if t < 33 else 35
            ktl = [(0, None), (1, None), (left, 0 if t > 2 else 2), (t, None), (right, 1 if t < 33 else 3)]
            obanks = (2, 3, 4)
            for h in range(16):
                kvh, base = h // 8, (h % 2) * 64
                psA, r_psA = kb.pb[5]
                psB, r_psB = kb.pb[7]
                for ki, (kt, mi) in enumerate(ktl):
                    ps, r_ps, c0 = (psA, r_psA, ki * 128) if ki < 4 else (psB, r_psB, 0)
                    kb.pe(lambda e, ps=ps, c0=c0, kvh=kvh, base=base, kt=kt, qT=qT, h=h, ki=ki, mi=mi:
                          e.matmul(ps[:, c0:c0 + 128], lhsT=KT[base:base + 64, kvh, kt * 128:(kt + 1) * 128], rhs=qT[base:base + 64, h // 2, :],
                                   start=(ki == 0 or ki == 4), stop=(mi is None), skip_group_check=True),
                          r=[r_KT, r_qT], w=[r_ps])
                    if mi is not None:
                        kb.pe(lambda e, ps=ps, c0=c0, mi=mi: e.matmul(ps[:, c0:c0 + 128], lhsT=kb.identb[:], rhs=mk[:, mi, :], start=False, stop=True,
                                                                      skip_group_check=True), r=[kb.r_identb, r_mk], w=[r_ps])
                pT, r_pT = ptrot.next()
                kb.act(lambda e, pT=pT, psA=psA: e.activation(out=pT[:, 0:512], in_=psA[:], func=AF.Exp, scale=0.125), r=[r_psA], w=[r_pT])
                kb.act(lambda e, pT=pT, psB=psB: e.activation(out=pT[:, 512:640], in_=psB[:, 0:128], func=AF.Exp, scale=0.125), r=[r_psB], w=[r_pT])
                ob, r_ob = kb.pb[obanks[h // 7]]
                oc = (h % 7) * 65
                for ki, (kt, mi) in enumerate(ktl):
                    kb.pe(lambda e, ob=ob, oc=oc, pT=pT, ki=ki, kt=kt, kvh=kvh, h=h:
                          e.matmul(ob[:, oc:oc + 65], lhsT=pT[:, ki * 128:(ki + 1) * 128], rhs=VV[:, kt, kvh, :],
                                   start=(ki == 0 and h % 7 == 0), stop=(ki == 4), skip_group_check=True),
                          r=[r_pT, r_VV], w=[r_ob])
            ot, r_ot = otrot.next()
            den, r_den = denrot.next()
            for bi, (h0, nh_) in enumerate(((0, 7), (7, 7), (14, 2))):
                ob, r_ob = kb.pb[obanks[bi]]
                ov = ob[:, 0:nh_ * 65].rearrange("p (h d) -> p h d", d=65)
                kb.dve(lambda e, ov=ov, h0=h0, nh_=nh_, den=den: e.tensor_tensor(out=den[:, h0:h0 + nh_], in0=ov[:, :, 64], in1=esink[:, h0:h0 + nh_], op=ALU.add),
                       r=[r_ob, r_esink], w=[r_den])
                kb.dve(lambda e, h0=h0, nh_=nh_, den=den: e.reciprocal(out=den[:, 16 + h0:16 + h0 + nh_], in_=den[:, h0:h0 + nh_]), r=[r_den], w=[r_den])
                kb.dve(lambda e, ov=ov, h0=h0, nh_=nh_, den=den, ot=ot: e.tensor_tensor(out=ot[:, h0:h0 + nh_, :], in0=ov[:, :, 0:64],
                                                                                       in1=den[:, 16 + h0:16 + h0 + nh_].unsqueeze(2).broadcast_to([128, nh_, 64]),
                                                                                       op=ALU.mult), r=[r_ob, r_den], w=[r_ot])
            kb.store(O_s[rows, :], ot[:].rearrange("p h d -> p (h d)"), r_ot)
    outproj_pass(kb, O_s, wo_d, h_src, hm_dst, mod, qtiles, tile_set)


def tile_set_C(t):
    return 1 if t < 2 else 0


def build_C(debug=False):
    nc = bass.Bass("TRN2", target_bir_lowering=False)
    d = lambda name, shape: nc.dram_tensor(name, list(shape), F32, kind="ExternalInput").ap()
    hcat = d("hcat", [36 * 128, 1024])
    rope = d("rope64", [36 * 128, 2, 64])
    c = d("c", [1, 1024]); cctx = d("cctx", [1, 1024])
    ada_w2 = d("ada_w2", [1024, 6144]); ada_b2 = d("ada_b2", [1, 6144]); gmix2 = d("gmix2", [1, 1024]); gffn2 = d("gffn2", [1, 1024])
    ada_w3 = d("ada_w3", [1024, 6144]); ada_b3 = d("ada_b3", [1, 6144]); gmix3 = d("gmix3", [1, 1024]); gffn3 = d("gffn3", [1, 1024])
    win = d("c_win", [1024, 2048]); lng = d("c_lng", [1, 1024]); lnb = d("c_lnb", [1, 1024])
    wsp = d("c_wsp", [8, 128, 128]); bsp = d("c_bsp", [1, 1024]); wout = d("c_wout", [1024, 1024])
    w13 = d("w13", [1024, 5632]); w2 = d("w2", [2816, 1024])
    dwqkv = d("d_wqkv", [1024, 1280]); sinks = d("d_sinks", [1, 16]); dwo = d("d_wo", [1024, 1024]); masks = d("masks", [4, 128, 128])
    router = d("router", [1024, 8]); mw13 = d("mw13", [8, 1024, 7168]); mw2 = d("mw2", [8, 3584, 1024])
    gfin_d = d("gfin", [1, 1024]); ident = d("ident", [128, 128])
    out = nc.dram_tensor("out", [4096, 1024], F32, kind="ExternalOutput").ap()
    kb = KB(nc)
    with kb.gst:
        kb.setup_globals(ident)
        hm2 = kb.dram("hm2", [36 * 128, 1024], F32)
        h3s = kb.dram("h3s", [36 * 128, 1024], F32, kind=("ExternalOutput" if debug else "Internal"))
        hm3 = kb.dram("hm3", [34 * 128, 1024], F32, kind=("ExternalOutput" if debug else "Internal"))
        with kb.scope():
            mod2 = kb.modulation(c, cctx, ada_w2, ada_b2, gmix2, gffn2)
            layer2_mixer(kb, hcat, mod2, win, lng, lnb, wsp, bsp, wout, hm2, list(range(36)), tile_set_C)
            blocks = [[0, 1, 34, 35]] + [list(range(b0, b0 + 8)) for b0 in range(2, 34, 8)]
            ffn_dense_pass(kb, hm2, h3s, mod2, w13, w2, 2816, blocks, tile_set_C)
        with kb.scope():
            mod3 = kb.modulation(c, cctx, ada_w3, ada_b3, gmix3, gffn3, need_ab2=True)
            layer3_mixer(kb, h3s, rope, mod3, dwqkv, sinks, dwo, masks, hm3)
            gfin, r_gfin = kb.sb([128, 1024], F32, "gfin")
            kb.load(gfin[:], r_gfin, gfin_d.broadcast_to([128, 1024]))
            blocks = [list(range(b0, b0 + 8)) for b0 in range(2, 34, 8)]
            moe_pass(kb, hm3, out, mod3, router, mw13, mw2, blocks, tile_set_C, final=(gfin, r_gfin), dst_row0=256)
        kb.P.emit(nc)
    return nc


def band_masks(hf):
    k = np.arange(128)[:, None]
    q = np.arange(128)[None, :]
    NEG = np.float32(-30000.0)
    left = np.where(k >= q, 0.0, NEG).astype(np.float32)
    right = np.where(k <= q, 0.0, NEG).astype(np.float32)
    allm = np.full((128, 128), NEG, np.float32)
    return np.ascontiguousarray(np.stack([left, right, allm if hf == 0 else left, allm if hf == 1 else right]))


def run_C(inp, h2, hc2, debug=False):
    if ("C", debug) not in _NC_CACHE:
        _NC_CACHE[("C", debug)] = build_C(debug)
    nc = _NC_CACHE[("C", debug)]
    f = lambda a: np.ascontiguousarray(np.asarray(a, dtype=np.float32))
    maps = []
    for core in range(8):
        b, hf = core // 2, core % 2
        own = np.arange(hf * 4096, (hf + 1) * 4096)
        lh = np.arange(hf * 4096 - 128, hf * 4096)
        rh = np.arange((hf + 1) * 4096, (hf + 1) * 4096 + 128)
        lh_ok, rh_ok = lh[0] >= 0, rh[-1] < 8192
        hl = h2[b][lh] if lh_ok else np.zeros((128, 1024), np.float32)
        hr = h2[b][rh] if rh_ok else np.zeros((128, 1024), np.float32)
        toks = np.concatenate([-np.ones(256, dtype=np.int64), own, lh if lh_ok else -np.ones(128, dtype=np.int64),
                               rh if rh_ok else -np.ones(128, dtype=np.int64)])
        hcat = np.concatenate([hc2[b], h2[b][own], hl, hr], axis=0)
        maps.append({
            "hcat": f(hcat), "rope64": rope_table(toks, 64),
            "c": f(inp["c"][b:b + 1]), "cctx": f(inp["c_ctx"][None, :]),
            "ada_w2": f(inp["ada_w"][2]), "ada_b2": f(inp["ada_b"][2:3]), "gmix2": f(inp["norm_mix"][2:3]), "gffn2": f(inp["norm_ffn"][2:3]),
            "ada_w3": f(inp["ada_w"][3]), "ada_b3": f(inp["ada_b"][3:4]), "gmix3": f(inp["norm_mix"][3:4]), "gffn3": f(inp["norm_ffn"][3:4]),
            "c_win": f(inp["c_w_in"][0]), "c_lng": f(inp["c_ln_g"][0:1]), "c_lnb": f(inp["c_ln_b"][0:1]),
            "c_wsp": f(inp["c_w_spatial"][0]), "c_bsp": f(inp["c_b_spatial"][0].reshape(1, 1024)), "c_wout": f(inp["c_w_out"][0]),
            "w13": f(inp["ffn_w13"][1]), "w2": f(inp["ffn_w2"][1]),
            "d_wqkv": f(inp["d_wqkv"][0]), "d_sinks": f(inp["d_sinks"][0:1]), "d_wo": f(inp["d_wo"][0]), "masks": band_masks(hf),
            "router": f(inp["moe_router"][1]), "mw13": f(inp["moe_w13"][1]), "mw2": f(inp["moe_w2"][1]),
            "gfin": f(inp["final_norm"][None, :]), "ident": np.eye(128, dtype=np.float32),
        })
    res = run_bass_kernel_spmd(nc, maps, core_ids=list(range(8)))
    out = np.zeros((4, 8192, 1024), np.float32)
    for core in range(8):
        b, hf = core // 2, core % 2
        out[b, hf * 4096:(hf + 1) * 4096] = res.results[core]["out"]
    if debug:
        return out, res.results
    return out


def kernel(**inputs):
    inp = {k: np.asarray(v) for k, v in inputs.items()}
    return run_fused(inp)


def tile_set_F(t):
    return 1 if t < 2 else 0


def build_fused():
    nc = bass.Bass("TRN2", target_bir_lowering=False)
    d = lambda name, shape: nc.dram_tensor(name, list(shape), F32, kind="ExternalInput").ap()
    xcat = d("xcat", [66 * 128, 1024])
    rope128 = d("rope128", [66 * 128, 2, 128])
    rope64 = d("rope64", [66 * 128, 2, 64])
    c = d("c", [1, 1024]); cctx = d("cctx", [1, 1024])
    ada_w = d("ada_w", [4, 1024, 6144]); ada_b = d("ada_b", [4, 6144]); gmix = d("gmix", [4, 1024]); gffn = d("gffn", [4, 1024])
    a_wqkv = d("a_wqkv", [1024, 1536]); a_qn = d("a_qn", [1, 128]); a_kn = d("a_kn", [1, 128]); a_wo = d("a_wo", [1024, 1024])
    b_wdown = d("b_wdown", [1024, 704]); b_qln = d("b_qln", [1, 384]); b_kvln = d("b_kvln", [1, 256])
    b_wuq = d("b_wuq", [384, 1536]); b_wukv = d("b_wukv", [256, 2048]); b_wo = d("b_wo", [1024, 1024])
    c_win = d("c_win", [1024, 2048]); c_lng = d("c_lng", [1, 1024]); c_lnb = d("c_lnb", [1, 1024])
    c_wsp = d("c_wsp", [8, 128, 128]); c_bsp = d("c_bsp", [1, 1024]); c_wout = d("c_wout", [1024, 1024])
    d_wqkv = d("d_wqkv", [1024, 1280]); d_sinks = d("d_sinks", [1, 16]); d_wo = d("d_wo", [1024, 1024]); masks = d("masks", [4, 128, 128])
    ffn_w13 = d("ffn_w13", [2, 1024, 5632]); ffn_w2 = d("ffn_w2", [2, 2816, 1024])
    router = d("router", [2, 1024, 8]); mw13 = d("mw13", [2, 8, 1024, 7168]); mw2 = d("mw2", [2, 8, 3584, 1024])
    gfin_d = d("gfin", [1, 1024]); ident = d("ident", [128, 128])
    out = nc.dram_tensor("out", [4096, 1024], F32, kind="ExternalOutput").ap()
    kb = KB(nc)
    halo = [34, 65]
    lat36 = list(range(2, 34)) + halo
    with kb.gst:
        kb.setup_globals(ident)
        hm0 = kb.dram("hm0", [66 * 128, 1024], F32)
        h1 = kb.dram("h1", [66 * 128, 1024], F32)
        hm1 = kb.dram("hm1", [66 * 128, 1024], F32)
        h2 = kb.dram("h2", [66 * 128, 1024], F32)
        hm2 = kb.dram("hm2", [66 * 128, 1024], F32)
        h3 = kb.dram("h3", [66 * 128, 1024], F32)
        hm3 = kb.dram("hm3", [34 * 128, 1024], F32)
        mk = lambda l, ab2: kb.modulation(c, cctx, ada_w[l], ada_b[l:l + 1, :], gmix[l:l + 1, :], gffn[l:l + 1, :], need_ab2=ab2)
        with kb.scope():
            mod = mk(0, False)
            layer0_mixer(kb, xcat, rope128, mod, a_wqkv, a_qn, a_kn, a_wo, hm0, nq_tiles=66, nk_tiles=66)
            blocks = [[0, 1]] + [list(range(b0, b0 + 8)) for b0 in range(2, 66, 8)]
            ffn_dense_pass(kb, hm0, h1, mod, ffn_w13[0], ffn_w2[0], 2816, blocks, tile_set_F)
        with kb.scope():
            mod = mk(1, True)
            layer1_mixer(kb, h1, rope64, mod, b_wdown, b_qln, b_kvln, b_wuq, b_wukv, b_wo, hm1, nk_tiles=66, qtiles=[0, 1] + lat36)
            blocks = [[0, 1], lat36[0:9], lat36[9:18], lat36[18:26], lat36[26:34]]
            moe_pass(kb, hm1, h2, mod, router[0], mw13[0], mw2[0], blocks, tile_set_F)
        with kb.scope():
            mod = mk(2, False)
            layer2_mixer(kb, h2, mod, c_win, c_lng, c_lnb, c_wsp, c_bsp, c_wout, hm2, [0, 1] + lat36, tile_set_F)
            blocks = [[0, 1] + halo] + [list(range(b0, b0 + 8)) for b0 in range(2, 34, 8)]
            ffn_dense_pass(kb, hm2, h3, mod, ffn_w13[1], ffn_w2[1], 2816, blocks, tile_set_F)
        with kb.scope():
            mod = mk(3, True)
            layer3_mixer(kb, h3, rope64, mod, d_wqkv, d_sinks, d_wo, masks, hm3, ktile_src=list(range(34)) + [65, 34])
            gfin, r_gfin = kb.sb([128, 1024], F32, "gfin")
            kb.load(gfin[:], r_gfin, gfin_d.broadcast_to([128, 1024]))
            blocks = [list(range(b0, b0 + 8)) for b0 in range(2, 34, 8)]
            moe_pass(kb, hm3, out, mod, router[1], mw13[1], mw2[1], blocks, tile_set_F, final=(gfin, r_gfin), dst_row0=256)
        kb.P.emit(nc)
    return nc


def run_fused(inp):
    if "F" not in _NC_CACHE:
        _NC_CACHE["F"] = build_fused()
    nc = _NC_CACHE["F"]
    f = lambda a: np.ascontiguousarray(np.asarray(a, dtype=np.float32))
    shared = {
        "cctx": f(inp["c_ctx"][None, :]), "ada_w": f(inp["ada_w"]), "ada_b": f(inp["ada_b"]), "gmix": f(inp["norm_mix"]), "gffn": f(inp["norm_ffn"]),
        "a_wqkv": f(inp["a_wqkv"][0]), "a_qn": f(inp["a_q_norm"][0:1]), "a_kn": f(inp["a_k_norm"][0:1]), "a_wo": f(inp["a_wo"][0]),
        "b_wdown": f(inp["b_w_down"][0]), "b_qln": f(inp["b_q_lora_norm"][0:1]), "b_kvln": f(inp["b_kv_lora_norm"][0:1]),
        "b_wuq": f(inp["b_w_uq"][0]), "b_wukv": f(inp["b_w_ukv"][0]), "b_wo": f(inp["b_wo"][0]),
        "c_win": f(inp["c_w_in"][0]), "c_lng": f(inp["c_ln_g"][0:1]), "c_lnb": f(inp["c_ln_b"][0:1]),
        "c_wsp": f(inp["c_w_spatial"][0]), "c_bsp": f(inp["c_b_spatial"][0].reshape(1, 1024)), "c_wout": f(inp["c_w_out"][0]),
        "d_wqkv": f(inp["d_wqkv"][0]), "d_sinks": f(inp["d_sinks"][0:1]), "d_wo": f(inp["d_wo"][0]),
        "ffn_w13": f(inp["ffn_w13"]), "ffn_w2": f(inp["ffn_w2"]),
        "router": f(inp["moe_router"]), "mw13": f(inp["moe_w13"]), "mw2": f(inp["moe_w2"]),
        "gfin": f(inp["final_norm"][None, :]), "ident": np.eye(128, dtype=np.float32),
    }
    maps = []
    for core in range(8):
        b, hf = core // 2, core % 2
        own, oth = core_tokens(hf)
        toks = np.concatenate([-np.ones(256, dtype=np.int64), own, oth])
        xcat = np.concatenate([inp["ctx"][b], inp["x"][b][own], inp["x"][b][oth]], axis=0)
        m = dict(shared)
        m.update({"xcat": f(xcat), "rope128": rope_table(toks, 128), "rope64": rope_table(toks, 64),
                  "c": f(inp["c"][b:b + 1]), "masks": band_masks(hf)})
        maps.append(m)
    res = run_bass_kernel_spmd(nc, maps, core_ids=list(range(8)))
    out = np.zeros((4, 8192, 1024), np.float32)
    for core in range(8):
        b, hf = core // 2, core % 2
        out[b, hf * 4096:(hf + 1) * 4096] = res.results[core]["out"]
    return out


def kernel_unfused(**inputs):
    inp = {k: np.asarray(v) for k, v in inputs.items()}
    h1, hc1 = run_A(inp)
    h2, hc2 = run_B(inp, h1, hc1)
    return run_C(inp, h2, hc2)
```

```python
import contextlib
import numpy as np
import concourse.bass as bass
import concourse.mybir as mybir
from concourse.bass_utils import run_bass_kernel_spmd

F32 = mybir.dt.float32
BF16 = mybir.dt.bfloat16
AF = mybir.ActivationFunctionType
ALU = mybir.AluOpType
AX = mybir.AxisListType

COMPUTE = ("pe", "act", "dve", "pool")
QUEUES = ("sp", "act", "pool")
STREAMS = ("pe", "act", "dve", "pool", "sp")
EPS = 1e-6


class Res:
    __slots__ = ("name", "last_w", "readers", "dma_sem", "dma_cnt")

    def __init__(self, name):
        self.name = name
        self.last_w = None
        self.readers = []
        self.dma_sem = None
        self.dma_cnt = 0


class Op:
    __slots__ = ("idx", "eng", "fn", "is_dma", "waits", "inc", "count", "res", "dma_val", "deps", "dma_sem")

    def __init__(self, idx, eng, fn, is_dma):
        self.idx = idx
        self.eng = eng
        self.fn = fn
        self.is_dma = is_dma
        self.waits = {}
        self.inc = False
        self.count = None
        self.res = None
        self.dma_val = None
        self.deps = ()


class Prog:
    def __init__(self):
        self.ops = []
        self.n_dma_sems = 0
        self.last_compute = {}
        self.last_dma = {}
        self.free_sems = []

    def _add(self, eng, fn, r, w, is_dma, sem_res=None):
        op = Op(len(self.ops), eng, fn, is_dma)
        deps = set()
        for x in r:
            if x.last_w is not None:
                deps.add(x.last_w)
        soft = set()
        for x in w:
            if x.last_w is not None:
                soft.add(x.last_w)
            soft.update(x.readers)
        for dd in soft:
            dop = self.ops[dd]
            if (not is_dma) and (not dop.is_dma) and dop.eng == eng:
                continue
            deps.add(dd)
        if eng == "pe" and not is_dma:
            deps = {dd for dd in deps if self.ops[dd].is_dma or self.ops[dd].eng != "pe"}
        op.deps = deps
        for x in r:
            if not is_dma:
                x.readers = [i for i in x.readers if self.ops[i].is_dma or self.ops[i].eng != eng]
            x.readers.append(op.idx)
        for x in w:
            x.last_w = op.idx
            x.readers = []
        if is_dma:
            if sem_res.dma_sem is None:
                if self.free_sems:
                    sem_res.dma_sem, sem_res.dma_cnt = self.free_sems.pop()
                else:
                    sem_res.dma_sem = self.n_dma_sems
                    self.n_dma_sems += 1
            sem_res.dma_cnt += 16
            op.res = sem_res
            op.dma_sem = sem_res.dma_sem
            op.dma_val = sem_res.dma_cnt
            self.last_dma[sem_res.dma_sem] = op.idx
        else:
            self.last_compute[eng] = op.idx
        self.ops.append(op)
        return op

    def op(self, eng, fn, r=(), w=()):
        return self._add(eng, fn, list(r), list(w), False)

    def dma(self, q, out, in_, r=(), w=(), sem=None, extra_deps=()):
        op = self._add(q, (out, in_), list(r), list(w), True, sem_res=sem)
        op.deps.update(extra_deps)
        return op

    def barrier(self):
        deps = set(self.last_compute.values()) | set(self.last_dma.values())
        for s in STREAMS:
            op = Op(len(self.ops), s, None, False)
            op.deps = set(deps)
            self.ops.append(op)

    def plan(self):
        ops = self.ops
        for op in ops:
            for d in op.deps:
                if not ops[d].is_dma:
                    ops[d].inc = True
        cnt = {e: 0 for e in COMPUTE}
        for op in ops:
            if not op.is_dma and op.fn is not None and op.inc:
                cnt[op.eng] += 1
                op.count = cnt[op.eng]
        known = {s: {} for s in STREAMS}
        for op in ops:
            kn = known[op.eng]
            for d in op.deps:
                dop = ops[d]
                if dop.is_dma:
                    key, val = ("d", dop.dma_sem), dop.dma_val
                else:
                    key, val = ("c", dop.eng), dop.count
                if kn.get(key, 0) >= val:
                    continue
                if op.waits.get(key, 0) < val:
                    op.waits[key] = val
            for k, v in op.waits.items():
                kn[k] = max(kn.get(k, 0), v)
        self.final_counts = cnt

    def emit(self, nc):
        self.plan()
        ops = self.ops
        with contextlib.ExitStack() as st:
            csem = {e: st.enter_context(nc.semaphore("c_" + e)) for e in COMPUTE}
            dsem = [st.enter_context(nc.semaphore(f"d{i}")) for i in range(self.n_dma_sems)]
            block = st.enter_context(nc.Block())

            def semof(key):
                return csem[key[1]] if key[0] == "c" else dsem[key[1]]

            streams = {s: [] for s in STREAMS}
            for op in ops:
                streams[op.eng].append(op)
            seen = {}
            for op in ops:
                if op.is_dma:
                    seen[op.dma_sem] = max(seen.get(op.dma_sem, 0), op.dma_val)

            def run(engname, eng):
                for op in streams[engname]:
                    for key, val in op.waits.items():
                        eng.wait_ge(semof(key), val)
                    if op.fn is None:
                        continue
                    if op.is_dma:
                        out, in_ = op.fn
                        eng.dma_start(out=out, in_=in_).then_inc(dsem[op.dma_sem], 16)
                    else:
                        ins = op.fn(eng)
                        if op.inc:
                            ins.then_inc(csem[engname], 1)

            @block.tensor
            def _(e):
                run("pe", e)

            @block.scalar
            def _(e):
                run("act", e)

            @block.vector
            def _(e):
                run("dve", e)

            @block.gpsimd
            def _(e):
                run("pool", e)

            @block.sync
            def _(e):
                run("sp", e)
                for k, v in seen.items():
                    e.wait_ge(dsem[k], v)
                for en in COMPUTE:
                    if self.final_counts[en]:
                        e.wait_ge(csem[en], self.final_counts[en])


class Rot:
    def __init__(self, items):
        self.items = items
        self.i = 0

    def next(self):
        it = self.items[self.i % len(self.items)]
        self.i += 1
        return it


class KB:
    def __init__(self, nc):
        self.nc = nc
        self.P = Prog()
        self.gst = contextlib.ExitStack()
        self.cur = self.gst
        self._n = 0
        self.scope_res = [[]]
        self.recent_stores = []

    def sb(self, shape, dt=F32, name=None):
        self._n += 1
        name = f"{name or 't'}_{self._n}"
        t = self.cur.enter_context(self.nc.sbuf_tensor(name, list(shape), dt))
        r = Res(name)
        self.scope_res[-1].append(r)
        return t, r

    def sbrot(self, n, shape, dt=F32, name=None):
        return Rot([self.sb(shape, dt, name) for _ in range(n)])

    @contextlib.contextmanager
    def scope(self):
        prev = self.cur
        with contextlib.ExitStack() as st:
            self.cur = st
            self.scope_res.append([])
            yield
            self.P.barrier()
            for r in self.scope_res.pop():
                if r.dma_sem is not None:
                    self.P.free_sems.append((r.dma_sem, r.dma_cnt))
                    r.dma_sem = None
        self.cur = prev

    def dram(self, name, shape, dt, kind="Internal"):
        return self.nc.dram_tensor(name, list(shape), dt, kind=kind).ap()

    def dve(self, fn, r=(), w=()):
        return self.P.op("dve", fn, r, w)

    def act(self, fn, r=(), w=()):
        return self.P.op("act", fn, r, w)

    def pe(self, fn, r=(), w=()):
        return self.P.op("pe", fn, r, w)

    def pool(self, fn, r=(), w=()):
        return self.P.op("pool", fn, r, w)

    def load(self, tile_ap, res, dram_ap, q="sp"):
        return self.P.dma(q, tile_ap, dram_ap, w=[res], sem=res)

    def store(self, dram_ap, tile_ap, res, q="sp"):
        rs = self.recent_stores
        extra = (rs[-STORE_WINDOW],) if len(rs) >= STORE_WINDOW else ()
        op = self.P.dma(q, dram_ap, tile_ap, r=[res], sem=res, extra_deps=extra)
        rs.append(op.idx)
        if len(rs) > 16:
            del rs[0]
        return op

    def setup_globals(self, ident_dram):
        nc = self.nc
        self.pb = []
        for i in range(8):
            t = self.gst.enter_context(nc.psum_tensor(f"pb{i}", [128, 512], F32))
            self.pb.append((t, Res(f"pb{i}")))
        self.identf, self.r_identf = self.sb([128, 128], F32, "identf")
        self.identb, self.r_identb = self.sb([128, 128], BF16, "identb")
        self.load(self.identf[:], self.r_identf, ident_dram)
        self.dve(lambda e: e.tensor_copy(out=self.identb[:], in_=self.identf[:]), r=[self.r_identf], w=[self.r_identb])
        self.nhalf, self.r_nhalf = self.sb([128, 1], F32, "nhalf")
        self.pool(lambda e: e.memset(self.nhalf[:], -0.5), w=[self.r_nhalf])
        self.onesf, self.r_onesf = self.sb([128, 128], F32, "onesf")
        self.pool(lambda e: e.memset(self.onesf[:], 1.0), w=[self.r_onesf])

    def rsqrt_cols(self, out_t, out_r, in_t, in_r, n, scale, tmp_t, tmp_r):
        self.dve(lambda e: e.tensor_scalar(out=tmp_t[:, 0:n], in0=in_t[:, 0:n], scalar1=scale, scalar2=EPS,
                                           op0=ALU.mult, op1=ALU.add), r=[in_r], w=[tmp_r])
        self.pool(lambda e: e.tensor_tensor(out=out_t[:, 0:n], in0=tmp_t[:, 0:n],
                                            in1=self.nhalf[:].broadcast_to([128, n]), op=ALU.pow),
                  r=[tmp_r, self.r_nhalf], w=[out_r])

    def diag_extract(self, col_ap, col_r, bc_ap, bc_r, n, tmp_t, tmp_r):
        self.dve(lambda e: e.tensor_tensor(out=tmp_t[:, 0:n, :], in0=bc_ap.rearrange("p (k j) -> p k j", j=128),
                                           in1=self.identf[:].unsqueeze(1).broadcast_to([128, n, 128]), op=ALU.mult),
                 r=[bc_r, self.r_identf], w=[tmp_r])
        self.dve(lambda e: e.tensor_reduce(out=col_ap, in_=tmp_t[:, 0:n, :], axis=AX.X, op=ALU.add), r=[tmp_r], w=[col_r])

    def modulation(self, c_row, cctx_row, ada_w, ada_b, gmix_row, gffn_row, need_ab2=False):
        m = {}
        m["cols"], m["r_cols"] = self.sb([128, 2, 4, 8], F32, "modcols")
        m["g"], m["r_g"] = self.sb([128, 2, 2, 1024], F32, "modg")
        if need_ab2:
            m["ab2"], m["r_ab2"] = self.sb([128, 2, 2, 1024], F32, "modab2")
        with self.scope():
            if not need_ab2:
                m["ab2"], m["r_ab2"] = self.sb([128, 2, 2, 1024], F32, "modab2")
            cbc, r_cbc = self.sb([128, 2, 1024], F32, "cbc")
            self.load(cbc[:, 0, :], r_cbc, c_row.broadcast_to([128, 1024]))
            self.load(cbc[:, 1, :], r_cbc, cctx_row.broadcast_to([128, 1024]))
            tmp, r_tmp = self.sb([128, 8, 128], F32, "dtmp")
            ccol, r_ccol = self.sb([128, 2, 8], F32, "ccol")
            for s in range(2):
                self.diag_extract(ccol[:, s, :], r_ccol, cbc[:, s, :], r_cbc, 8, tmp, r_tmp)
            scol, r_scol = self.sb([128, 2, 8], F32, "scol")
            self.act(lambda e: e.activation(out=scol[:], in_=ccol[:], func=AF.Silu), r=[r_ccol], w=[r_scol])
            srep, r_srep = self.sb([128, 2, 8, 128], F32, "srep")
            for s in range(2):
                for k in range(8):
                    self.dve(lambda e, s=s, k=k: e.tensor_copy(out=srep[:, s, k, :],
                                                               in_=scol[:, s, k:k + 1].broadcast_to([128, 128])),
                             r=[r_scol], w=[r_srep])
            adab, r_adab = self.sb([128, 6144], F32, "adab")
            self.load(adab[:], r_adab, ada_b.broadcast_to([128, 6144]))
            gn, r_gn = self.sb([128, 2, 1024], F32, "gn")
            self.load(gn[:, 0, :], r_gn, gmix_row.broadcast_to([128, 1024]))
            self.load(gn[:, 1, :], r_gn, gffn_row.broadcast_to([128, 1024]))
            modbc, r_modbc = self.sb([128, 2, 6144], F32, "modbc")
            wrot = self.sbrot(2, [128, 8, 512], F32, "adaw")
            for cg in range(12):
                wt, r_wt = wrot.next()
                self.load(wt[:], r_wt, ada_w[:, cg * 512:(cg + 1) * 512].rearrange("(k p) n -> p k n", p=128))
                for s in range(2):
                    pt, r_pt = self.pb[(cg * 2 + s) % 8]
                    for k in range(8):
                        self.pe(lambda e, s=s, k=k, wt=wt, pt=pt: e.matmul(pt[:], lhsT=srep[:, s, k, :], rhs=wt[:, k, :],
                                                                            start=(k == 0), stop=(k == 7)),
                                r=[r_srep, r_wt], w=[r_pt])
                    self.dve(lambda e, s=s, cg=cg, pt=pt: e.tensor_tensor(out=modbc[:, s, cg * 512:(cg + 1) * 512], in0=pt[:],
                                                                          in1=adab[:, cg * 512:(cg + 1) * 512], op=ALU.add),
                             r=[r_pt, r_adab], w=[r_modbc])
            abc, r_abc = self.sb([128, 1024], F32, "abc")
            for s in range(2):
                for which in range(2):
                    o = which * 3072
                    self.dve(lambda e, s=s, o=o, which=which: e.scalar_tensor_tensor(
                        out=(abc[:] if which == 0 else m["ab2"][:, s, 0, :]), in0=modbc[:, s, o + 1024:o + 2048], scalar=1.0,
                        in1=gn[:, which, :], op0=ALU.add, op1=ALU.mult), r=[r_modbc, r_gn], w=[r_abc if which == 0 else m["r_ab2"]])
                    src_ap = abc[:] if which == 0 else m["ab2"][:, s, 0, :]
                    src_r = r_abc if which == 0 else m["r_ab2"]
                    self.diag_extract(m["cols"][:, s, which * 2, :], m["r_cols"], src_ap, src_r, 8, tmp, r_tmp)
                    self.diag_extract(m["cols"][:, s, which * 2 + 1, :], m["r_cols"], modbc[:, s, o:o + 1024], r_modbc, 8, tmp, r_tmp)
                    self.dve(lambda e, s=s, o=o, which=which: e.tensor_copy(out=m["g"][:, s, which, :], in_=modbc[:, s, o + 2048:o + 3072]),
                             r=[r_modbc], w=[m["r_g"]])
                self.dve(lambda e, s=s: e.tensor_copy(out=m["ab2"][:, s, 1, :], in_=modbc[:, s, 3072:4096]), r=[r_modbc], w=[m["r_ab2"]])
        return m


ILV = 2
STORE_WINDOW = 3


ILV_OFF = set()


def interleave(gens, width=3, name=None):
    width = min(width, ILV)
    if name in ILV_OFF:
        width = 1
    active = []
    it = iter(gens)
    more = True
    while True:
        if more and len(active) < width:
            try:
                active.append(next(it))
            except StopIteration:
                more = False
        if not active:
            break
        for g in list(active):
            try:
                next(g)
            except StopIteration:
                active.remove(g)


def _front_bufs(kb, nh=3):
    fb = {}
    nh = max(nh, 4)
    fb["h"] = kb.sbrot(nh, [128, 1024], F32, "h")
    fb["junk"] = kb.sbrot(2, [128, 1024], BF16, "junk")
    fb["st"] = kb.sbrot(4, [128, 4], F32, "st")
    fb["hn"] = kb.sbrot(3, [128, 1024], BF16, "hn")
    return fb


def front(kb, fb, h_dram, mod, s, which, aT_ap, r_aT, pbank=7):
    ht, r_h = fb["h"].next()
    kb.load(ht[:], r_h, h_dram)
    stt, r_st = fb["st"].next()
    junk, r_junk = fb["junk"].next()
    kb.dve(lambda e: e.scalar_tensor_tensor(out=junk[:], in0=ht[:], scalar=1.0, in1=ht[:], op0=ALU.mult, op1=ALU.mult,
                                            accum_out=stt[:, 0:1]), r=[r_h], w=[r_junk, r_st])
    yield
    kb.rsqrt_cols(stt[:, 2:3], r_st, stt[:, 0:1], r_st, 1, 1.0 / 1024, stt[:, 1:2], r_st)
    yield
    hn, r_hn = fb["hn"].next()
    kb.act(lambda e: e.activation(out=hn[:], in_=ht[:], func=AF.Copy, scale=stt[:, 2:3]), r=[r_h, r_st], w=[r_hn])
    yield
    pt, r_pt = kb.pb[pbank]
    ptb = pt[:].bitcast(BF16).rearrange("p (k j) -> p k j", j=128)
    for k in range(8):
        kb.pe(lambda e, k=k: e.transpose(out=ptb[:, k, :], in_=hn[:, k * 128:(k + 1) * 128], identity=kb.identb[:]),
              r=[r_hn, kb.r_identb], w=[r_pt])
    yield
    cols = mod["cols"]
    for k in range(8):
        kb.dve(lambda e, k=k: e.tensor_scalar(out=aT_ap[:, k, :], in0=ptb[:, k, :], scalar1=cols[:, s, which * 2, k:k + 1],
                                              scalar2=cols[:, s, which * 2 + 1, k:k + 1], op0=ALU.mult, op1=ALU.add),
               r=[r_pt, mod["r_cols"]], w=[r_aT])
    yield
    return ht, r_h, stt, r_st


def load_weight(kb, dst_ap, res, w_dram, q="pool"):
    K, N = w_dram.shape
    step = 2048
    for c0 in range(0, N, step):
        c1 = min(N, c0 + step)
        kb.P.dma(q, dst_ap[:, :, c0:c1], w_dram[:, c0:c1].rearrange("(k p) n -> p k n", p=128), w=[res], sem=res)


def qk_norm_rope(kb, xv, r_xf, H, D, gain, cs, out_ap, r_out, tb):
    fl = lambda t: t[:, 0:H * D].rearrange("p (h d) -> p h d", d=D)
    t1, r_t1 = tb["t1"].next()
    t2, r_t2 = tb["t2"].next()
    t3, r_t3 = tb["t3"].next()
    st, r_st = tb["st"].next()
    cur, r_cur = xv, r_xf
    if gain is not None:
        g_t, r_g = gain
        kb.dve(lambda e: e.tensor_tensor(out=fl(t1), in0=xv, in1=xv, op=ALU.mult), r=[r_xf], w=[r_t1])
        kb.dve(lambda e: e.tensor_reduce(out=st[:, 0:H], in_=fl(t1), axis=AX.X, op=ALU.add), r=[r_t1], w=[r_st])
        yield
        kb.rsqrt_cols(st[:, 32:32 + H], r_st, st[:, 0:H], r_st, H, 1.0 / D, st[:, 16:16 + H], r_st)
        yield
        kb.dve(lambda e: e.tensor_tensor(out=fl(t1), in0=xv, in1=st[:, 32:32 + H].unsqueeze(2).broadcast_to([128, H, D]), op=ALU.mult),
               r=[r_xf, r_st], w=[r_t1])
        yield
        kb.pool(lambda e: e.tensor_tensor(out=fl(t1), in0=fl(t1), in1=g_t[:, 0:D].unsqueeze(1).broadcast_to([128, H, D]), op=ALU.mult),
                r=[r_t1, r_g], w=[r_t1])
        yield
        cur, r_cur = fl(t1), r_t1
    if cs is None:
        kb.dve(lambda e: e.tensor_copy(out=out_ap, in_=cur), r=[r_cur], w=[r_out])
        yield
        return
    cs_t, r_cs = cs
    q4 = D // 4
    cv = cur.rearrange("p h (a r i) -> p h a r i", a=2, r=2, i=q4)
    mv = fl(t2).rearrange("p h (a r i) -> p h a r i", a=2, r=2, i=q4)
    sv = cs_t[:, 1, 0:D].rearrange("p (a r i) -> p a r i", a=2, r=2, i=q4)
    for r_ in range(2):
        kb.pool(lambda e, r_=r_: e.tensor_tensor(out=mv[:, :, :, r_, :], in0=cv[:, :, :, 1 - r_, :],
                                                 in1=sv[:, :, r_, :].unsqueeze(1).broadcast_to([128, H, 2, q4]), op=ALU.mult),
                r=[r_cur, r_cs], w=[r_t2])
    kb.dve(lambda e: e.tensor_tensor(out=fl(t3), in0=cur, in1=cs_t[:, 0, 0:D].unsqueeze(1).broadcast_to([128, H, D]), op=ALU.mult),
           r=[r_cur, r_cs], w=[r_t3])
    yield
    kb.dve(lambda e: e.tensor_tensor(out=out_ap, in0=fl(t3), in1=fl(t2), op=ALU.add), r=[r_t3, r_t2], w=[r_out])
    yield


def norm_bufs(kb):
    return {"t1": kb.sbrot(2, [128, 1024], F32, "t1"), "t2": kb.sbrot(2, [128, 1024], F32, "t2"), "t3": kb.sbrot(2, [128, 1024], F32, "t3"),
            "st": kb.sbrot(4, [128, 48], F32, "qst")}


def attention_core(kb, heads, loaders, groups, dv, O_s, exp_scale):
    ptrot = kb.sbrot(3, [128, 512], BF16, "pT")
    otrot = kb.sbrot(2, [128, 4, dv], BF16, "ot")
    rdrot = kb.sbrot(2, [128, 4], F32, "rden")

    def iters():
        cur_kv = None
        accsel = 0
        kpieces = vv = None
        for (h, kvkey) in heads:
            if kvkey != cur_kv:
                kpieces, vv = loaders["kv"](kvkey)
                cur_kv = kvkey
            qpieces = loaders["q"](h)
            for (q0, nq, kts) in groups:
                banks = (1, 2) if accsel == 0 else (3, 4)
                accsel ^= 1
                for ki, kt in enumerate(kts):
                    yield dict(h=h, q0=q0, nq=nq, kt=kt, ki=ki, nkt=len(kts), kp=kpieces, qp=qpieces, vv=vv, banks=banks)

    cnt = [0]

    def emit_qk(it):
        ps, r_ps = kb.pb[5 + (cnt[0] % 2)]
        cnt[0] += 1
        it["ps"] = (ps, r_ps)
        npz = len(it["kp"])
        kt, q0, nq = it["kt"], it["q0"], it["nq"]
        for pi in range(npz):
            kt_t, r_kt, nrows, base = it["kp"][pi]
            qt_t, r_qt, _, _ = it["qp"][pi]
            kb.pe(lambda e, kt_t=kt_t, qt_t=qt_t, nrows=nrows, base=base, kt=kt, q0=q0, nq=nq, ps=ps, pi=pi, npz=npz:
                  e.matmul(ps[:, 0:nq], lhsT=kt_t[base:base + nrows, kt * 128:(kt + 1) * 128],
                           rhs=qt_t[base:base + nrows, q0:q0 + nq], start=(pi == 0), stop=(pi == npz - 1)),
                  r=[r_kt, r_qt], w=[r_ps])

    def emit_rest(it):
        ps, r_ps = it["ps"]
        vt, r_vt = it["vv"]
        kt, q0, nq, ki, nkt, banks, h = it["kt"], it["q0"], it["nq"], it["ki"], it["nkt"], it["banks"], it["h"]
        nqs = nq // 128
        pT, r_pT = ptrot.next()
        kb.act(lambda e, pT=pT, ps=ps, nq=nq: e.activation(out=pT[:, 0:nq], in_=ps[:, 0:nq], func=AF.Exp, scale=exp_scale),
               r=[r_ps], w=[r_pT])
        for qs in range(nqs):
            at, r_at = kb.pb[banks[qs // 2]]
            c0 = (qs % 2) * (dv + 1)
            kb.pe(lambda e, at=at, c0=c0, pT=pT, qs=qs, kt=kt, ki=ki, vt=vt, nkt=nkt:
                  e.matmul(at[:, c0:c0 + dv + 1], lhsT=pT[:, qs * 128:(qs + 1) * 128], rhs=vt[:, kt, :],
                           start=(ki == 0 and qs % 2 == 0), stop=(ki == nkt - 1), skip_group_check=True),
                  r=[r_pT, r_vt], w=[r_at])
        if ki != nkt - 1:
            return
        ot, r_ot = otrot.next()
        rd, r_rd = rdrot.next()
        for qs in range(nqs):
            at, r_at = kb.pb[banks[qs // 2]]
            c0 = (qs % 2) * (dv + 1)
            kb.dve(lambda e, at=at, c0=c0, rd=rd, qs=qs: e.reciprocal(out=rd[:, qs:qs + 1], in_=at[:, c0 + dv:c0 + dv + 1]),
                   r=[r_at], w=[r_rd])
            kb.act(lambda e, at=at, c0=c0, rd=rd, qs=qs, ot=ot: e.activation(out=ot[:, qs, :], in_=at[:, c0:c0 + dv], func=AF.Copy,
                                                                              scale=rd[:, qs:qs + 1]),
                   r=[r_at, r_rd], w=[r_ot])
        kb.store(O_s[q0:q0 + nq, h * dv:(h + 1) * dv].rearrange("(qs p) d -> p qs d", p=128), ot[:, 0:nqs, :], r_ot)

    prev = None
    for it in iters():
        emit_qk(it)
        if prev is not None:
            emit_rest(prev)
        prev = it
    if prev is not None:
        emit_rest(prev)


def outproj_pass(kb, O_s, wo_dram, h_src, h_dst, mod, tiles, tile_set):
    with kb.scope():
        wo, r_wo = kb.sb([128, 8, 1024], BF16, "wo")
        load_weight(kb, wo[:], r_wo, wo_dram)
        orot = kb.sbrot(3, [128, 1024], BF16, "o")
        oTrot = kb.sbrot(3, [128, 8, 128], BF16, "oT")
        hrot = kb.sbrot(3, [128, 1024], F32, "h")
        trot = kb.sbrot(3, [128, 1024], F32, "tmp")
        if isinstance(tiles, int):
            tiles = list(range(tiles))
        def _tile(tt, par):
            oi, t = tt if isinstance(tt, tuple) else (tt, tt)
            s = tile_set(t)
            o, r_o = orot.next()
            kb.load(o[:], r_o, O_s[oi * 128:(oi + 1) * 128, :])
            ht, r_h = hrot.next()
            kb.load(ht[:], r_h, h_src[t * 128:(t + 1) * 128, :])
            pt, r_pt = kb.pb[(3 + 4 * par) % 8]
            ptb = pt[:].bitcast(BF16).rearrange("p (k j) -> p k j", j=128)
            for k in range(8):
                kb.pe(lambda e, k=k, o=o, ptb=ptb: e.transpose(out=ptb[:, k, :], in_=o[:, k * 128:(k + 1) * 128], identity=kb.identb[:]),
                      r=[r_o, kb.r_identb], w=[r_pt])
            yield
            oT, r_oT = oTrot.next()
            kb.act(lambda e, oT=oT, ptb=ptb: e.activation(out=oT[:], in_=ptb, func=AF.Copy), r=[r_pt], w=[r_oT])
            yield
            tmp, r_tmp = trot.next()
            for half in range(2):
                po, r_po = kb.pb[(half + 4 * par) % 8]
                for k in range(8):
                    kb.pe(lambda e, k=k, half=half, po=po, oT=oT: e.matmul(po[:], lhsT=oT[:, k, :], rhs=wo[:, k, half * 512:(half + 1) * 512],
                                                                          start=(k == 0), stop=(k == 7)),
                          r=[r_oT, r_wo], w=[r_po])
                kb.dve(lambda e, half=half, po=po, tmp=tmp, s=s: e.tensor_tensor(out=tmp[:, half * 512:(half + 1) * 512], in0=po[:],
                                                                                 in1=mod["g"][:, s, 0, half * 512:(half + 1) * 512], op=ALU.mult),
                       r=[r_po, mod["r_g"]], w=[r_tmp])
            yield
            kb.pool(lambda e, tmp=tmp, ht=ht: e.tensor_tensor(out=tmp[:], in0=tmp[:], in1=ht[:], op=ALU.add), r=[r_tmp, r_h], w=[r_tmp])
            kb.store(h_dst[t * 128:(t + 1) * 128, :], tmp[:], r_tmp)
        interleave((_tile(tt, i % 2) for i, tt in enumerate(tiles)), 3, 'outproj')


def ffn_dense_pass(kb, h_src, h_dst, mod, w13_dram, w2_dram, FF, blocks, tile_set):
    nf = FF // 128
    with kb.scope():
        fb = _front_bufs(kb, nh=2)
        maxnt = max(len(b) for b in blocks)
        tT, r_tT = kb.sb([128, 8, maxnt * 128], BF16, "tT")
        hid, r_hid = kb.sb([128, nf, maxnt * 128], BF16, "hid")
        w13rot = kb.sbrot(2, [128, 8, 2, 256], BF16, "w13c")
        w2, r_w2 = kb.sb([128, nf, 1024], BF16, "w2")
        sarot = kb.sbrot(2, [128, 512], BF16, "sa")
        hrot = kb.sbrot(3, [128, 1024], F32, "h2")
        trot = kb.sbrot(3, [128, 1024], F32, "tmp")
        w2_loaded = False
        for blk in blocks:
            nt = len(blk)
            TB = nt * 128
            interleave((front(kb, fb, h_src[t * 128:(t + 1) * 128, :], mod, tile_set(t), 1, tT[:, :, j * 128:(j + 1) * 128], r_tT,
                              pbank=(3 + 4 * (j % 2)) % 8) for j, t in enumerate(blk)), 3, 'ffnfront')
            if not w2_loaded:
                for f0 in range(0, nf, 2):
                    kb.P.dma("pool", w2[:, f0:f0 + 2, :], w2_dram[f0 * 128:(f0 + 2) * 128, :].rearrange("(k p) n -> p k n", p=128),
                             w=[r_w2], sem=r_w2)
                w2_loaded = True
            tgs = [(g0, min(512, TB - g0)) for g0 in range(0, TB, 512)]
            for fc in range(0, nf, 2):
                wc, r_wc = w13rot.next()
                for ab in range(2):
                    kb.P.dma("pool", wc[:, :, ab, :], w13_dram[:, ab * FF + fc * 128: ab * FF + (fc + 2) * 128].rearrange("(k p) n -> p k n", p=128),
                             w=[r_wc], sem=r_wc)
                for sub in range(2):
                    f = fc + sub
                    for gi, (g0, gn) in enumerate(tgs):
                        pa, r_pa = kb.pb[(gi % 2) * 2]
                        pbk, r_pbk = kb.pb[(gi % 2) * 2 + 1]
                        for ab, (pp, r_pp) in enumerate(((pa, r_pa), (pbk, r_pbk))):
                            for k in range(8):
                                kb.pe(lambda e, pp=pp, wc=wc, k=k, ab=ab, sub=sub, g0=g0, gn=gn:
                                      e.matmul(pp[:, 0:gn], lhsT=wc[:, k, ab, sub * 128:(sub + 1) * 128], rhs=tT[:, k, g0:g0 + gn],
                                               start=(k == 0), stop=(k == 7)), r=[r_wc, r_tT], w=[r_pp])
                        sa, r_sa = sarot.next()
                        kb.act(lambda e, sa=sa, pa=pa, gn=gn: e.activation(out=sa[:, 0:gn], in_=pa[:, 0:gn], func=AF.Silu), r=[r_pa], w=[r_sa])
                        kb.dve(lambda e, sa=sa, pbk=pbk, f=f, g0=g0, gn=gn: e.tensor_tensor(out=hid[:, f, g0:g0 + gn], in0=pbk[:, 0:gn],
                                                                                             in1=sa[:, 0:gn], op=ALU.mult),
                               r=[r_pbk, r_sa], w=[r_hid])
            def _epi(j, t):
                s = tile_set(t)
                ht, r_h = hrot.next()
                kb.load(ht[:], r_h, h_src[t * 128:(t + 1) * 128, :])
                tmp, r_tmp = trot.next()
                for half in range(2):
                    po, r_po = kb.pb[4 + (j % 2) * 2 + half]
                    for f in range(nf):
                        kb.pe(lambda e, po=po, f=f, j=j, half=half: e.matmul(po[:], lhsT=hid[:, f, j * 128:(j + 1) * 128],
                                                                            rhs=w2[:, f, half * 512:(half + 1) * 512],
                                                                            start=(f == 0), stop=(f == nf - 1)),
                              r=[r_hid, r_w2], w=[r_po])
                    kb.dve(lambda e, half=half, po=po, tmp=tmp, s=s: e.tensor_tensor(out=tmp[:, half * 512:(half + 1) * 512], in0=po[:],
                                                                                     in1=mod["g"][:, s, 1, half * 512:(half + 1) * 512], op=ALU.mult),
                           r=[r_po, mod["r_g"]], w=[r_tmp])
                yield
                kb.pool(lambda e, tmp=tmp, ht=ht: e.tensor_tensor(out=tmp[:], in0=tmp[:], in1=ht[:], op=ALU.add), r=[r_tmp, r_h], w=[r_tmp])
                kb.store(h_dst[t * 128:(t + 1) * 128, :], tmp[:], r_tmp)
            interleave((_epi(j, t) for j, t in enumerate(blk)), 2, 'ffnepi')


NT_Q = 34
NT_ALL = 66


def tile_set(t):
    return 1 if t < 2 else 0


def std_groups(nq_tiles, nk_tiles):
    groups = [(0, 256, [0, 1])]
    for g0 in range(2, nq_tiles, 4):
        groups.append((g0 * 128, min(4, nq_tiles - g0) * 128, list(range(nk_tiles))))
    return groups


def layer0_mixer(kb, xcat, rope, mod, wqkv_d, qn_d, kn_d, wo_d, hm_dst, nq_tiles=NT_Q, nk_tiles=NT_ALL):
    NQ, NK = nq_tiles * 128, nk_tiles * 128
    KT_s = kb.dram("a_KT", [2, 128, NK], BF16)
    V_s = kb.dram("a_V", [2, NK, 129], BF16)
    QT_s = kb.dram("a_QT", [8, 128, NQ], BF16)
    O_s = kb.dram("a_O", [NQ, 1024], BF16)
    with kb.scope():
        wqkv, r_wqkv = kb.sb([128, 8, 1536], BF16, "wqkv")
        load_weight(kb, wqkv[:], r_wqkv, wqkv_d)
        gq, r_gq = kb.sb([128, 128], F32, "gq")
        gk, r_gk = kb.sb([128, 128], F32, "gk")
        kb.load(gq[:], r_gq, qn_d.broadcast_to([128, 128]))
        kb.load(gk[:], r_gk, kn_d.broadcast_to([128, 128]))
        kb.dve(lambda e: e.tensor_scalar(out=gq[:], in0=gq[:], scalar1=float(128 ** -0.5), scalar2=None, op0=ALU.mult), r=[r_gq], w=[r_gq])
        fb = _front_bufs(kb, nh=2)
        aTrot = kb.sbrot(4, [128, 8, 128], BF16, "aT")
        csrot = kb.sbrot(4, [128, 2, 128], F32, "cs")
        vtrot = kb.sbrot(4, [128, 2, 129], BF16, "vt")
        for vt, r_vt in vtrot.items:
            kb.pool(lambda e, vt=vt: e.memset(vt[:], 1.0), w=[r_vt])
        kfrot = kb.sbrot(4, [128, 2, 128], F32, "kf")
        qfrot = kb.sbrot(4, [128, 8, 128], F32, "qf")
        tb = norm_bufs(kb)
        kbrot = kb.sbrot(4, [128, 2, 128], BF16, "kb16")
        qbrot = kb.sbrot(4, [128, 8, 128], BF16, "qb16")
        kTrot = kb.sbrot(4, [128, 2, 128], BF16, "kT")
        qTrot = kb.sbrot(4, [128, 8, 128], BF16, "qT")
        def _tile(t, par):
            rows = slice(t * 128, (t + 1) * 128)
            aT, r_aT = aTrot.next()
            yield from front(kb, fb, xcat[rows, :], mod, tile_set(t), 0, aT[:], r_aT, pbank=(3 + 4 * par) % 8)
            cs, r_cs = csrot.next()
            kb.load(cs[:], r_cs, rope[rows, :, :])
            pkv, r_pkv = kb.pb[(0 + 4 * par) % 8]
            for k in range(8):
                kb.pe(lambda e, k=k, aT=aT: e.matmul(pkv[:], lhsT=aT[:, k, :], rhs=wqkv[:, k, 1024:1536], start=(k == 0), stop=(k == 7)),
                      r=[r_aT, r_wqkv], w=[r_pkv])
            yield
            vt, r_vt = vtrot.next()
            kb.act(lambda e, vt=vt: e.activation(out=vt[:, :, 0:128], in_=pkv[:, 256:512].rearrange("p (h d) -> p h d", d=128), func=AF.Copy),
                   r=[r_pkv], w=[r_vt])
            kb.store(V_s[:, rows, :].rearrange("h p d -> p h d"), vt[:], r_vt)
            kf, r_kf = kfrot.next()
            kb.act(lambda e, kf=kf: e.activation(out=kf[:], in_=pkv[:, 0:256].rearrange("p (h d) -> p h d", d=128), func=AF.Copy),
                   r=[r_pkv], w=[r_kf])
            k16, r_k16 = kbrot.next()
            yield
            yield from qk_norm_rope(kb, kf[:], r_kf, 2, 128, (gk, r_gk), (cs, r_cs), k16[:], r_k16, tb)
            pt, r_pt = kb.pb[(3 + 4 * par) % 8]
            ptb = pt[:].bitcast(BF16).rearrange("p (k j) -> p k j", j=128)
            for hh in range(2):
                kb.pe(lambda e, hh=hh, k16=k16, ptb=ptb: e.transpose(out=ptb[:, hh, :], in_=k16[:, hh, :], identity=kb.identb[:]),
                      r=[r_k16, kb.r_identb], w=[r_pt])
            yield
            kT, r_kT = kTrot.next()
            kb.act(lambda e, kT=kT, ptb=ptb: e.activation(out=kT[:], in_=ptb[:, 0:2, :], func=AF.Copy), r=[r_pt], w=[r_kT])
            kb.store(KT_s[:, :, rows].rearrange("h d t -> d h t"), kT[:], r_kT)
            yield
            if t < nq_tiles:
                qf, r_qf = qfrot.next()
                for half in range(2):
                    pq, r_pq = kb.pb[(1 + half + 4 * par) % 8]
                    for k in range(8):
                        kb.pe(lambda e, k=k, aT=aT, pq=pq, half=half: e.matmul(pq[:], lhsT=aT[:, k, :], rhs=wqkv[:, k, half * 512:(half + 1) * 512],
                                                                              start=(k == 0), stop=(k == 7)),
                              r=[r_aT, r_wqkv], w=[r_pq])
                    kb.act(lambda e, qf=qf, pq=pq, half=half: e.activation(out=qf[:, half * 4:(half + 1) * 4, :],
                                                                           in_=pq[:].rearrange("p (h d) -> p h d", d=128), func=AF.Copy),
                           r=[r_pq], w=[r_qf])
                q16, r_q16 = qbrot.next()
                yield
                yield from qk_norm_rope(kb, qf[:], r_qf, 8, 128, (gq, r_gq), (cs, r_cs), q16[:], r_q16, tb)
                pt2, r_pt2 = kb.pb[(0 + 4 * par) % 8]
                ptb2 = pt2[:].bitcast(BF16).rearrange("p (k j) -> p k j", j=128)
                for hh in range(8):
                    kb.pe(lambda e, hh=hh, q16=q16, ptb2=ptb2: e.transpose(out=ptb2[:, hh, :], in_=q16[:, hh, :], identity=kb.identb[:]),
                          r=[r_q16, kb.r_identb], w=[r_pt2])
                yield
                qT, r_qT = qTrot.next()
                kb.act(lambda e, qT=qT, ptb2=ptb2: e.activation(out=qT[:], in_=ptb2, func=AF.Copy), r=[r_pt2], w=[r_qT])
                kb.store(QT_s[:, :, rows].rearrange("h d t -> d h t"), qT[:], r_qT)
        interleave((_tile(t, i % 2) for i, t in enumerate(range(nk_tiles))), 3, 'l0qkv')
    with kb.scope():
        ktrot = kb.sbrot(2, [128, NK], BF16, "KT")
        vrot = kb.sbrot(2, [128, nk_tiles, 129], BF16, "V")
        qrot = kb.sbrot(2, [128, NQ], BF16, "QT")

        def load_kv(kvh):
            kt_t, r_kt = ktrot.next()
            kb.load(kt_t[:], r_kt, KT_s[kvh])
            v_t, r_v = vrot.next()
            kb.load(v_t[:], r_v, V_s[kvh].rearrange("(kt p) d -> p kt d", p=128))
            return [(kt_t, r_kt, 128, 0)], (v_t, r_v)

        def load_q(h):
            q_t, r_q = qrot.next()
            kb.load(q_t[:], r_q, QT_s[h])
            return [(q_t, r_q, 128, 0)]

        heads = [(h, h // 4) for h in range(8)]
        attention_core(kb, heads, {"kv": load_kv, "q": load_q}, std_groups(nq_tiles, nk_tiles), 128, O_s, 1.0)
    outproj_pass(kb, O_s, wo_d, xcat, hm_dst, mod, nq_tiles, tile_set)


def ffn_blocks(nq_tiles):
    blocks = [[0, 1]]
    for b0 in range(2, nq_tiles, 8):
        blocks.append(list(range(b0, min(nq_tiles, b0 + 8))))
    return blocks


def build_A():
    nc = bass.Bass("TRN2", target_bir_lowering=False)
    d = lambda name, shape: nc.dram_tensor(name, list(shape), F32, kind="ExternalInput").ap()
    xcat = d("xcat", [NT_ALL * 128, 1024])
    rope = d("rope128", [NT_ALL * 128, 2, 128])
    c = d("c", [1, 1024]); cctx = d("cctx", [1, 1024])
    ada_w = d("ada_w", [1024, 6144]); ada_b = d("ada_b", [1, 6144])
    gmix = d("gmix", [1, 1024]); gffn = d("gffn", [1, 1024])
    wqkv = d("wqkv", [1024, 1536]); qn = d("qn", [1, 128]); kn = d("kn", [1, 128]); wo = d("wo", [1024, 1024])
    w13 = d("w13", [1024, 5632]); w2 = d("w2", [2816, 1024]); ident = d("ident", [128, 128])
    hout = nc.dram_tensor("hout", [NT_Q * 128, 1024], F32, kind="ExternalOutput").ap()
    kb = KB(nc)
    with kb.gst:
        kb.setup_globals(ident)
        mod = kb.modulation(c, cctx, ada_w, ada_b, gmix, gffn)
        hm = kb.dram("hm0", [NT_Q * 128, 1024], F32)
        layer0_mixer(kb, xcat, rope, mod, wqkv, qn, kn, wo, hm)
        ffn_dense_pass(kb, hm, hout, mod, w13, w2, 2816, ffn_blocks(NT_Q), tile_set)
        kb.P.emit(nc)
    return nc


def rope_table(tokens, dim):
    tokens = np.asarray(tokens)
    q = dim // 4
    inv = (10000.0 ** (-np.arange(q, dtype=np.float32) / np.float32(q))).astype(np.float32)
    r = (np.maximum(tokens, 0) // 64).astype(np.float32)[:, None] * inv
    c = (np.maximum(tokens, 0) % 64).astype(np.float32)[:, None] * inv
    ang = np.concatenate([r, r, c, c], axis=-1).astype(np.float32)
    cos = np.cos(ang).astype(np.float32)
    sin = np.sin(ang).astype(np.float32)
    sgn = np.concatenate([-np.ones(q), np.ones(q), -np.ones(q), np.ones(q)]).astype(np.float32)
    out = np.stack([cos, sin * sgn], axis=1)
    nopos = tokens < 0
    out[nopos, 0, :] = 1.0
    out[nopos, 1, :] = 0.0
    return np.ascontiguousarray(out.astype(np.float32))


def core_tokens(hf):
    own = np.arange(hf * 4096, (hf + 1) * 4096)
    oth = np.arange((1 - hf) * 4096, (2 - hf) * 4096)
    return own, oth


_NC_CACHE = {}


def run_A(inp):
    if "A" not in _NC_CACHE:
        _NC_CACHE["A"] = build_A()
    nc = _NC_CACHE["A"]
    f = lambda a: np.ascontiguousarray(np.asarray(a, dtype=np.float32))
    maps = []
    for core in range(8):
        b, hf = core // 2, core % 2
        own, oth = core_tokens(hf)
        toks = np.concatenate([-np.ones(256, dtype=np.int64), own, oth])
        xcat = np.concatenate([inp["ctx"][b], inp["x"][b][own], inp["x"][b][oth]], axis=0)
        maps.append({
            "xcat": f(xcat), "rope128": rope_table(toks, 128),
            "c": f(inp["c"][b:b + 1]), "cctx": f(inp["c_ctx"][None, :]),
            "ada_w": f(inp["ada_w"][0]), "ada_b": f(inp["ada_b"][0:1]),
            "gmix": f(inp["norm_mix"][0:1]), "gffn": f(inp["norm_ffn"][0:1]),
            "wqkv": f(inp["a_wqkv"][0]), "qn": f(inp["a_q_norm"][0:1]), "kn": f(inp["a_k_norm"][0:1]), "wo": f(inp["a_wo"][0]),
            "w13": f(inp["ffn_w13"][0]), "w2": f(inp["ffn_w2"][0]), "ident": np.eye(128, dtype=np.float32),
        })
    res = run_bass_kernel_spmd(nc, maps, core_ids=list(range(8)))
    h1 = np.zeros((4, 8192, 1024), np.float32)
    hc1 = np.zeros((4, 256, 1024), np.float32)
    for core in range(8):
        b, hf = core // 2, core % 2
        o = res.results[core]["hout"]
        hc1[b] = o[0:256]
        h1[b, hf * 4096:(hf + 1) * 4096] = o[256:]
    return h1, hc1


def layer1_mixer(kb, hcat, rope64, mod, wdown_d, qln_d, kvln_d, wuq_d, wukv_d, wo_d, hm_dst, nq_tiles=NT_Q, nk_tiles=NT_ALL, qtiles=None):
    if qtiles is None:
        qtiles = list(range(nq_tiles))
    nq_tiles = len(qtiles)
    qpos = {t: i for i, t in enumerate(qtiles)}
    NQ, NK = nq_tiles * 128, nk_tiles * 128
    KT_s = kb.dram("b_KT", [8, 128, NK], BF16)
    KR_s = kb.dram("b_KR", [128, NK], BF16)
    V_s = kb.dram("b_V", [8, NK, 129], BF16)
    QT_s = kb.dram("b_QT", [8, 128, NQ], BF16)
    QR_s = kb.dram("b_QR", [4, 128, NQ], BF16)
    O_s = kb.dram("b_O", [NQ, 1024], BF16)
    with kb.scope():
        wdown, r_wdown = kb.sb([128, 8, 704], BF16, "wdown")
        load_weight(kb, wdown[:], r_wdown, wdown_d)
        wuq, r_wuq = kb.sb([128, 3, 1536], BF16, "wuq")
        load_weight(kb, wuq[:], r_wuq, wuq_d)
        wukv, r_wukv = kb.sb([128, 2, 2048], BF16, "wukv")
        load_weight(kb, wukv[:], r_wukv, wukv_d)
        gql, r_gql = kb.sb([128, 384], F32, "gql")
        gkv, r_gkv = kb.sb([128, 256], F32, "gkv")
        kb.load(gql[:], r_gql, qln_d.broadcast_to([128, 384]))
        kb.load(gkv[:], r_gkv, kvln_d.broadcast_to([128, 256]))
        fb = _front_bufs(kb, nh=2)
        tb = norm_bufs(kb)
        aTrot = kb.sbrot(4, [128, 8, 128], BF16, "aT")
        csrot = kb.sbrot(4, [128, 2, 64], F32, "cs")
        ckfrot = kb.sbrot(4, [128, 320], F32, "ckf")
        cknrot = kb.sbrot(4, [128, 256], BF16, "ckn")
        cknTrot = kb.sbrot(4, [128, 2, 128], BF16, "cknT")
        krrot = kb.sbrot(4, [128, 2, 64], BF16, "kr")
        krTrot = kb.sbrot(4, [128, 128], BF16, "krT")
        kTrot = kb.sbrot(4, [128, 8, 128], BF16, "kT")
        vtrot = kb.sbrot(4, [128, 8, 129], BF16, "vt")
        for vt, r_vt in vtrot.items:
            kb.pool(lambda e, vt=vt: e.memset(vt[:], 1.0), w=[r_vt])
        dqfrot = kb.sbrot(4, [128, 384], F32, "dqf")
        dqnrot = kb.sbrot(4, [128, 384], BF16, "dqn")
        dqnTrot = kb.sbrot(4, [128, 3, 128], BF16, "dqnT")
        qTrot = kb.sbrot(4, [128, 8, 128], BF16, "qT")
        qrfrot = kb.sbrot(4, [128, 8, 64], F32, "qrf")
        qr16rot = kb.sbrot(4, [128, 8, 64], BF16, "qr16")
        qrTrot = kb.sbrot(4, [128, 4, 128], BF16, "qrT")
        wukv_v = wukv[:].rearrange("p k (h x) -> p k h x", x=256)
        wuq_v = wuq[:].rearrange("p k (h x) -> p k h x", x=192)

        def bfview(bank):
            pt, r_pt = kb.pb[bank]
            return pt[:].bitcast(BF16).rearrange("p (k j) -> p k j", j=128), r_pt

        def _tile(t, par):
            rows = slice(t * 128, (t + 1) * 128)
            aT, r_aT = aTrot.next()
            yield from front(kb, fb, hcat[rows, :], mod, tile_set(t), 0, aT[:], r_aT, pbank=(3 + 4 * par) % 8)
            cs, r_cs = csrot.next()
            kb.load(cs[:], r_cs, rope64[rows, :, :])
            pkv, r_pkv = kb.pb[(0 + 4 * par) % 8]
            for k in range(8):
                kb.pe(lambda e, k=k, aT=aT: e.matmul(pkv[:, 0:320], lhsT=aT[:, k, :], rhs=wdown[:, k, 384:704], start=(k == 0), stop=(k == 7)),
                      r=[r_aT, r_wdown], w=[r_pkv])
            yield
            ckf, r_ckf = ckfrot.next()
            kb.act(lambda e, ckf=ckf: e.activation(out=ckf[:], in_=pkv[:, 0:320], func=AF.Copy), r=[r_pkv], w=[r_ckf])
            ckn, r_ckn = cknrot.next()
            yield from qk_norm_rope(kb, ckf[:, 0:256].unsqueeze(1), r_ckf, 1, 256, (gkv, r_gkv), None, ckn[:].unsqueeze(1), r_ckn, tb)
            yield
            ptb, r_pt = bfview((3 + 4 * par) % 8)
            for kc in range(2):
                kb.pe(lambda e, kc=kc, ckn=ckn, ptb=ptb: e.transpose(out=ptb[:, kc, :], in_=ckn[:, kc * 128:(kc + 1) * 128], identity=kb.identb[:]),
                      r=[r_ckn, kb.r_identb], w=[r_pt])
            yield
            cknT, r_cknT = cknTrot.next()
            kb.act(lambda e, cknT=cknT, ptb=ptb: e.activation(out=cknT[:], in_=ptb[:, 0:2, :], func=AF.Copy), r=[r_pt], w=[r_cknT])
            kr, r_kr = krrot.next()
            yield from qk_norm_rope(kb, ckf[:, 256:320].unsqueeze(1), r_ckf, 1, 64, None, (cs, r_cs), kr[:, 0:1, :], r_kr, tb)
            kb.dve(lambda e, kr=kr: e.tensor_copy(out=kr[:, 1, :], in_=kr[:, 0, :]), r=[r_kr], w=[r_kr])
            yield
            ptb, r_pt = bfview((3 + 4 * par) % 8)
            kb.pe(lambda e, kr=kr, ptb=ptb: e.transpose(out=ptb[:, 2, :], in_=kr[:].rearrange("p a d -> p (a d)"), identity=kb.identb[:]),
                  r=[r_kr, kb.r_identb], w=[r_pt])
            yield
            krT, r_krT = krTrot.next()
            kb.act(lambda e, krT=krT, ptb=ptb: e.activation(out=krT[:], in_=ptb[:, 2, :], func=AF.Copy), r=[r_pt], w=[r_krT])
            kb.store(KR_s[:, rows], krT[:], r_krT)
            yield
            kT, r_kT = kTrot.next()
            for hg in range(2):
                pk, r_pk = kb.pb[(1 + hg + 4 * par) % 8]
                for hh in range(4):
                    h = hg * 4 + hh
                    for kc in range(2):
                        kb.pe(lambda e, pk=pk, hh=hh, h=h, kc=kc, cknT=cknT: e.matmul(pk[:, hh * 128:(hh + 1) * 128], lhsT=wukv_v[:, kc, h, 0:128],
                                                                                     rhs=cknT[:, kc, :], start=(kc == 0 and hh == 0), stop=(kc == 1),
                                                                                     skip_group_check=True),
                              r=[r_wukv, r_cknT], w=[r_pk])
                kb.act(lambda e, pk=pk, hg=hg, kT=kT: e.activation(out=kT[:, hg * 4:(hg + 1) * 4, :], in_=pk[:].rearrange("p (h t) -> p h t", t=128),
                                                                  func=AF.Copy), r=[r_pk], w=[r_kT])
            kb.store(KT_s[:, :, rows].rearrange("h d t -> d h t"), kT[:], r_kT)
            yield
            vt, r_vt = vtrot.next()
            for hg in range(2):
                pv, r_pv = kb.pb[(1 + hg + 4 * par) % 8]
                for kc in range(2):
                    kb.pe(lambda e, pv=pv, hg=hg, kc=kc, cknT=cknT: e.matmul(pv[:].rearrange("p (h d) -> p h d", d=128), lhsT=cknT[:, kc, :],
                                                                           rhs=wukv_v[:, kc, hg * 4:(hg + 1) * 4, 128:256],
                                                                           start=(kc == 0), stop=(kc == 1)),
                          r=[r_wukv, r_cknT], w=[r_pv])
                kb.dve(lambda e, pv=pv, hg=hg, vt=vt: e.tensor_copy(out=vt[:, hg * 4:(hg + 1) * 4, 0:128], in_=pv[:].rearrange("p (h d) -> p h d", d=128)),
                       r=[r_pv], w=[r_vt])
            kb.store(V_s[:, rows, :].rearrange("h p d -> p h d"), vt[:], r_vt)
            if t not in qpos:
                return
            qrows = slice(qpos[t] * 128, (qpos[t] + 1) * 128)
            yield
            pdq, r_pdq = kb.pb[(0 + 4 * par) % 8]
            for k in range(8):
                kb.pe(lambda e, k=k, aT=aT: e.matmul(pdq[:, 0:384], lhsT=aT[:, k, :], rhs=wdown[:, k, 0:384], start=(k == 0), stop=(k == 7)),
                      r=[r_aT, r_wdown], w=[r_pdq])
            yield
            dqf, r_dqf = dqfrot.next()
            kb.act(lambda e, dqf=dqf: e.activation(out=dqf[:], in_=pdq[:, 0:384], func=AF.Copy), r=[r_pdq], w=[r_dqf])
            dqn, r_dqn = dqnrot.next()
            yield from qk_norm_rope(kb, dqf[:].unsqueeze(1), r_dqf, 1, 384, (gql, r_gql), None, dqn[:].unsqueeze(1), r_dqn, tb)
            yield
            ptb, r_pt = bfview((3 + 4 * par) % 8)
            for kc in range(3):
                kb.pe(lambda e, kc=kc, dqn=dqn, ptb=ptb: e.transpose(out=ptb[:, 3 + kc, :], in_=dqn[:, kc * 128:(kc + 1) * 128], identity=kb.identb[:]),
                      r=[r_dqn, kb.r_identb], w=[r_pt])
            yield
            dqnT, r_dqnT = dqnTrot.next()
            kb.act(lambda e, dqnT=dqnT, ptb=ptb: e.activation(out=dqnT[:], in_=ptb[:, 3:6, :], func=AF.Copy), r=[r_pt], w=[r_dqnT])
            yield
            qT, r_qT = qTrot.next()
            for hg in range(2):
                pk, r_pk = kb.pb[(1 + hg + 4 * par) % 8]
                for hh in range(4):
                    h = hg * 4 + hh
                    for kc in range(3):
                        kb.pe(lambda e, pk=pk, hh=hh, h=h, kc=kc, dqnT=dqnT: e.matmul(pk[:, hh * 128:(hh + 1) * 128], lhsT=wuq_v[:, kc, h, 0:128],
                                                                                     rhs=dqnT[:, kc, :], start=(kc == 0 and hh == 0), stop=(kc == 2),
                                                                                     skip_group_check=True),
                              r=[r_wuq, r_dqnT], w=[r_pk])
                kb.act(lambda e, pk=pk, hg=hg, qT=qT: e.activation(out=qT[:, hg * 4:(hg + 1) * 4, :], in_=pk[:].rearrange("p (h t) -> p h t", t=128),
                                                                  func=AF.Copy), r=[r_pk], w=[r_qT])
            kb.store(QT_s[:, :, qrows].rearrange("h d t -> d h t"), qT[:], r_qT)
            yield
            pqr, r_pqr = kb.pb[(0 + 4 * par) % 8]
            for kc in range(3):
                kb.pe(lambda e, kc=kc, dqnT=dqnT: e.matmul(pqr[:].rearrange("p (h d) -> p h d", d=64), lhsT=dqnT[:, kc, :],
                                                           rhs=wuq_v[:, kc, :, 128:192], start=(kc == 0), stop=(kc == 2)),
                      r=[r_wuq, r_dqnT], w=[r_pqr])
            yield
            qrf, r_qrf = qrfrot.next()
            kb.act(lambda e, qrf=qrf: e.activation(out=qrf[:], in_=pqr[:].rearrange("p (h d) -> p h d", d=64), func=AF.Copy), r=[r_pqr], w=[r_qrf])
            qr16, r_qr16 = qr16rot.next()
            yield from qk_norm_rope(kb, qrf[:], r_qrf, 8, 64, None, (cs, r_cs), qr16[:], r_qr16, tb)
            yield
            ptb5, r_pt5 = bfview((3 + 4 * par) % 8)
            for pr in range(4):
                kb.pe(lambda e, pr=pr, qr16=qr16, ptb5=ptb5: e.transpose(out=ptb5[:, pr, :], in_=qr16[:, 2 * pr:2 * pr + 2, :].rearrange("p a d -> p (a d)"),
                                                                       identity=kb.identb[:]), r=[r_qr16, kb.r_identb], w=[r_pt5])
            yield
            qrT, r_qrT = qrTrot.next()
            kb.act(lambda e, qrT=qrT, ptb5=ptb5: e.activation(out=qrT[:], in_=ptb5[:, 0:4, :], func=AF.Copy), r=[r_pt5], w=[r_qrT])
            kb.store(QR_s[:, :, qrows].rearrange("h d t -> d h t"), qrT[:], r_qrT)
        interleave((_tile(t, i % 2) for i, t in enumerate(range(nk_tiles))), 3, 'l1qkv')
    with kb.scope():
        krt, r_krt = kb.sb([128, NK], BF16, "KR")
        kb.load(krt[:], r_krt, KR_s)
        ktrot = kb.sbrot(2, [128, NK], BF16, "KT")
        vrot = kb.sbrot(2, [128, nk_tiles, 129], BF16, "V")
        qrot = kb.sbrot(2, [128, NQ], BF16, "QT")
        qrrot = kb.sbrot(2, [128, NQ], BF16, "QR")
        state = {}

        def load_kv(h):
            kt_t, r_kt = ktrot.next()
            kb.load(kt_t[:], r_kt, KT_s[h])
            v_t, r_v = vrot.next()
            kb.load(v_t[:], r_v, V_s[h].rearrange("(kt p) d -> p kt d", p=128))
            return [(kt_t, r_kt, 128, 0), (krt, r_krt, 64, (h % 2) * 64)], (v_t, r_v)

        def load_q(h):
            q_t, r_q = qrot.next()
            kb.load(q_t[:], r_q, QT_s[h])
            if h % 2 == 0:
                state["qr"] = qrrot.next()
                kb.load(state["qr"][0][:], state["qr"][1], QR_s[h // 2])
            qr_t, r_qr = state["qr"]
            return [(q_t, r_q, 128, 0), (qr_t, r_qr, 64, (h % 2) * 64)]

        heads = [(h, h) for h in range(8)]
        attention_core(kb, heads, {"kv": load_kv, "q": load_q}, std_groups(nq_tiles, nk_tiles), 128, O_s, float(192 ** -0.5))
    outproj_pass(kb, O_s, wo_d, hcat, hm_dst, mod, [(i, t) for i, t in enumerate(qtiles)], tile_set)


def moe_pass(kb, h_src, h_dst, mod, router_d, w13_d, w2_d, blocks, tile_set, n_exp=8, FF=3584, final=None, dst_row0=0, dbg=None, exp_loop=8):
    for blk in blocks:
        _moe_block(kb, blk, h_src, h_dst, mod, router_d, w13_d, w2_d, tile_set, n_exp, FF, final, dst_row0, dbg, exp_loop)


def _moe_block(kb, blk, h_src, h_dst, mod, router_d, w13_d, w2_d, tile_set, n_exp, FF, final, dst_row0, dbg, exp_loop):
    UF = FF // 2
    nfu = UF // 128
    if True:
        nt = len(blk)
        TB = nt * 128
        s = tile_set(blk[0])
        assert all(tile_set(t) == s for t in blk)
        with kb.scope():
            tT, r_tT = kb.sb([128, 8, TB], BF16, "tT")
            acc, r_acc = kb.sb([128, nt, 1024], F32, "acc")
            gates, r_gates = kb.sb([128, nt, 8], F32, "gates")
            with kb.scope():
                fb = _front_bufs(kb, nh=2)
                rcol, r_rcol = kb.sb([128, 8, 8], F32, "rcol")
                kb.load(rcol[:], r_rcol, router_d.rearrange("(k p) e -> p k e", p=128))
                Rbc, r_Rbc = kb.sb([128, 8, 1024], F32, "Rbc")
                dex, r_dex = kb.sb([128, 8, 128], F32, "dex")
                constc, r_constc = kb.sb([128, 8], F32, "constc")
                jf, r_jf = kb.sb([128, 1024], F32, "junkf")
                for e_ in range(n_exp):
                    kb.dve(lambda e, e_=e_: e.tensor_tensor(out=dex[:], in0=kb.identf[:].unsqueeze(1).broadcast_to([128, 8, 128]),
                                                            in1=rcol[:, :, e_:e_ + 1].broadcast_to([128, 8, 128]), op=ALU.mult),
                           r=[kb.r_identf, r_rcol], w=[r_dex])
                    for half in range(2):
                        pr, r_pr = kb.pb[half]
                        kb.pe(lambda e, half=half, pr=pr: e.matmul(pr[:], lhsT=kb.onesf[:], rhs=dex[:, half * 4:(half + 1) * 4, :],
                                                                   start=True, stop=True), r=[kb.r_onesf, r_dex], w=[r_pr])
                        kb.act(lambda e, half=half, pr=pr, e_=e_: e.activation(out=Rbc[:, e_, half * 512:(half + 1) * 512], in_=pr[:], func=AF.Copy),
                               r=[r_pr], w=[r_Rbc])
                    kb.dve(lambda e, e_=e_: e.scalar_tensor_tensor(out=jf[:], in0=Rbc[:, e_, :], scalar=1.0, in1=mod["ab2"][:, s, 1, :],
                                                                    op0=ALU.mult, op1=ALU.mult, accum_out=constc[:, e_:e_ + 1]),
                           r=[r_Rbc, mod["r_ab2"]], w=[r_jf, r_constc])
                    kb.dve(lambda e, e_=e_: e.tensor_tensor(out=Rbc[:, e_, :], in0=Rbc[:, e_, :], in1=mod["ab2"][:, s, 0, :], op=ALU.mult),
                           r=[r_Rbc, mod["r_ab2"]], w=[r_Rbc])
                lgrot = kb.sbrot(4, [128, 64], F32, "lg")
                def _rt(j, t):
                    ht, r_h, stt, r_st = yield from front(kb, fb, h_src[t * 128:(t + 1) * 128, :], mod, s, 1, tT[:, :, j * 128:(j + 1) * 128], r_tT,
                                                          pbank=(3 + 4 * (j % 2)) % 8)
                    lg, r_lg = lgrot.next()
                    for e_ in range(n_exp):
                        kb.dve(lambda e, e_=e_, ht=ht, stt=stt, lg=lg: e.scalar_tensor_tensor(out=jf[:], in0=ht[:], scalar=stt[:, 2:3], in1=Rbc[:, e_, :],
                                                                                               op0=ALU.mult, op1=ALU.mult, accum_out=lg[:, e_:e_ + 1]),
                               r=[r_h, r_st, r_Rbc], w=[r_jf, r_lg])
                    yield
                    kb.dve(lambda e, lg=lg: e.tensor_tensor(out=lg[:, 0:8], in0=lg[:, 0:8], in1=constc[:], op=ALU.add), r=[r_lg, r_constc], w=[r_lg])
                    kb.dve(lambda e, lg=lg: e.tensor_reduce(out=lg[:, 8:9], in_=lg[:, 0:8], axis=AX.X, op=ALU.max), r=[r_lg], w=[r_lg])
                    kb.dve(lambda e, lg=lg: e.tensor_scalar(out=lg[:, 16:24], in0=lg[:, 0:8], scalar1=lg[:, 8:9], scalar2=None, op0=ALU.is_equal),
                           r=[r_lg], w=[r_lg])
                    kb.dve(lambda e, lg=lg: e.scalar_tensor_tensor(out=lg[:, 24:32], in0=lg[:, 16:24], scalar=-1e30, in1=lg[:, 0:8],
                                                                   op0=ALU.mult, op1=ALU.add), r=[r_lg], w=[r_lg])
                    kb.dve(lambda e, lg=lg: e.tensor_reduce(out=lg[:, 9:10], in_=lg[:, 24:32], axis=AX.X, op=ALU.max), r=[r_lg], w=[r_lg])
                    kb.dve(lambda e, lg=lg: e.tensor_scalar(out=lg[:, 32:40], in0=lg[:, 24:32], scalar1=lg[:, 9:10], scalar2=None, op0=ALU.is_equal),
                           r=[r_lg], w=[r_lg])
                    kb.dve(lambda e, lg=lg: e.tensor_tensor(out=lg[:, 10:11], in0=lg[:, 9:10], in1=lg[:, 8:9], op=ALU.subtract), r=[r_lg], w=[r_lg])
                    yield
                    kb.act(lambda e, lg=lg: e.activation(out=lg[:, 11:12], in_=lg[:, 10:11], func=AF.Exp), r=[r_lg], w=[r_lg])
                    yield
                    kb.dve(lambda e, lg=lg: e.tensor_scalar(out=lg[:, 12:13], in0=lg[:, 11:12], scalar1=1.0, scalar2=None, op0=ALU.add), r=[r_lg], w=[r_lg])
                    kb.dve(lambda e, lg=lg: e.reciprocal(out=lg[:, 13:14], in_=lg[:, 12:13]), r=[r_lg], w=[r_lg])
                    kb.dve(lambda e, lg=lg: e.tensor_tensor(out=lg[:, 14:15], in0=lg[:, 11:12], in1=lg[:, 13:14], op=ALU.mult), r=[r_lg], w=[r_lg])
                    kb.dve(lambda e, lg=lg: e.tensor_scalar(out=lg[:, 40:48], in0=lg[:, 16:24], scalar1=lg[:, 13:14], scalar2=None, op0=ALU.mult),
                           r=[r_lg], w=[r_lg])
                    kb.dve(lambda e, lg=lg, j=j: e.scalar_tensor_tensor(out=gates[:, j, :], in0=lg[:, 32:40], scalar=lg[:, 14:15], in1=lg[:, 40:48],
                                                                        op0=ALU.mult, op1=ALU.add), r=[r_lg], w=[r_gates])
                    if dbg is not None:
                        kb.store(dbg[t * 128:(t + 1) * 128, 0:64], lg[:], r_lg)
                        kb.store(dbg[t * 128:(t + 1) * 128, 64:72], gates[:, j, :], r_gates)
                interleave((_rt(j, t) for j, t in enumerate(blk)), 3, 'moert')
            with kb.scope():
                hid, r_hid = kb.sb([128, nfu, TB], BF16, "hid")
                w13rot = kb.sbrot(2, [128, 8, 2, 256], BF16, "w13c")
                w2t, r_w2t = kb.sb([128, nfu, 1024], BF16, "w2")
                sarot = kb.sbrot(2, [128, 512], BF16, "sa")
                hrot = kb.sbrot(3, [128, 1024], F32, "h2")
                fstrot = kb.sbrot(3, [128, 4], F32, "fst")
                tgs = [(g0, min(512, TB - g0)) for g0 in range(0, TB, 512)]
                first = True
                for e_ in range(exp_loop):
                    for uh in range(2):
                        base = uh * UF
                        for fc in range(0, nfu, 2):
                            wc, r_wc = w13rot.next()
                            for ab in range(2):
                                c0 = ab * FF + base + fc * 128
                                kb.P.dma("pool", wc[:, :, ab, :], w13_d[e_, :, c0:c0 + 256].rearrange("(k p) n -> p k n", p=128), w=[r_wc], sem=r_wc)
                            for sub in range(2):
                                f = fc + sub
                                for gi, (g0, gn) in enumerate(tgs):
                                    pa, r_pa = kb.pb[(gi % 2) * 2]
                                    pbk, r_pbk = kb.pb[(gi % 2) * 2 + 1]
                                    for ab, (pp, r_pp) in enumerate(((pa, r_pa), (pbk, r_pbk))):
                                        for k in range(8):
                                            kb.pe(lambda e, pp=pp, wc=wc, k=k, ab=ab, sub=sub, g0=g0, gn=gn:
                                                  e.matmul(pp[:, 0:gn], lhsT=wc[:, k, ab, sub * 128:(sub + 1) * 128], rhs=tT[:, k, g0:g0 + gn],
                                                           start=(k == 0), stop=(k == 7)), r=[r_wc, r_tT], w=[r_pp])
                                    sa, r_sa = sarot.next()
                                    kb.act(lambda e, sa=sa, pa=pa, gn=gn: e.activation(out=sa[:, 0:gn], in_=pa[:, 0:gn], func=AF.Silu), r=[r_pa], w=[r_sa])
                                    kb.dve(lambda e, sa=sa, pbk=pbk, f=f, g0=g0, gn=gn: e.tensor_tensor(out=hid[:, f, g0:g0 + gn], in0=pbk[:, 0:gn],
                                                                                                         in1=sa[:, 0:gn], op=ALU.mult),
                                           r=[r_pbk, r_sa], w=[r_hid])
                        for f0 in range(0, nfu, 2):
                            kb.P.dma("pool", w2t[:, f0:f0 + 2, :], w2_d[e_, base + f0 * 128:base + (f0 + 2) * 128, :].rearrange("(k p) n -> p k n", p=128),
                                     w=[r_w2t], sem=r_w2t)
                        for j in range(nt):
                            for half in range(2):
                                po, r_po = kb.pb[4 + (j % 2) * 2 + half]
                                for f in range(nfu):
                                    kb.pe(lambda e, po=po, f=f, j=j, half=half: e.matmul(po[:], lhsT=hid[:, f, j * 128:(j + 1) * 128],
                                                                                        rhs=w2t[:, f, half * 512:(half + 1) * 512],
                                                                                        start=(f == 0), stop=(f == nfu - 1)),
                                          r=[r_hid, r_w2t], w=[r_po])
                                if first:
                                    kb.dve(lambda e, po=po, j=j, half=half, e_=e_: e.tensor_scalar(out=acc[:, j, half * 512:(half + 1) * 512], in0=po[:],
                                                                                                   scalar1=gates[:, j, e_:e_ + 1], scalar2=None, op0=ALU.mult),
                                           r=[r_po, r_gates], w=[r_acc])
                                else:
                                    kb.dve(lambda e, po=po, j=j, half=half, e_=e_: e.scalar_tensor_tensor(out=acc[:, j, half * 512:(half + 1) * 512], in0=po[:],
                                                                                                          scalar=gates[:, j, e_:e_ + 1],
                                                                                                          in1=acc[:, j, half * 512:(half + 1) * 512],
                                                                                                          op0=ALU.mult, op1=ALU.add),
                                           r=[r_po, r_gates, r_acc], w=[r_acc])
                        first = False
                def _epi(j, t):
                    ht, r_h = hrot.next()
                    kb.load(ht[:], r_h, h_src[t * 128:(t + 1) * 128, :])
                    kb.dve(lambda e, j=j: e.tensor_tensor(out=acc[:, j, :], in0=acc[:, j, :], in1=mod["g"][:, s, 1, :], op=ALU.mult),
                           r=[r_acc, mod["r_g"]], w=[r_acc])
                    yield
                    kb.pool(lambda e, j=j, ht=ht: e.tensor_tensor(out=ht[:], in0=ht[:], in1=acc[:, j, :], op=ALU.add), r=[r_acc, r_h], w=[r_h])
                    yield
                    if final is not None:
                        gfin, r_gfin = final
                        fst, r_fst = fstrot.next()
                        kb.dve(lambda e, j=j, ht=ht, fst=fst: e.scalar_tensor_tensor(out=acc[:, j, :], in0=ht[:], scalar=1.0, in1=ht[:], op0=ALU.mult,
                                                                                     op1=ALU.mult, accum_out=fst[:, 0:1]), r=[r_h], w=[r_acc, r_fst])
                        kb.rsqrt_cols(fst[:, 2:3], r_fst, fst[:, 0:1], r_fst, 1, 1.0 / 1024, fst[:, 1:2], r_fst)
                        kb.dve(lambda e, ht=ht, fst=fst: e.tensor_scalar(out=ht[:], in0=ht[:], scalar1=fst[:, 2:3], scalar2=None, op0=ALU.mult),
                               r=[r_h, r_fst], w=[r_h])
                        kb.pool(lambda e, ht=ht: e.tensor_tensor(out=ht[:], in0=ht[:], in1=gfin[:], op=ALU.mult), r=[r_h, r_gfin], w=[r_h])
                    kb.store(h_dst[t * 128 - dst_row0:(t + 1) * 128 - dst_row0, :], ht[:], r_h)
                interleave((_epi(j, t) for j, t in enumerate(blk)), 2, 'moeepi')


def build_B(stage='all'):
    nc = bass.Bass("TRN2", target_bir_lowering=False)
    d = lambda name, shape: nc.dram_tensor(name, list(shape), F32, kind="ExternalInput").ap()
    hcat = d("hcat", [NT_ALL * 128, 1024])
    rope = d("rope64", [NT_ALL * 128, 2, 64])
    c = d("c", [1, 1024]); cctx = d("cctx", [1, 1024])
    ada_w = d("ada_w", [1024, 6144]); ada_b = d("ada_b", [1, 6144])
    gmix = d("gmix", [1, 1024]); gffn = d("gffn", [1, 1024])
    wdown = d("wdown", [1024, 704]); qln = d("qln", [1, 384]); kvln = d("kvln", [1, 256])
    wuq = d("wuq", [384, 1536]); wukv = d("wukv", [256, 2048]); wo = d("wo", [1024, 1024])
    router = d("router", [1024, 8]); w13 = d("mw13", [8, 1024, 7168]); w2 = d("mw2", [8, 3584, 1024]); ident = d("ident", [128, 128])
    hout = nc.dram_tensor("hout", [NT_Q * 128, 1024], F32, kind="ExternalOutput").ap()
    kb = KB(nc)
    with kb.gst:
        kb.setup_globals(ident)
        mod = kb.modulation(c, cctx, ada_w, ada_b, gmix, gffn, need_ab2=True)
        hm = kb.dram("hm1", [NT_Q * 128, 1024], F32)
        if stage == 'mixer':
            layer1_mixer(kb, hcat, rope, mod, wdown, qln, kvln, wuq, wukv, wo, hout)
        elif stage == 'moe':
            moe_pass(kb, hcat, hout, mod, router, w13, w2, ffn_blocks(NT_Q), tile_set)
        else:
            layer1_mixer(kb, hcat, rope, mod, wdown, qln, kvln, wuq, wukv, wo, hm)
            moe_pass(kb, hm, hout, mod, router, w13, w2, ffn_blocks(NT_Q), tile_set)
        kb.P.emit(nc)
    return nc


def run_B(inp, h1, hc1, layer=1, stage='all'):
    if "B" + stage not in _NC_CACHE:
        _NC_CACHE["B" + stage] = build_B(stage)
    nc = _NC_CACHE["B" + stage]
    f = lambda a: np.ascontiguousarray(np.asarray(a, dtype=np.float32))
    p = layer // 2
    maps = []
    for core in range(8):
        b, hf = core // 2, core % 2
        own, oth = core_tokens(hf)
        toks = np.concatenate([-np.ones(256, dtype=np.int64), own, oth])
        hcat = np.concatenate([hc1[b], h1[b][own], h1[b][oth]], axis=0)
        maps.append({
            "hcat": f(hcat), "rope64": rope_table(toks, 64),
            "c": f(inp["c"][b:b + 1]), "cctx": f(inp["c_ctx"][None, :]),
            "ada_w": f(inp["ada_w"][layer]), "ada_b": f(inp["ada_b"][layer:layer + 1]),
            "gmix": f(inp["norm_mix"][layer:layer + 1]), "gffn": f(inp["norm_ffn"][layer:layer + 1]),
            "wdown": f(inp["b_w_down"][0]), "qln": f(inp["b_q_lora_norm"][0:1]), "kvln": f(inp["b_kv_lora_norm"][0:1]),
            "wuq": f(inp["b_w_uq"][0]), "wukv": f(inp["b_w_ukv"][0]), "wo": f(inp["b_wo"][0]),
            "router": f(inp["moe_router"][p]), "mw13": f(inp["moe_w13"][p]), "mw2": f(inp["moe_w2"][p]),
            "ident": np.eye(128, dtype=np.float32),
        })
    res = run_bass_kernel_spmd(nc, maps, core_ids=list(range(8)))
    h2 = np.zeros((4, 8192, 1024), np.float32)
    hc2 = np.zeros((4, 256, 1024), np.float32)
    for core in range(8):
        b, hf = core // 2, core % 2
        o = res.results[core]["hout"]
        hc2[b] = o[0:256]
        h2[b, hf * 4096:(hf + 1) * 4096] = o[256:]
    return h2, hc2


def layer2_mixer(kb, h_src, mod, win_d, lng_d, lnb_d, wsp_d, bsp_d, wout_d, hm_dst, tiles, tile_set):
    NT = max(tiles) + 1
    O_s = kb.dram("c_O", [NT * 128, 1024], BF16)
    with kb.scope():
        win, r_win = kb.sb([128, 8, 2048], BF16, "win")
        load_weight(kb, win[:], r_win, win_d)
        lng, r_lng = kb.sb([128, 1024], F32, "lng")
        lnb, r_lnb = kb.sb([128, 1024], F32, "lnb")
        kb.load(lng[:], r_lng, lng_d.broadcast_to([128, 1024]))
        kb.load(lnb[:], r_lnb, lnb_d.broadcast_to([128, 1024]))
        bsbc, r_bsbc = kb.sb([128, 1024], F32, "bsbc")
        kb.load(bsbc[:], r_bsbc, bsp_d.broadcast_to([128, 1024]))
        bscol, r_bscol = kb.sb([128, 8], F32, "bscol")
        dtmp, r_dtmp = kb.sb([128, 8, 128], F32, "dtmp")
        kb.diag_extract(bscol[:], r_bscol, bsbc[:], r_bsbc, 8, dtmp, r_dtmp)
        wsp, r_wsp = kb.sb([128, 8, 128], BF16, "wsp")
        kb.P.dma("pool", wsp[:], wsp_d.rearrange("g p q -> p g q"), w=[r_wsp], sem=r_wsp)
        wsT, r_wsT = kb.sb([128, 8, 128], BF16, "wsT")
        pt, r_pt = kb.pb[6]
        ptb = pt[:].bitcast(BF16).rearrange("p (k j) -> p k j", j=128)
        for g in range(8):
            kb.pe(lambda e, g=g: e.transpose(out=ptb[:, g, :], in_=wsp[:, g, :], identity=kb.identb[:]), r=[r_wsp, kb.r_identb], w=[r_pt])
        kb.act(lambda e: e.activation(out=wsT[:], in_=ptb, func=AF.Copy), r=[r_pt], w=[r_wsT])
        fb = _front_bufs(kb, nh=2)
        aTrot = kb.sbrot(4, [128, 8, 128], BF16, "aT")
        urot = kb.sbrot(3, [128, 1024], BF16, "u")
        vrot = kb.sbrot(3, [128, 1024], F32, "v")
        vnrot = kb.sbrot(3, [128, 1024], BF16, "vn")
        strot = kb.sbrot(3, [128, 32], F32, "lnst")
        gtrot = kb.sbrot(3, [128, 1024], BF16, "gt")
        def _tile(t, par):
            rows = slice(t * 128, (t + 1) * 128)
            aT, r_aT = aTrot.next()
            yield from front(kb, fb, h_src[rows, :], mod, tile_set(t), 0, aT[:], r_aT, pbank=(3 + 4 * par) % 8)
            u, r_u = urot.next()
            v, r_v = vrot.next()
            for cb in range(4):
                pu, r_pu = kb.pb[(cb % 2 + 4 * par) % 8]
                for k in range(8):
                    kb.pe(lambda e, k=k, cb=cb, pu=pu, aT=aT: e.matmul(pu[:], lhsT=aT[:, k, :], rhs=win[:, k, cb * 512:(cb + 1) * 512],
                                                                      start=(k == 0), stop=(k == 7)), r=[r_aT, r_win], w=[r_pu])
                yield
                if cb < 2:
                    kb.act(lambda e, cb=cb, pu=pu, u=u: e.activation(out=u[:, cb * 512:(cb + 1) * 512], in_=pu[:], func=AF.Gelu), r=[r_pu], w=[r_u])
                else:
                    kb.act(lambda e, cb=cb, pu=pu, v=v: e.activation(out=v[:, (cb - 2) * 512:(cb - 1) * 512], in_=pu[:], func=AF.Gelu), r=[r_pu], w=[r_v])
            yield
            st, r_st = strot.next()
            for hb in range(2):
                kb.dve(lambda e, hb=hb, st=st, v=v: e.bn_stats(out=st[:, hb * 6:(hb + 1) * 6], in_=v[:, hb * 512:(hb + 1) * 512]), r=[r_v], w=[r_st])
            kb.dve(lambda e, st=st: e.bn_aggr(out=st[:, 12:14], in_=st[:, 0:12]), r=[r_st], w=[r_st])
            yield
            kb.rsqrt_cols(st[:, 16:17], r_st, st[:, 13:14], r_st, 1, 1.0, st[:, 15:16], r_st)
            kb.dve(lambda e, st=st, v=v: e.tensor_scalar(out=v[:], in0=v[:], scalar1=st[:, 12:13], scalar2=st[:, 16:17], op0=ALU.subtract, op1=ALU.mult),
                   r=[r_v, r_st], w=[r_v])
            yield
            kb.pool(lambda e, v=v: e.tensor_tensor(out=v[:], in0=v[:], in1=lng[:], op=ALU.mult), r=[r_v, r_lng], w=[r_v])
            yield
            vn, r_vn = vnrot.next()
            kb.dve(lambda e, v=v, vn=vn: e.tensor_tensor(out=vn[:], in0=v[:], in1=lnb[:], op=ALU.add), r=[r_v, r_lnb], w=[r_vn])
            yield
            gt, r_gt = gtrot.next()
            for hb in range(2):
                pm, r_pm = kb.pb[(2 + 4 * par) % 8]
                for gg in range(4):
                    g = hb * 4 + gg
                    kb.pe(lambda e, g=g, gg=gg, pm=pm, vn=vn: e.matmul(pm[:, gg * 128:(gg + 1) * 128], lhsT=wsT[:, g, :], rhs=vn[:, g * 128:(g + 1) * 128],
                                                                      start=(gg == 0), stop=True, skip_group_check=True), r=[r_wsT, r_vn], w=[r_pm])
                yield
                for gg in range(4):
                    g = hb * 4 + gg
                    kb.dve(lambda e, g=g, gg=gg, pm=pm, gt=gt, u=u: e.scalar_tensor_tensor(out=gt[:, g * 128:(g + 1) * 128], in0=pm[:, gg * 128:(gg + 1) * 128],
                                                                                          scalar=bscol[:, g:g + 1], in1=u[:, g * 128:(g + 1) * 128],
                                                                                          op0=ALU.add, op1=ALU.mult), r=[r_pm, r_bscol, r_u], w=[r_gt])
            kb.store(O_s[rows, :], gt[:], r_gt)
        interleave((_tile(t, i % 2) for i, t in enumerate(tiles)), 3, 'l2')
    outproj_pass(kb, O_s, wout_d, h_src, hm_dst, mod, tiles, tile_set)


def layer3_mixer(kb, h_src, rope64, mod, wqkv_d, sinks_d, wo_d, masks_d, hm_dst, ktile_src=None):
    NTK = 36
    if ktile_src is None:
        ktile_src = list(range(36))
    qtiles = list(range(2, 34))
    O_s = kb.dram("d_O", [34 * 128, 1024], BF16)
    with kb.scope():
        wqkv, r_wqkv = kb.sb([128, 8, 1280], BF16, "wqkv")
        load_weight(kb, wqkv[:], r_wqkv, wqkv_d)
        esink, r_esink = kb.sb([128, 16], F32, "esink")
        kb.load(esink[:], r_esink, sinks_d.broadcast_to([128, 16]))
        kb.act(lambda e: e.activation(out=esink[:], in_=esink[:], func=AF.Exp), r=[r_esink], w=[r_esink])
        mkf, r_mkf = kb.sb([128, 4, 128], F32, "mkf")
        kb.load(mkf[:], r_mkf, masks_d.rearrange("m k q -> k m q"))
        mk, r_mk = kb.sb([128, 4, 128], BF16, "mk")
        kb.dve(lambda e: e.tensor_copy(out=mk[:], in_=mkf[:]), r=[r_mkf], w=[r_mk])
        KT, r_KT = kb.sb([128, 2, NTK * 128], BF16, "KT")
        VV, r_VV = kb.sb([128, NTK, 2, 65], BF16, "VV")
        kb.pool(lambda e: e.memset(VV[:], 1.0), w=[r_VV])
        fb = _front_bufs(kb, nh=2)
        tb = norm_bufs(kb)
        aTrot = kb.sbrot(4, [128, 8, 128], BF16, "aT")
        csrot = kb.sbrot(4, [128, 2, 64], F32, "cs")
        kfrot = kb.sbrot(4, [128, 2, 64], F32, "kf")
        k16rot = kb.sbrot(4, [128, 2, 2, 64], BF16, "k16")
        qfrot = kb.sbrot(4, [128, 16, 64], F32, "qf")
        q16rot = kb.sbrot(2, [128, 16, 64], BF16, "q16")
        qTrot = kb.sbrot(4, [128, 8, 128], BF16, "qT")
        ptrot = kb.sbrot(3, [128, 640], BF16, "pT")
        otrot = kb.sbrot(2, [128, 16, 64], BF16, "ot")
        denrot = kb.sbrot(2, [128, 32], F32, "den")

        def bfview(bank):
            pt, r_pt = kb.pb[bank]
            return pt[:].bitcast(BF16).rearrange("p (k j) -> p k j", j=128), r_pt

        def _ktile(t, par):
            rows = slice(ktile_src[t] * 128, (ktile_src[t] + 1) * 128)
            aT, r_aT = aTrot.next()
            yield from front(kb, fb, h_src[rows, :], mod, tile_set(t), 0, aT[:], r_aT, pbank=(3 + 4 * par) % 8)
            cs, r_cs = csrot.next()
            kb.load(cs[:], r_cs, rope64[rows, :, :])
            pkv, r_pkv = kb.pb[(0 + 4 * par) % 8]
            for k in range(8):
                kb.pe(lambda e, k=k, aT=aT: e.matmul(pkv[:, 0:256], lhsT=aT[:, k, :], rhs=wqkv[:, k, 1024:1280], start=(k == 0), stop=(k == 7)),
                      r=[r_aT, r_wqkv], w=[r_pkv])
            yield
            kb.act(lambda e, t=t: e.activation(out=VV[:, t, :, 0:64], in_=pkv[:, 128:256].rearrange("p (h d) -> p h d", d=64), func=AF.Copy),
                   r=[r_pkv], w=[r_VV])
            kf, r_kf = kfrot.next()
            kb.act(lambda e, kf=kf: e.activation(out=kf[:], in_=pkv[:, 0:128].rearrange("p (h d) -> p h d", d=64), func=AF.Copy), r=[r_pkv], w=[r_kf])
            k16, r_k16 = k16rot.next()
            yield
            yield from qk_norm_rope(kb, kf[:], r_kf, 2, 64, None, (cs, r_cs), k16[:, :, 0, :], r_k16, tb)
            kb.dve(lambda e, k16=k16: e.tensor_copy(out=k16[:, :, 1, :], in_=k16[:, :, 0, :]), r=[r_k16], w=[r_k16])
            yield
            ptb, r_pt = bfview((3 + 4 * par) % 8)
            for kvh in range(2):
                kb.pe(lambda e, kvh=kvh, k16=k16, ptb=ptb: e.transpose(out=ptb[:, kvh, :], in_=k16[:, kvh, :, :].rearrange("p a d -> p (a d)"),
                                                                     identity=kb.identb[:]), r=[r_k16, kb.r_identb], w=[r_pt])
            yield
            kb.act(lambda e, t=t, ptb=ptb: e.activation(out=KT[:, :, t * 128:(t + 1) * 128], in_=ptb[:, 0:2, :], func=AF.Copy), r=[r_pt], w=[r_KT])
        interleave((_ktile(t, i % 2) for i, t in enumerate(range(NTK))), 3, 'l3k')
        def _qtile(t):
            rows = slice(t * 128, (t + 1) * 128)
            aT, r_aT = aTrot.next()
            yield from front(kb, fb, h_src[rows, :], mod, 0, 0, aT[:], r_aT)
            cs, r_cs = csrot.next()
            kb.load(cs[:], r_cs, rope64[rows, :, :])
            qf, r_qf = qfrot.next()
            for half in range(2):
                pq, r_pq = kb.pb[half]
                for k in range(8):
                    kb.pe(lambda e, k=k, aT=aT, pq=pq, half=half: e.matmul(pq[:], lhsT=aT[:, k, :], rhs=wqkv[:, k, half * 512:(half + 1) * 512],
                                                                          start=(k == 0), stop=(k == 7)), r=[r_aT, r_wqkv], w=[r_pq])
                kb.act(lambda e, qf=qf, pq=pq, half=half: e.activation(out=qf[:, half * 8:(half + 1) * 8, :],
                                                                       in_=pq[:].rearrange("p (h d) -> p h d", d=64), func=AF.Copy), r=[r_pq], w=[r_qf])
            q16, r_q16 = q16rot.next()
            yield from qk_norm_rope(kb, qf[:], r_qf, 16, 64, None, (cs, r_cs), q16[:], r_q16, tb)
            ptb, r_pt = bfview(6)
            for pr in range(8):
                kb.pe(lambda e, pr=pr, q16=q16, ptb=ptb: e.transpose(out=ptb[:, pr, :], in_=q16[:, 2 * pr:2 * pr + 2, :].rearrange("p a d -> p (a d)"),
                                                                   identity=kb.identb[:]), r=[r_q16, kb.r_identb], w=[r_pt])
            qT, r_qT = qTrot.next()
            kb.act(lambda e, qT=qT, ptb=ptb: e.activation(out=qT[:], in_=ptb, func=AF.Copy), r=[r_pt], w=[r_qT])
            left = t - 1 if t > 2 else 34
            right = t + 1 if t < 33 else 35
            ktl = [(0, None), (1, None), (left, 0 if t > 2 else 2), (t, None), (right, 1 if t < 33 else 3)]
            obanks = (2, 3, 4)
            for h in range(16):
                kvh, base = h // 8, (h % 2) * 64
                psA, r_psA = kb.pb[5]
                psB, r_psB = kb.pb[7]
                for ki, (kt, mi) in enumerate(ktl):
                    ps, r_ps, c0 = (psA, r_psA, ki * 128) if ki < 4 else (psB, r_psB, 0)
                    kb.pe(lambda e, ps=ps, c0=c0, kvh=kvh, base=base, kt=kt, qT=qT, h=h, ki=ki, mi=mi:
                          e.matmul(ps[:, c0:c0 + 128], lhsT=KT[base:base + 64, kvh, kt * 128:(kt + 1) * 128], rhs=qT[base:base + 64, h // 2, :],
                                   start=(ki == 0 or ki == 4), stop=(mi is None), skip_group_check=True),
                          r=[r_KT, r_qT], w=[r_ps])
                    if mi is not None:
                        kb.pe(lambda e, ps=ps, c0=c0, mi=mi: e.matmul(ps[:, c0:c0 + 128], lhsT=kb.identb[:], rhs=mk[:, mi, :], start=False, stop=True,
                                                                      skip_group_check=True), r=[kb.r_identb, r_mk], w=[r_ps])
                pT, r_pT = ptrot.next()
                kb.act(lambda e, pT=pT, psA=psA: e.activation(out=pT[:, 0:512], in_=psA[:], func=AF.Exp, scale=0.125), r=[r_psA], w=[r_pT])
                kb.act(lambda e, pT=pT, psB=psB: e.activation(out=pT[:, 512:640], in_=psB[:, 0:128], func=AF.Exp, scale=0.125), r=[r_psB], w=[r_pT])
                ob, r_ob = kb.pb[obanks[h // 7]]
                oc = (h % 7) * 65
                for ki, (kt, mi) in enumerate(ktl):
                    kb.pe(lambda e, ob=ob, oc=oc, pT=pT, ki=ki, kt=kt, kvh=kvh, h=h:
                          e.matmul(ob[:, oc:oc + 65], lhsT=pT[:, ki * 128:(ki + 1) * 128], rhs=VV[:, kt, kvh, :],
                                   start=(ki == 0 and h % 7 == 0), stop=(ki == 4), skip_group_check=True),
                          r=[r_pT, r_VV], w=[r_ob])
            ot, r_ot = otrot.next()
            den, r_den = denrot.next()
            for bi, (h0, nh_) in enumerate(((0, 7), (7, 7), (14, 2))):
                ob, r_ob = kb.pb[obanks[bi]]
                ov = ob[:, 0:nh_ * 65].rearrange("p (h d) -> p h d", d=65)
                kb.dve(lambda e, ov=ov, h0=h0, nh_=nh_, den=den: e.tensor_tensor(out=den[:, h0:h0 + nh_], in0=ov[:, :, 64], in1=esink[:, h0:h0 + nh_], op=ALU.add),
                       r=[r_ob, r_esink], w=[r_den])
                kb.dve(lambda e, h0=h0, nh_=nh_, den=den: e.reciprocal(out=den[:, 16 + h0:16 + h0 + nh_], in_=den[:, h0:h0 + nh_]), r=[r_den], w=[r_den])
                kb.dve(lambda e, ov=ov, h0=h0, nh_=nh_, den=den, ot=ot: e.tensor_tensor(out=ot[:, h0:h0 + nh_, :], in0=ov[:, :, 0:64],
                                                                                       in1=den[:, 16 + h0:16 + h0 + nh_].unsqueeze(2).broadcast_to([128, nh_, 64]),
                                                                                       op=ALU.mult), r=[r_ob, r_den], w=[r_ot])
            kb.store(O_s[rows, :], ot[:].rearrange("p h d -> p (h d)"), r_ot)
        interleave((_qtile(t) for t in qtiles), 1)
    outproj_pass(kb, O_s, wo_d, h_src, hm_dst, mod, qtiles, tile_set)


def tile_set_C(t):
    return 1 if t < 2 else 0


def build_C(debug=False):
    nc = bass.Bass("TRN2", target_bir_lowering=False)
    d = lambda name, shape: nc.dram_tensor(name, list(shape), F32, kind="ExternalInput").ap()
    hcat = d("hcat", [36 * 128, 1024])
    rope = d("rope64", [36 * 128, 2, 64])
    c = d("c", [1, 1024]); cctx = d("cctx", [1, 1024])
    ada_w2 = d("ada_w2", [1024, 6144]); ada_b2 = d("ada_b2", [1, 6144]); gmix2 = d("gmix2", [1, 1024]); gffn2 = d("gffn2", [1, 1024])
    ada_w3 = d("ada_w3", [1024, 6144]); ada_b3 = d("ada_b3", [1, 6144]); gmix3 = d("gmix3", [1, 1024]); gffn3 = d("gffn3", [1, 1024])
    win = d("c_win", [1024, 2048]); lng = d("c_lng", [1, 1024]); lnb = d("c_lnb", [1, 1024])
    wsp = d("c_wsp", [8, 128, 128]); bsp = d("c_bsp", [1, 1024]); wout = d("c_wout", [1024, 1024])
    w13 = d("w13", [1024, 5632]); w2 = d("w2", [2816, 1024])
    dwqkv = d("d_wqkv", [1024, 1280]); sinks = d("d_sinks", [1, 16]); dwo = d("d_wo", [1024, 1024]); masks = d("masks", [4, 128, 128])
    router = d("router", [1024, 8]); mw13 = d("mw13", [8, 1024, 7168]); mw2 = d("mw2", [8, 3584, 1024])
    gfin_d = d("gfin", [1, 1024]); ident = d("ident", [128, 128])
    out = nc.dram_tensor("out", [4096, 1024], F32, kind="ExternalOutput").ap()
    kb = KB(nc)
    with kb.gst:
        kb.setup_globals(ident)
        hm2 = kb.dram("hm2", [36 * 128, 1024], F32)
        h3s = kb.dram("h3s", [36 * 128, 1024], F32, kind=("ExternalOutput" if debug else "Internal"))
        hm3 = kb.dram("hm3", [34 * 128, 1024], F32, kind=("ExternalOutput" if debug else "Internal"))
        with kb.scope():
            mod2 = kb.modulation(c, cctx, ada_w2, ada_b2, gmix2, gffn2)
            layer2_mixer(kb, hcat, mod2, win, lng, lnb, wsp, bsp, wout, hm2, list(range(36)), tile_set_C)
            blocks = [[0, 1, 34, 35]] + [list(range(b0, b0 + 8)) for b0 in range(2, 34, 8)]
            ffn_dense_pass(kb, hm2, h3s, mod2, w13, w2, 2816, blocks, tile_set_C)
        with kb.scope():
            mod3 = kb.modulation(c, cctx, ada_w3, ada_b3, gmix3, gffn3, need_ab2=True)
            layer3_mixer(kb, h3s, rope, mod3, dwqkv, sinks, dwo, masks, hm3)
            gfin, r_gfin = kb.sb([128, 1024], F32, "gfin")
            kb.load(gfin[:], r_gfin, gfin_d.broadcast_to([128, 1024]))
            blocks = [list(range(b0, b0 + 8)) for b0 in range(2, 34, 8)]
            moe_pass(kb, hm3, out, mod3, router, mw13, mw2, blocks, tile_set_C, final=(gfin, r_gfin), dst_row0=256)
        kb.P.emit(nc)
    return nc


def band_masks(hf):
    k = np.arange(128)[:, None]
    q = np.arange(128)[None, :]
    NEG = np.float32(-30000.0)
    left = np.where(k >= q, 0.0, NEG).astype(np.float32)
    right = np.where(k <= q, 0.0, NEG).astype(np.float32)
    allm = np.full((128, 128), NEG, np.float32)
    return np.ascontiguousarray(np.stack([left, right, allm if hf == 0 else left, allm if hf == 1 else right]))


def run_C(inp, h2, hc2, debug=False):
    if ("C", debug) not in _NC_CACHE:
        _NC_CACHE[("C", debug)] = build_C(debug)
    nc = _NC_CACHE[("C", debug)]
    f = lambda a: np.ascontiguousarray(np.asarray(a, dtype=np.float32))
    maps = []
    for core in range(8):
        b, hf = core // 2, core % 2
        own = np.arange(hf * 4096, (hf + 1) * 4096)
        lh = np.arange(hf * 4096 - 128, hf * 4096)
        rh = np.arange((hf + 1) * 4096, (hf + 1) * 4096 + 128)
        lh_ok, rh_ok = lh[0] >= 0, rh[-1] < 8192
        hl = h2[b][lh] if lh_ok else np.zeros((128, 1024), np.float32)
        hr = h2[b][rh] if rh_ok else np.zeros((128, 1024), np.float32)
        toks = np.concatenate([-np.ones(256, dtype=np.int64), own, lh if lh_ok else -np.ones(128, dtype=np.int64),
                               rh if rh_ok else -np.ones(128, dtype=np.int64)])
        hcat = np.concatenate([hc2[b], h2[b][own], hl, hr], axis=0)
        maps.append({
            "hcat": f(hcat), "rope64": rope_table(toks, 64),
            "c": f(inp["c"][b:b + 1]), "cctx": f(inp["c_ctx"][None, :]),
            "ada_w2": f(inp["ada_w"][2]), "ada_b2": f(inp["ada_b"][2:3]), "gmix2": f(inp["norm_mix"][2:3]), "gffn2": f(inp["norm_ffn"][2:3]),
            "ada_w3": f(inp["ada_w"][3]), "ada_b3": f(inp["ada_b"][3:4]), "gmix3": f(inp["norm_mix"][3:4]), "gffn3": f(inp["norm_ffn"][3:4]),
            "c_win": f(inp["c_w_in"][0]), "c_lng": f(inp["c_ln_g"][0:1]), "c_lnb": f(inp["c_ln_b"][0:1]),
            "c_wsp": f(inp["c_w_spatial"][0]), "c_bsp": f(inp["c_b_spatial"][0].reshape(1, 1024)), "c_wout": f(inp["c_w_out"][0]),
            "w13": f(inp["ffn_w13"][1]), "w2": f(inp["ffn_w2"][1]),
            "d_wqkv": f(inp["d_wqkv"][0]), "d_sinks": f(inp["d_sinks"][0:1]), "d_wo": f(inp["d_wo"][0]), "masks": band_masks(hf),
            "router": f(inp["moe_router"][1]), "mw13": f(inp["moe_w13"][1]), "mw2": f(inp["moe_w2"][1]),
            "gfin": f(inp["final_norm"][None, :]), "ident": np.eye(128, dtype=np.float32),
        })
    res = run_bass_kernel_spmd(nc, maps, core_ids=list(range(8)))
    out = np.zeros((4, 8192, 1024), np.float32)
    for core in range(8):
        b, hf = core // 2, core % 2
        out[b, hf * 4096:(hf + 1) * 4096] = res.results[core]["out"]
    if debug:
        return out, res.results
    return out


def kernel(**inputs):
    inp = {k: np.asarray(v) for k, v in inputs.items()}
    return run_fused(inp)


def tile_set_F(t):
    return 1 if t < 2 else 0


def build_fused():
    nc = bass.Bass("TRN2", target_bir_lowering=False)
    d = lambda name, shape: nc.dram_tensor(name, list(shape), F32, kind="ExternalInput").ap()
    xcat = d("xcat", [66 * 128, 1024])
    rope128 = d("rope128", [66 * 128, 2, 128])
    rope64 = d("rope64", [66 * 128, 2, 64])
    c = d("c", [1, 1024]); cctx = d("cctx", [1, 1024])
    ada_w = d("ada_w", [4, 1024, 6144]); ada_b = d("ada_b", [4, 6144]); gmix = d("gmix", [4, 1024]); gffn = d("gffn", [4, 1024])
    a_wqkv = d("a_wqkv", [1024, 1536]); a_qn = d("a_qn", [1, 128]); a_kn = d("a_kn", [1, 128]); a_wo = d("a_wo", [1024, 1024])
    b_wdown = d("b_wdown", [1024, 704]); b_qln = d("b_qln", [1, 384]); b_kvln = d("b_kvln", [1, 256])
    b_wuq = d("b_wuq", [384, 1536]); b_wukv = d("b_wukv", [256, 2048]); b_wo = d("b_wo", [1024, 1024])
    c_win = d("c_win", [1024, 2048]); c_lng = d("c_lng", [1, 1024]); c_lnb = d("c_lnb", [1, 1024])
    c_wsp = d("c_wsp", [8, 128, 128]); c_bsp = d("c_bsp", [1, 1024]); c_wout = d("c_wout", [1024, 1024])
    d_wqkv = d("d_wqkv", [1024, 1280]); d_sinks = d("d_sinks", [1, 16]); d_wo = d("d_wo", [1024, 1024]); masks = d("masks", [4, 128, 128])
    ffn_w13 = d("ffn_w13", [2, 1024, 5632]); ffn_w2 = d("ffn_w2", [2, 2816, 1024])
    router = d("router", [2, 1024, 8]); mw13 = d("mw13", [2, 8, 1024, 7168]); mw2 = d("mw2", [2, 8, 3584, 1024])
    gfin_d = d("gfin", [1, 1024]); ident = d("ident", [128, 128])
    out = nc.dram_tensor("out", [4096, 1024], F32, kind="ExternalOutput").ap()
    kb = KB(nc)
    halo = [34, 65]
    lat36 = list(range(2, 34)) + halo
    with kb.gst:
        kb.setup_globals(ident)
        hm0 = kb.dram("hm0", [66 * 128, 1024], F32)
        h1 = kb.dram("h1", [66 * 128, 1024], F32)
        hm1 = kb.dram("hm1", [66 * 128, 1024], F32)
        h2 = kb.dram("h2", [66 * 128, 1024], F32)
        hm2 = kb.dram("hm2", [66 * 128, 1024], F32)
        h3 = kb.dram("h3", [66 * 128, 1024], F32)
        hm3 = kb.dram("hm3", [34 * 128, 1024], F32)
        mk = lambda l, ab2: kb.modulation(c, cctx, ada_w[l], ada_b[l:l + 1, :], gmix[l:l + 1, :], gffn[l:l + 1, :], need_ab2=ab2)
        with kb.scope():
            mod = mk(0, False)
            layer0_mixer(kb, xcat, rope128, mod, a_wqkv, a_qn, a_kn, a_wo, hm0, nq_tiles=66, nk_tiles=66)
            blocks = [[0, 1]] + [list(range(b0, b0 + 8)) for b0 in range(2, 66, 8)]
            ffn_dense_pass(kb, hm0, h1, mod, ffn_w13[0], ffn_w2[0], 2816, blocks, tile_set_F)
        with kb.scope():
            mod = mk(1, True)
            layer1_mixer(kb, h1, rope64, mod, b_wdown, b_qln, b_kvln, b_wuq, b_wukv, b_wo, hm1, nk_tiles=66, qtiles=[0, 1] + lat36)
            blocks = [[0, 1], lat36[0:9], lat36[9:18], lat36[18:26], lat36[26:34]]
            moe_pass(kb, hm1, h2, mod, router[0], mw13[0], mw2[0], blocks, tile_set_F)
        with kb.scope():
            mod = mk(2, False)
            layer2_mixer(kb, h2, mod, c_win, c_lng, c_lnb, c_wsp, c_bsp, c_wout, hm2, [0, 1] + lat36, tile_set_F)
            blocks = [[0, 1] + halo] + [list(range(b0, b0 + 8)) for b0 in range(2, 34, 8)]
            ffn_dense_pass(kb, hm2, h3, mod, ffn_w13[1], ffn_w2[1], 2816, blocks, tile_set_F)
        with kb.scope():
            mod = mk(3, True)
            layer3_mixer(kb, h3, rope64, mod, d_wqkv, d_sinks, d_wo, masks, hm3, ktile_src=list(range(34)) + [65, 34])
            gfin, r_gfin = kb.sb([128, 1024], F32, "gfin")
            kb.load(gfin[:], r_gfin, gfin_d.broadcast_to([128, 1024]))
            blocks = [list(range(b0, b0 + 8)) for b0 in range(2, 34, 8)]
            moe_pass(kb, hm3, out, mod, router[1], mw13[1], mw2[1], blocks, tile_set_F, final=(gfin, r_gfin), dst_row0=256)
        kb.P.emit(nc)
    return nc


def run_fused(inp):
    if "F" not in _NC_CACHE:
        _NC_CACHE["F"] = build_fused()
    nc = _NC_CACHE["F"]
    f = lambda a: np.ascontiguousarray(np.asarray(a, dtype=np.float32))
    shared = {
        "cctx": f(inp["c_ctx"][None, :]), "ada_w": f(inp["ada_w"]), "ada_b": f(inp["ada_b"]), "gmix": f(inp["norm_mix"]), "gffn": f(inp["norm_ffn"]),
        "a_wqkv": f(inp["a_wqkv"][0]), "a_qn": f(inp["a_q_norm"][0:1]), "a_kn": f(inp["a_k_norm"][0:1]), "a_wo": f(inp["a_wo"][0]),
        "b_wdown": f(inp["b_w_down"][0]), "b_qln": f(inp["b_q_lora_norm"][0:1]), "b_kvln": f(inp["b_kv_lora_norm"][0:1]),
        "b_wuq": f(inp["b_w_uq"][0]), "b_wukv": f(inp["b_w_ukv"][0]), "b_wo": f(inp["b_wo"][0]),
        "c_win": f(inp["c_w_in"][0]), "c_lng": f(inp["c_ln_g"][0:1]), "c_lnb": f(inp["c_ln_b"][0:1]),
        "c_wsp": f(inp["c_w_spatial"][0]), "c_bsp": f(inp["c_b_spatial"][0].reshape(1, 1024)), "c_wout": f(inp["c_w_out"][0]),
        "d_wqkv": f(inp["d_wqkv"][0]), "d_sinks": f(inp["d_sinks"][0:1]), "d_wo": f(inp["d_wo"][0]),
        "ffn_w13": f(inp["ffn_w13"]), "ffn_w2": f(inp["ffn_w2"]),
        "router": f(inp["moe_router"]), "mw13": f(inp["moe_w13"]), "mw2": f(inp["moe_w2"]),
        "gfin": f(inp["final_norm"][None, :]), "ident": np.eye(128, dtype=np.float32),
    }
    maps = []
    for core in range(8):
        b, hf = core // 2, core % 2
        own, oth = core_tokens(hf)
        toks = np.concatenate([-np.ones(256, dtype=np.int64), own, oth])
        xcat = np.concatenate([inp["ctx"][b], inp["x"][b][own], inp["x"][b][oth]], axis=0)
        m = dict(shared)
        m.update({"xcat": f(xcat), "rope128": rope_table(toks, 128), "rope64": rope_table(toks, 64),
                  "c": f(inp["c"][b:b + 1]), "masks": band_masks(hf)})
        maps.append(m)
    res = run_bass_kernel_spmd(nc, maps, core_ids=list(range(8)))
    out = np.zeros((4, 8192, 1024), np.float32)
    for core in range(8):
        b, hf = core // 2, core % 2
        out[b, hf * 4096:(hf + 1) * 4096] = res.results[core]["out"]
    return out


def kernel_unfused(**inputs):
    inp = {k: np.asarray(v) for k, v in inputs.items()}
    h1, hc1 = run_A(inp)
    h2, hc2 = run_B(inp, h1, hc1)
    return run_C(inp, h2, hc2)
```
